# Optimizing a Trainium2 kernel written in Bass

```python
import math
import jax, jax.numpy as jnp
from jax import lax
import numpy as np

D_MODEL = 1024
BATCH = 8
SEQ = 4096
DEPTH = 2

A_HEADS = 4
A_HEAD_DIM = 64
A_WIDTH = A_HEADS * A_HEAD_DIM
IDX_HEADS = 4
IDX_DIM = 64
TOPK_MAX = 256
B_GROUPS = 4
B_WIDTH = 256
B_GROUP_DIM = B_WIDTH // B_GROUPS
CHUNK = 128
C_HEADS = 4
C_HEAD_DIM = 32
C_V_DIM = 2 * C_HEAD_DIM
C_WIDTH = C_HEADS * C_V_DIM
N_BRANCH = 3
D_FF = 4 * D_MODEL
Q_BLOCK = 128
EPS = 1e-6
NEG = -1e30

IN_SIZES = (
    A_WIDTH, A_WIDTH, A_WIDTH,
    IDX_HEADS * IDX_DIM, IDX_DIM, IDX_HEADS,
    2 * B_WIDTH,
    C_HEADS * 2 * C_HEAD_DIM,
    C_HEADS * 2 * C_HEAD_DIM,
    C_WIDTH,
    N_BRANCH * D_MODEL,
)
N_IN = sum(IN_SIZES)

kernel_name = "hybrid_dsa_gmlp_diffattn_gated"


def rms_norm(x, g):
    xf = x.astype(jnp.float32)
    y = xf * lax.rsqrt(jnp.mean(xf * xf, axis=-1, keepdims=True) + EPS)
    return (y * g.astype(jnp.float32)).astype(x.dtype)


def layer_norm(x, g, b):
    xf = x.astype(jnp.float32)
    mu = jnp.mean(xf, axis=-1, keepdims=True)
    xc = xf - mu
    y = xc * lax.rsqrt(jnp.mean(xc * xc, axis=-1, keepdims=True) + EPS)
    return (y * g.astype(jnp.float32) + b.astype(jnp.float32)).astype(x.dtype)


def split_cols(t):
    offs = np.cumsum(np.array(IN_SIZES))[:-1].tolist()
    return jnp.split(t, offs, axis=-1)


def dsa_attention(q, k, v, q_idx, k_idx, w_idx):
    bsz, seq = q.shape[0], q.shape[1]
    n_keys = seq
    top_k = min(TOPK_MAX, n_keys // 4)
    key_pos = jnp.arange(n_keys)
    idx_scale = IDX_DIM ** -0.5
    w_scale = IDX_HEADS ** -0.5
    attn_scale = A_HEAD_DIM ** -0.5

    def block(i):
        t0 = i * Q_BLOCK
        qb = lax.dynamic_slice_in_dim(q, t0, Q_BLOCK, axis=1)
        qib = lax.dynamic_slice_in_dim(q_idx, t0, Q_BLOCK, axis=1)
        wb = lax.dynamic_slice_in_dim(w_idx, t0, Q_BLOCK, axis=1)
        q_pos = t0 + jnp.arange(Q_BLOCK)
        causal = key_pos[None, :] <= q_pos[:, None]
        dots = jnp.einsum('bqhd,bsd->bqhs', qib, k_idx).astype(jnp.float32) * idx_scale
        score = jnp.einsum('bqh,bqhs->bqs', wb.astype(jnp.float32) * w_scale, jax.nn.relu(dots))
        score = jnp.where(causal[None], score, -jnp.inf)
        _, sel = lax.top_k(score, top_k)
        valid = sel <= q_pos[None, :, None]
        kg = jax.vmap(lambda kb, ib: kb[ib])(k, sel)
        vg = jax.vmap(lambda vb, ib: vb[ib])(v, sel)
        logits = jnp.einsum('bqhd,bqkhd->bqhk', qb, kg).astype(jnp.float32) * attn_scale
        logits = jnp.where(valid[:, :, None, :], logits, NEG)
        p = jax.nn.softmax(logits, axis=-1).astype(v.dtype)
        return jnp.einsum('bqhk,bqkhd->bqhd', p, vg)

    out = lax.map(block, jnp.arange(seq // Q_BLOCK))
    return out.transpose(1, 0, 2, 3, 4).reshape(bsz, seq, A_WIDTH)


def chunked_sgu(u, v, ln_g, ln_b, w_s, b_s):
    bsz, seq = u.shape[0], u.shape[1]
    vn = layer_norm(v, ln_g, ln_b)
    vc = vn.reshape(bsz, seq // CHUNK, CHUNK, B_GROUPS, B_GROUP_DIM)
    w_causal = w_s * jnp.tril(jnp.ones((CHUNK, CHUNK), w_s.dtype))
    s = jnp.einsum('gts,bcsgd->bctgd', w_causal, vc) + b_s.T[None, None, :, :, None]
    return u * s.reshape(bsz, seq, B_WIDTH)


def diff_attention(q, k, v, lam, lambda_init, subln_g):
    bsz, seq = q.shape[0], q.shape[1]
    key_pos = jnp.arange(seq)
    scale = C_HEAD_DIM ** -0.5

    def block(i):
        t0 = i * Q_BLOCK
        qb = lax.dynamic_slice_in_dim(q, t0, Q_BLOCK, axis=1)
        q_pos = t0 + jnp.arange(Q_BLOCK)
        causal = key_pos[None, :] <= q_pos[:, None]
        logits = jnp.einsum('bqhcd,bshcd->bhcqs', qb, k).astype(jnp.float32) * scale
        logits = jnp.where(causal[None, None, None], logits, NEG)
        p = jax.nn.softmax(logits, axis=-1)
        a = p[:, :, 0] - lam * p[:, :, 1]
        return jnp.einsum('bhqs,bshe->bqhe', a.astype(v.dtype), v)

    out = lax.map(block, jnp.arange(seq // Q_BLOCK))
    out = out.transpose(1, 0, 2, 3, 4).reshape(bsz, seq, C_HEADS, C_V_DIM)
    out = rms_norm(out, subln_g) * (1.0 - lambda_init)
    return out.reshape(bsz, seq, C_WIDTH)


def setup_inputs(seed: int = 0) -> dict:
    key = jax.random.key(seed)
    ks = jax.random.split(key, 20)
    f32 = jnp.float32

    def nrm(k, shape, scale):
        return jax.random.normal(k, shape, f32) * scale

    def gain(k, shape):
        return 1.0 + 0.01 * jax.random.normal(k, shape, f32)

    return {
        "x": nrm(ks[0], (BATCH, SEQ, D_MODEL), 1.0),
        "attn_norm_g": gain(ks[1], (DEPTH, D_MODEL)),
        "w_in": nrm(ks[2], (DEPTH, D_MODEL, N_IN), D_MODEL ** -0.5),
        "idx_k_norm_g": gain(ks[3], (DEPTH, IDX_DIM)),
        "idx_k_norm_b": nrm(ks[4], (DEPTH, IDX_DIM), 0.01),
        "sgu_norm_g": gain(ks[5], (DEPTH, B_WIDTH)),
        "sgu_norm_b": nrm(ks[6], (DEPTH, B_WIDTH), 0.01),
        "sgu_w_s": nrm(ks[7], (DEPTH, B_GROUPS, CHUNK, CHUNK), CHUNK ** -0.5),
        "sgu_b_s": gain(ks[8], (DEPTH, B_GROUPS, CHUNK)),
        "diff_lambda": nrm(ks[9], (DEPTH, 4, C_HEAD_DIM), 0.1),
        "diff_subln_g": gain(ks[10], (DEPTH, C_V_DIM)),
        "w_branch_a": nrm(ks[11], (DEPTH, A_WIDTH, D_MODEL), A_WIDTH ** -0.5),
        "w_branch_b": nrm(ks[12], (DEPTH, B_WIDTH, D_MODEL), B_WIDTH ** -0.5),
        "w_branch_c": nrm(ks[13], (DEPTH, C_WIDTH, D_MODEL), C_WIDTH ** -0.5),
        "w_out": nrm(ks[14], (DEPTH, D_MODEL, D_MODEL), D_MODEL ** -0.5),
        "mlp_norm_g": gain(ks[15], (DEPTH, D_MODEL)),
        "w_ff1": nrm(ks[16], (DEPTH, D_MODEL, D_FF), D_MODEL ** -0.5),
        "w_ff2": nrm(ks[17], (DEPTH, D_FF, D_MODEL), D_FF ** -0.5),
        "final_norm_g": gain(ks[18], (D_MODEL,)),
    }


def reference(x, attn_norm_g, w_in, idx_k_norm_g, idx_k_norm_b, sgu_norm_g, sgu_norm_b,
              sgu_w_s, sgu_b_s, diff_lambda, diff_subln_g, w_branch_a, w_branch_b,
              w_branch_c, w_out, mlp_norm_g, w_ff1, w_ff2, final_norm_g):
    bsz, seq = x.shape[0], x.shape[1]
    for l in range(DEPTH):
        lambda_init = 0.8 - 0.6 * math.exp(-0.3 * l)
        h = rms_norm(x, attn_norm_g[l])
        proj = h @ w_in[l]
        (a_q, a_k, a_v, i_q, i_k, i_w, b_uv, c_q, c_k, c_v, gates) = split_cols(proj)

        y_a = dsa_attention(
            a_q.reshape(bsz, seq, A_HEADS, A_HEAD_DIM),
            a_k.reshape(bsz, seq, A_HEADS, A_HEAD_DIM),
            a_v.reshape(bsz, seq, A_HEADS, A_HEAD_DIM),
            i_q.reshape(bsz, seq, IDX_HEADS, IDX_DIM),
            layer_norm(i_k, idx_k_norm_g[l], idx_k_norm_b[l]),
            i_w)

        b_act = jax.nn.gelu(b_uv)
        u, v = jnp.split(b_act, 2, axis=-1)
        y_b = chunked_sgu(u, v, sgu_norm_g[l], sgu_norm_b[l], sgu_w_s[l], sgu_b_s[l])

        lq = diff_lambda[l].astype(jnp.float32)
        lam = jnp.exp(jnp.sum(lq[0] * lq[1])) - jnp.exp(jnp.sum(lq[2] * lq[3])) + lambda_init
        y_c = diff_attention(
            c_q.reshape(bsz, seq, C_HEADS, 2, C_HEAD_DIM),
            c_k.reshape(bsz, seq, C_HEADS, 2, C_HEAD_DIM),
            c_v.reshape(bsz, seq, C_HEADS, C_V_DIM),
            lam, lambda_init, diff_subln_g[l])

        g = jax.nn.sigmoid(gates).reshape(bsz, seq, N_BRANCH, D_MODEL)
        merged = (g[:, :, 0] * (y_a @ w_branch_a[l])
                  + g[:, :, 1] * (y_b @ w_branch_b[l])
                  + g[:, :, 2] * (y_c @ w_branch_c[l]))
        x = x + merged @ w_out[l]

        h2 = rms_norm(x, mlp_norm_g[l])
        x = x + jnp.square(jax.nn.relu(h2 @ w_ff1[l])) @ w_ff2[l]
    return rms_norm(x, final_norm_g)
```

```python
import math, contextlib
import numpy as np
import ml_dtypes
import concourse.bass as bass
import concourse.mybir as mybir
from concourse.bass_utils import run_bass_kernel_spmd

F32 = mybir.dt.float32; BF16 = mybir.dt.bfloat16; U8 = mybir.dt.uint8
AF = mybir.ActivationFunctionType; ALU = mybir.AluOpType; AX = mybir.AxisListType

D = 1024; SEQ = 4096; NL = 2; NIN = 5444; DFF = 4096
EPS = 1e-6
NBIS = 24
RNG = 32.0
MASKV = -30000.0
FM_AQ, FM_AK, FM_IQ, FM_CQ, FM_CK, FM_G = 0, 256, 512, 768, 1024, 1280
TM_AV, TM_IK, TM_IW, TM_CV, TM_BUV = 4352, 4608, 4672, 4676, 4932


class Trk:
    __slots__ = ("w", "r", "excl")

    def __init__(self, excl=False):
        self.w = {}; self.r = {}; self.excl = excl


class Eng:
    def __init__(self, key, h, sem, inorder=False):
        self.key = key; self.h = h; self.sem = sem; self.count = 0; self.seen = {}; self.inorder = inorder


class Sync:
    def __init__(self, nc, es, ndma=24):
        self.nc = nc
        self.sems = {}

        def mk(key, h, inorder=False):
            s = es.enter_context(nc.semaphore("s_" + key))
            self.sems[key] = s
            return Eng(key, h, s, inorder)
        self.pe = mk("pe", nc.tensor, True)
        self.act = mk("act", nc.scalar)
        self.dve = mk("dve", nc.vector)
        self.pool = mk("pool", nc.gpsimd)
        self.sp = Eng("sp", nc.sync, None)
        self.dsem = []
        for k in range(ndma):
            s = es.enter_context(nc.semaphore("s_d%d" % k))
            self.sems[("d", k)] = s
            self.dsem.append([s, 0])
        self.dnext = 0
        self.ninstr = 0

    def _waits(self, eng, r, w, wa):
        waits = {}
        for t in r:
            for k, v in t.w.items():
                if v > waits.get(k, 0): waits[k] = v
        for t in w:
            for k, v in t.w.items():
                if v > waits.get(k, 0): waits[k] = v
            for k, v in t.r.items():
                if v > waits.get(k, 0): waits[k] = v
        for t in wa:
            for k, v in t.r.items():
                if v > waits.get(k, 0): waits[k] = v
        for k, v in waits.items():
            if k == eng.key and eng.inorder: continue
            if eng.seen.get(k, 0) >= v: continue
            eng.h.wait_ge(self.sems[k], v)
            eng.seen[k] = v

    def op(self, eng, fn, r=(), w=(), wa=()):
        if any(t.excl for t in r):
            w = list(w) + [t for t in r if t.excl]
            r = [t for t in r if not t.excl]
        self._waits(eng, r, w, wa)
        ins = fn()
        eng.count += 1
        ins.then_inc(eng.sem, 1)
        self.ninstr += 1
        for t in r: t.r[eng.key] = eng.count
        for t in w: t.w[eng.key] = eng.count
        for t in wa: t.w[eng.key] = eng.count
        return ins

    def dma(self, out, in_, r=(), w=(), wa=(), **kw):
        q = self.sp
        self._waits(q, r, w, wa)
        k = self.dnext; self.dnext = (self.dnext + 1) % len(self.dsem)
        s, c = self.dsem[k]
        key = ("d", k)
        if c > 0 and q.seen.get(key, 0) < c:
            q.h.wait_ge(s, c); q.seen[key] = c
        q.h.dma_start(out=out, in_=in_, **kw).then_inc(s, 16)
        c += 16
        self.dsem[k][1] = c
        self.ninstr += 1
        for t in r: t.r[key] = c
        for t in w: t.w[key] = c
        for t in wa: t.w[key] = c

    def barrier(self):
        engs = [self.pe, self.act, self.dve, self.pool, self.sp]
        for e in engs:
            for o in [self.pe, self.act, self.dve, self.pool]:
                if o is e or o.count == 0: continue
                if e.seen.get(o.key, 0) < o.count:
                    e.h.wait_ge(o.sem, o.count); e.seen[o.key] = o.count
            for k, (s, c) in enumerate(self.dsem):
                if c and e.seen.get(("d", k), 0) < c:
                    e.h.wait_ge(s, c); e.seen[("d", k)] = c


class _Stop(Exception):
    pass


def build_nc(n_layers=NL, n_tiles=8, dbg=False, stop=None):
    nc = bass.Bass("TRN2", target_bir_lowering=False, dynamic_dma_scratch_size=512)
    es = contextlib.ExitStack()
    S = Sync(nc, es)

    def din(name, shape):
        return nc.dram_tensor(name, list(shape), F32, kind="ExternalInput").ap()
    x_in = din("x", [SEQ, D])
    attn_norm_g = din("attn_norm_g", [NL, D]); w_in = din("w_in", [NL, D, NIN])
    idx_g = din("idx_k_norm_g", [NL, 64]); idx_b = din("idx_k_norm_b", [NL, 64])
    sgu_g = din("sgu_norm_g", [NL, 256]); sgu_b = din("sgu_norm_b", [NL, 256])
    sgu_w = din("sgu_w_s", [NL, 4, 128, 128]); sgu_bs = din("sgu_b_s", [NL, 4, 128])
    dlam = din("diff_lambda", [NL, 4, 32]); subln = din("diff_subln_g", [NL, 64])
    wbrs = [din("w_branch_a", [NL, 256, D]), din("w_branch_b", [NL, 256, D]), din("w_branch_c", [NL, 256, D])]
    w_out = din("w_out", [NL, D, D]); mlp_norm_g = din("mlp_norm_g", [NL, D])
    w_ff1 = din("w_ff1", [NL, D, DFF]); w_ff2 = din("w_ff2", [NL, DFF, D])
    fin_g = din("final_norm_g", [1, D])
    consts = din("consts", [128, 512]); rampv = din("rampv", [1, SEQ])
    out = nc.dram_tensor("out", [SEQ, D], F32, kind="ExternalOutput").ap()

    def dscr(name, shape, dt):
        return nc.dram_tensor(name, list(shape), dt, kind="Internal").ap()
    win_bf = [dscr("win_bf%d" % l, [8, 128, NIN], BF16) for l in range(NL)]
    wff1_bf = [dscr("wff1_bf%d" % l, [8, 128, DFF], BF16) for l in range(NL)]
    wff2_bf = [dscr("wff2_bf%d" % l, [32, 128, D], BF16) for l in range(NL)]
    wout_bf = [dscr("wout_bf%d" % l, [8, 128, D], BF16) for l in range(NL)]
    wbr_bf = [dscr("wbr_bf%d" % l, [6, 128, D], BF16) for l in range(NL)]
    xs = dscr("xs", [SEQ, D], F32)
    t_prep = [Trk() for _ in range(NL)]
    t_xs = [Trk() for _ in range(8)]
    t_in = Trk()

    def sb(name, shape, dt, stack=es):
        return stack.enter_context(nc.sbuf_tensor(name, list(shape), dt))

    pe, act, dve, pool = S.pe, S.act, S.dve, S.pool

    cst = sb("cst", [128, 512], F32); t_cst = Trk()
    ident_bf = sb("ident_bf", [128, 128], BF16)
    cmb_bf = sb("cmb_bf", [128, 128], BF16)
    neghalf = sb("neghalf", [128, 1], F32)
    gpre = sb("gpre", [128, 2 * NL * 8], F32); t_gpre = Trk()
    S.dma(cst[:], consts[:, :], r=[t_in], w=[t_cst])
    causal_big = cst[:, 128:256]
    tril = cst[:, 384:512]
    S.op(dve, lambda: nc.vector.tensor_copy(out=ident_bf[:], in_=cst[:, 0:128]), r=[t_cst], w=[t_cst])
    S.op(dve, lambda: nc.vector.tensor_copy(out=cmb_bf[:], in_=cst[:, 256:384]), r=[t_cst], w=[t_cst])
    S.op(pool, lambda: nc.gpsimd.memset(neghalf[:], -0.5), w=[t_cst])
    for l in range(NL):
        S.dma(gpre[:, l * 16:l * 16 + 8], attn_norm_g[l].rearrange("(k p) -> p k", p=128), r=[t_in], wa=[t_gpre],
              allow_slow_non_contiguous=True)
        S.dma(gpre[:, l * 16 + 8:l * 16 + 16], mlp_norm_g[l].rearrange("(k p) -> p k", p=128), r=[t_in], wa=[t_gpre],
              allow_slow_non_contiguous=True)

    with contextlib.ExitStack() as pes:
        NB = 3
        pin = [sb("pin%d" % i, [128, 1024], F32, pes) for i in range(NB)]
        pout = [sb("pout%d" % i, [128, 1024], BF16, pes) for i in range(NB)]
        t_pin = [Trk() for _ in range(NB)]; t_pout = [Trk() for _ in range(NB)]
        cnt = [0]

        def piece(l, src_ap, dst_ap, w, gcol, dst_view=None):
            i = cnt[0] % NB; cnt[0] += 1
            S.dma(pin[i][:, 0:w], src_ap, r=[t_in], w=[t_pin[i]])
            e = [dve, act, pool][cnt[0] % 3]
            if gcol is None:
                if e is act:
                    fn = lambda: nc.scalar.copy(out=pout[i][:, 0:w], in_=pin[i][:, 0:w])
                else:
                    fn = lambda: e.h.tensor_copy(out=pout[i][:, 0:w], in_=pin[i][:, 0:w])
            else:
                g = gpre[:, gcol:gcol + 1]
                if e is act:
                    fn = lambda: nc.scalar.activation(out=pout[i][:, 0:w], in_=pin[i][:, 0:w], func=AF.Copy, scale=g)
                else:
                    fn = lambda: e.h.tensor_scalar(out=pout[i][:, 0:w], in0=pin[i][:, 0:w], scalar1=g, scalar2=None,
                                                   op0=ALU.mult)
            S.op(e, fn, r=[t_pin[i], t_gpre], w=[t_pout[i]])
            src = pout[i][:, 0:w] if dst_view is None else dst_view(pout[i])
            S.dma(dst_ap, src, r=[t_pout[i]], wa=[t_prep[l]])

        segs = [(0, FM_AQ, 512), (768, FM_IQ, 256), (1604, FM_CQ, 512), (512, TM_AV, 256), (1024, TM_IK, 68),
                (2116, TM_CV, 256), (1092, TM_BUV, 512)]
        for l in range(n_layers):
            for kc in range(8):
                rows = slice(kc * 128, (kc + 1) * 128)
                for (s0, d0, w) in segs:
                    piece(l, w_in[l, rows, s0:s0 + w], win_bf[l][kc, :, d0:d0 + w], w, l * 16 + kc)
                for i in range(3):
                    dst = win_bf[l][kc, :, FM_G:FM_G + 3072].rearrange("p (f i c) -> p f i c", f=8, i=3)[:, :, i, :]
                    piece(l, w_in[l, rows, 2372 + i * 1024:2372 + (i + 1) * 1024], dst, 1024, l * 16 + kc,
                          dst_view=lambda t: t[:, 0:1024].rearrange("p (f c) -> p f c", f=8))
                for c in range(4):
                    piece(l, w_ff1[l, rows, c * 1024:(c + 1) * 1024], wff1_bf[l][kc, :, c * 1024:(c + 1) * 1024], 1024,
                          l * 16 + 8 + kc)
                piece(l, w_out[l, rows, :], wout_bf[l][kc, :, :], 1024, None)
            for kc in range(32):
                piece(l, w_ff2[l, kc * 128:(kc + 1) * 128, :], wff2_bf[l][kc, :, :], 1024, None)
            for i in range(3):
                for c in range(2):
                    piece(l, wbrs[i][l, c * 128:(c + 1) * 128, :], wbr_bf[l][i * 2 + c, :, :], 1024, None)
        S.barrier()
    if stop == 'prep':
        S.barrier(); es.close(); return nc, S, {}

    ramp = sb("ramp", [128, SEQ], BF16); t_ramp = Trk()
    KaT = sb("KaT", [128, 2, SEQ], BF16); t_KaT = [Trk() for _ in range(8)]
    KcT = sb("KcT", [128, 2, SEQ], BF16); t_KcT = [Trk() for _ in range(8)]
    KiT = sb("KiT", [128, SEQ], BF16); t_KiT = [Trk() for _ in range(8)]
    Vae = sb("Vae", [128, 32, 260], BF16); t_Vae = [Trk() for _ in range(8)]
    Vce = sb("Vce", [128, 32, 260], BF16); t_Vce = [Trk() for _ in range(8)]
    xb = sb("xb", [128, 4, D], F32); t_xb = [Trk() for _ in range(4)]
    hT = sb("hT", [128, 8, 512], BF16); t_hT = Trk()
    QaT = sb("QaT", [128, 2, 512], BF16); t_QaT = Trk()
    QiT = sb("QiT", [128, 2, 512], BF16); t_QiT = Trk()
    QcT = sb("QcT", [128, 2, 512], BF16); t_QcT = Trk()
    acc = sb("acc", [128, SEQ], F32); t_acc = Trk()
    aT = acc[:, :].bitcast(BF16).rearrange("p (c t) -> p c t", c=16); t_aT = t_acc
    o1 = acc[:, 0:1024].rearrange("p (q e) -> p q e", q=4); t_o1 = [t_acc] * 4
    mb = sb("mb", [128, 4, SEQ], BF16); t_mb = [Trk() for _ in range(4)]
    mergedT = mb[:, 0, :].rearrange("p (k t) -> p k t", k=8); t_mT = t_mb[0]
    gfin = mb[:, 1, :].bitcast(F32)[:, 0:D]; t_gfin = t_mb[1]
    yT = sb("yT", [128, 3, 2, 512], BF16); t_yT = [Trk() for _ in range(3)]
    NW = 2
    wbuf = [sb("wbuf%d" % i, [128, 8, 512], BF16) for i in range(NW)]; t_wbuf = [Trk() for _ in range(NW)]
    wbr_sb = sb("wbr_sb", [128, 6, 512], BF16); t_wbr = Trk()
    NPT = 3
    PT = [sb("PT%d" % i, [128, 512], BF16) for i in range(NPT)]; t_PT = [Trk() for _ in range(NPT)]
    RR = sb("RR", [128, 5, 512], F32); t_RR = [Trk() for _ in range(5)]
    sig = [RR[:, i, :] for i in range(3)]; t_sig = t_RR[0:3]
    tm = [RR[:, 3 + i, :] for i in range(2)]; t_tm = t_RR[3:5]
    rt = [RR[:, i, :] for i in range(2)]; t_rt = t_RR[0:2]
    gbuf = RR[:, 2, :]; t_gbuf = t_RR[2]
    hn = sb("hn", [128, D], BF16); t_hn = Trk()
    ss = sb("ss", [128, 4], F32); t_ss = [Trk() for _ in range(4)]
    ms = sb("ms", [128, 4], F32); t_ms = [Trk() for _ in range(4)]
    rstd = sb("rstd", [128, 4], F32); t_rstd = [Trk() for _ in range(4)]
    absw = sb("absw", [128, 16], F32); sgn = sb("sgn", [128, 16], F32); wsc = sb("wsc", [128, 16], F32)
    t_iw = [Trk() for _ in range(4)]
    st6 = sb("st6", [128, 6], F32); mv = sb("mv", [128, 2], F32); lnr = sb("lnr", [128, 2], F32); t_ln = Trk()
    ln1 = sb("ln1", [128, 256], F32); ln2 = sb("ln2", [128, 256], F32); t_ln1 = Trk(); t_ln2 = Trk()
    knd = sb("knd", [128, 128], BF16); t_knd = Trk()
    vn = sb("vn", [128, 256], BF16); t_vn = Trk()
    ybt = sb("ybt", [128, 256], BF16); t_ybt = Trk()
    mid = sb("mid", [128, 1], F32); cntt = sb("cntt", [128, 1], F32); ttt = sb("ttt", [128, 1], F32)
    thr = sb("thr", [128, 1], F32); t_bis = Trk()
    rden = sb("rden", [128, 4], F32); t_rden = Trk()
    sq = sb("sq", [128, 256], F32); t_sq = Trk()
    oo = sb("oo", [128, 256], F32); t_oo = Trk()
    ssc = sb("ssc", [128, 4], F32); t_ssc = Trk()
    idxg_bc = sb("idxg_bc", [128, 64], F32); idxb_bc = sb("idxb_bc", [128, 64], F32)
    sgug_bc = sb("sgug_bc", [128, 256], F32); sgub_bc = sb("sgub_bc", [128, 256], F32)
    wsf = sb("wsf", [128, 4, 128], F32); wsb = sb("wsb", [128, 4, 128], BF16); WcT = sb("WcT", [128, 4, 128], BF16)
    bs = sb("bs", [128, 4], F32)
    lamt = sb("lamt", [128, 128], F32); lamp = sb("lamp", [128, 64], F32); lam2 = sb("lam2", [128, 2], F32)
    neglam = sb("neglam", [128, 1], F32)
    gsub = sb("gsub", [128, 64], F32)
    t_par = Trk()

    ps = [es.enter_context(nc.psum_tensor("ps%d" % i, [128, 512], F32)) for i in range(7)]
    psT = es.enter_context(nc.psum_tensor("psT", [128, 1024], BF16))
    t_ps = [Trk(True) for _ in range(7)]; t_psT = Trk(True)
    rot = {"i": 0}

    def nb(lst=(4, 5, 6)):
        rot["i"] += 1
        return lst[rot["i"] % len(lst)]
    ALLB = (0, 1, 2, 3, 4, 5, 6)
    wrot = {"i": 0}

    def wslot():
        wrot["i"] += 1
        return wrot["i"] % NW
    evrot = {"i": 0}

    def evac_copy(out_ap, in_ap, r, w=(), wa=()):
        evrot["i"] += 1
        if evrot["i"] % 2:
            S.op(act, lambda: nc.scalar.copy(out=out_ap, in_=in_ap), r=r, w=w, wa=wa)
        else:
            S.op(dve, lambda: nc.vector.tensor_copy(out=out_ap, in_=in_ap), r=r, w=w, wa=wa)

    S.op(pool, lambda: nc.gpsimd.memset(Vae[:], 1.0), w=t_Vae)
    S.op(pool, lambda: nc.gpsimd.memset(Vce[:], 1.0), w=t_Vce)
    for q in range(4):
        S.dma(acc[:, q * 1024:(q + 1) * 1024], rampv[0:1, q * 1024:(q + 1) * 1024].partition_broadcast(128), r=[t_in],
              wa=[t_acc])
    S.op(dve, lambda: nc.vector.tensor_copy(out=ramp[:], in_=acc[:]), r=[t_acc], w=[t_ramp])

    dbg_outs = {}

    chkc = {}

    def chk(tag):
        chkc[tag] = chkc.get(tag, 0) + 1
        if stop == tag or stop == "%s:%d" % (tag, chkc[tag]):
            raise _Stop()

    def dump(name, ap, trks, shape, dt=F32):
        if not dbg: return
        d = nc.dram_tensor("dbg_" + name, list(shape), dt, kind="ExternalOutput").ap()
        S.dma(d, ap, r=trks)
        dbg_outs[name] = shape

    def rmsnorm_to_hT(b):
        S.op(act, lambda: nc.scalar.activation(out=hn[:], in_=xb[:, b, :], func=AF.Square, accum_out=ss[:, b:b + 1]),
             r=[t_xb[b]], w=[t_hn, t_ss[b]])
        S.op(dve, lambda: nc.vector.tensor_scalar(out=ms[:, b:b + 1], in0=ss[:, b:b + 1], scalar1=1.0 / D, scalar2=EPS,
                                                  op0=ALU.mult, op1=ALU.add), r=[t_ss[b]], w=[t_ms[b]])
        S.op(pool, lambda: nc.gpsimd.tensor_tensor(out=rstd[:, b:b + 1], in0=ms[:, b:b + 1], in1=neghalf[:, 0:1],
                                                   op=ALU.pow), r=[t_ms[b], t_cst], w=[t_rstd[b]])
        S.op(dve, lambda: nc.vector.tensor_scalar(out=hn[:], in0=xb[:, b, :], scalar1=rstd[:, b:b + 1], scalar2=None,
                                                  op0=ALU.mult), r=[t_xb[b], t_rstd[b]], w=[t_hn])
        for kc in range(8):
            S.op(pe, lambda kc=kc: nc.tensor.transpose(psT[:, kc * 128:(kc + 1) * 128], hn[:, kc * 128:(kc + 1) * 128],
                                                      ident_bf[:]), r=[t_hn, t_cst], w=[t_psT])
        evac_copy(hT[:, :, b * 128:(b + 1) * 128], psT[:, :].rearrange("p (k t) -> p k t", k=8), r=[t_psT], wa=[t_hT])

    def load_w(src_ap, nk, cw, prep_t):
        s = wslot()
        S.dma(wbuf[s][:, 0:nk, 0:cw], src_ap, r=[prep_t], w=[t_wbuf[s]])
        return s

    def layernorm_stats(in_ap):
        S.op(dve, lambda: nc.vector.bn_stats(out=st6[:], in_=in_ap[0]), r=in_ap[1], w=[t_ln])
        chk('LN1')
        S.op(dve, lambda: nc.vector.bn_aggr(out=mv[:], in_=st6[:]), r=[t_ln], w=[t_ln])
        chk('LN2')
        S.op(dve, lambda: nc.vector.tensor_scalar(out=lnr[:, 1:2], in0=mv[:, 1:2], scalar1=EPS, scalar2=None, op0=ALU.add),
             r=[t_ln], w=[t_ln])
        chk('LN3')
        S.op(pool, lambda: nc.gpsimd.tensor_tensor(out=lnr[:, 0:1], in0=lnr[:, 1:2], in1=neghalf[:, 0:1], op=ALU.pow),
             r=[t_ln, t_cst], w=[t_ln])

    try:
      for l in range(n_layers):
        lambda_init = 0.8 - 0.6 * math.exp(-0.3 * l)
        last = (l == n_layers - 1)
        xsrc = x_in if l == 0 else xs
        S.dma(idxg_bc[:], idx_g[l:l + 1, :].partition_broadcast(128), r=[t_in], w=[t_par])
        S.dma(idxb_bc[:], idx_b[l:l + 1, :].partition_broadcast(128), r=[t_in], wa=[t_par])
        S.dma(sgug_bc[:], sgu_g[l:l + 1, :].partition_broadcast(128), r=[t_in], wa=[t_par])
        S.dma(sgub_bc[:], sgu_b[l:l + 1, :].partition_broadcast(128), r=[t_in], wa=[t_par])
        S.dma(wsf[:], sgu_w[l].rearrange("g t s -> t g s"), r=[t_in], wa=[t_par])
        S.dma(bs[:], sgu_bs[l].rearrange("g t -> t g"), r=[t_in], wa=[t_par], allow_slow_non_contiguous=True)
        S.dma(lamt[:], dlam[l:l + 1].rearrange("o a b -> o (a b)").partition_broadcast(128), r=[t_in], wa=[t_par])
        S.dma(gsub[:], subln[l:l + 1, :].partition_broadcast(128), r=[t_in], wa=[t_par])
        for g in range(4):
            S.op(dve, lambda g=g: nc.vector.tensor_tensor(out=wsb[:, g, :], in0=wsf[:, g, :], in1=tril, op=ALU.mult),
                 r=[t_par, t_cst], w=[t_par])
        for g in range(4):
            S.op(pe, lambda g=g: nc.tensor.transpose(psT[:, g * 128:(g + 1) * 128], wsb[:, g, :], ident_bf[:]),
                 r=[t_par, t_cst], w=[t_psT])
        S.op(dve, lambda: nc.vector.tensor_copy(out=WcT[:], in_=psT[:, 0:512].rearrange("p (g t) -> p g t", g=4)),
             r=[t_psT], w=[t_par])
        lt4 = lamt[:].rearrange("p (a b) -> p a b", a=4)
        S.op(dve, lambda: nc.vector.tensor_tensor(out=lamp[:].rearrange("p (a b) -> p a b", a=2), in0=lt4[:, 0:4:2, :],
                                                  in1=lt4[:, 1:4:2, :], op=ALU.mult), r=[t_par], w=[t_par])
        S.op(dve, lambda: nc.vector.tensor_reduce(out=lam2[:], in_=lamp[:].rearrange("p (a b) -> p a b", a=2), axis=AX.X,
                                                  op=ALU.add), r=[t_par], w=[t_par])
        S.op(act, lambda: nc.scalar.activation(out=lam2[:], in_=lam2[:], func=AF.Exp), r=[t_par], w=[t_par])
        S.op(dve, lambda: nc.vector.tensor_tensor(out=neglam[:], in0=lam2[:, 1:2], in1=lam2[:, 0:1], op=ALU.subtract),
             r=[t_par], w=[t_par])
        S.op(dve, lambda: nc.vector.tensor_scalar(out=neglam[:], in0=neglam[:], scalar1=-lambda_init, scalar2=None,
                                                  op0=ALU.add), r=[t_par], w=[t_par])
        S.op(dve, lambda: nc.vector.tensor_scalar(out=gsub[:], in0=gsub[:], scalar1=(1.0 - lambda_init), scalar2=None,
                                                  op0=ALU.mult), r=[t_par], w=[t_par])

        chk('par')
        for j in range(n_tiles):
            nkb = 4 * j + 4
            for b in range(4):
                gb = 4 * j + b
                S.dma(xb[:, b, :], xsrc[gb * 128:(gb + 1) * 128, :], r=[t_in if l == 0 else t_xs[j]], w=[t_xb[b]])
            for b in range(4):
                rmsnorm_to_hT(b)

            chk('A')
            s1 = load_w(win_bf[l][:, :, TM_AV:TM_AV + 324].rearrange("k p c -> p k c"), 8, 324, t_prep[l])
            s2 = load_w(win_bf[l][:, :, TM_CV:TM_CV + 256].rearrange("k p c -> p k c"), 8, 256, t_prep[l])
            for b in range(4):
                gb = 4 * j + b
                bk = nb(ALLB)
                for kc in range(8):
                    S.op(pe, lambda kc=kc: nc.tensor.matmul(ps[bk][:, 0:324], lhsT=hT[:, kc, b * 128:(b + 1) * 128],
                                                            rhs=wbuf[s1][:, kc, 0:324], start=(kc == 0), stop=(kc == 7)),
                         r=[t_hT, t_wbuf[s1]], w=[t_ps[bk]])
                chk('B1a')
                evac_copy(Vae[:, gb, :].rearrange("p (h e) -> p h e", h=4)[:, :, 0:64],
                          ps[bk][:, 0:256].rearrange("p (h e) -> p h e", h=4), r=[t_ps[bk]], wa=[t_Vae[j]])
                chk('B1b')
                S.op(act, lambda: nc.scalar.copy(out=ln2[:, 64:128], in_=ps[bk][:, 256:320]), r=[t_ps[bk]], w=[t_ln2])
                chk('LN0')
                layernorm_stats((ln2[:, 64:128], [t_ln2]))
                chk('B1b1')
                S.op(dve, lambda: nc.vector.tensor_scalar(out=ln1[:, 0:64], in0=ln2[:, 64:128], scalar1=mv[:, 0:1],
                                                          scalar2=lnr[:, 0:1], op0=ALU.subtract, op1=ALU.mult),
                     r=[t_ln2, t_ln], w=[t_ln1])
                chk('B1b2')
                S.op(dve, lambda: nc.vector.tensor_tensor(out=ln2[:, 0:64], in0=ln1[:, 0:64], in1=idxg_bc[:], op=ALU.mult),
                     r=[t_ln1, t_par], w=[t_ln2])
                chk('B1b3')
                S.op(dve, lambda: nc.vector.tensor_tensor(out=knd[:, 0:64], in0=ln2[:, 0:64], in1=idxb_bc[:], op=ALU.add),
                     r=[t_ln2, t_par], w=[t_knd])
                S.op(dve, lambda: nc.vector.tensor_tensor(out=knd[:, 64:128], in0=ln2[:, 0:64], in1=idxb_bc[:], op=ALU.add),
                     r=[t_ln2, t_par], wa=[t_knd])
                chk('B1c0')
                S.op(pe, lambda: nc.tensor.transpose(psT[:, 0:128], knd[:], ident_bf[:]), r=[t_knd, t_cst], w=[t_psT])
                evac_copy(KiT[:, gb * 128:(gb + 1) * 128], psT[:, 0:128], r=[t_psT], wa=[t_KiT[j]])
                chk('B1c')
                S.op(dve, lambda: nc.vector.tensor_scalar(out=wsc[:, b * 4:b * 4 + 4], in0=ps[bk][:, 320:324],
                                                          scalar1=1.0 / 16.0, scalar2=None, op0=ALU.mult),
                     r=[t_ps[bk]], w=[t_iw[b]])
                S.op(dve, lambda: nc.vector.scalar_tensor_tensor(out=absw[:, b * 4:b * 4 + 4], in0=wsc[:, b * 4:b * 4 + 4],
                                                                 scalar=-1.0, in1=wsc[:, b * 4:b * 4 + 4], op0=ALU.mult,
                                                                 op1=ALU.max), r=[t_iw[b]], w=[t_iw[b]])
                S.op(dve, lambda: nc.vector.tensor_scalar(out=sgn[:, b * 4:b * 4 + 4], in0=wsc[:, b * 4:b * 4 + 4],
                                                          scalar1=0.0, scalar2=2.0, op0=ALU.is_ge, op1=ALU.mult),
                     r=[t_iw[b]], w=[t_iw[b]])
                S.op(dve, lambda: nc.vector.tensor_scalar(out=sgn[:, b * 4:b * 4 + 4], in0=sgn[:, b * 4:b * 4 + 4],
                                                          scalar1=-1.0, scalar2=None, op0=ALU.add),
                     r=[t_iw[b]], w=[t_iw[b]])
                chk('B1d')
                bk2 = nb(ALLB)
                for kc in range(8):
                    S.op(pe, lambda kc=kc: nc.tensor.matmul(ps[bk2][:, 0:256], lhsT=hT[:, kc, b * 128:(b + 1) * 128],
                                                            rhs=wbuf[s2][:, kc, 0:256], start=(kc == 0), stop=(kc == 7)),
                         r=[t_hT, t_wbuf[s2]], w=[t_ps[bk2]])
                evac_copy(Vce[:, gb, :].rearrange("p (h e) -> p h e", h=4)[:, :, 0:64],
                          ps[bk2][:, 0:256].rearrange("p (h e) -> p h e", h=4), r=[t_ps[bk2]], wa=[t_Vce[j]])
            chk('B1')
            chk('B1e')
            s3 = load_w(win_bf[l][:, :, TM_BUV:TM_BUV + 512].rearrange("k p c -> p k c"), 8, 512, t_prep[l])
            for b in range(4):
                bk = nb(ALLB)
                for kc in range(8):
                    S.op(pe, lambda kc=kc: nc.tensor.matmul(ps[bk][:, 0:512], lhsT=hT[:, kc, b * 128:(b + 1) * 128],
                                                            rhs=wbuf[s3][:, kc, 0:512], start=(kc == 0), stop=(kc == 7)),
                         r=[t_hT, t_wbuf[s3]], w=[t_ps[bk]])
                S.op(act, lambda: nc.scalar.activation(out=gbuf, in_=ps[bk][:, 0:512], func=AF.Gelu_apprx_tanh),
                     r=[t_ps[bk]], w=[t_gbuf])
                layernorm_stats((gbuf[:, 256:512], [t_gbuf]))
                S.op(dve, lambda: nc.vector.tensor_scalar(out=ln1[:], in0=gbuf[:, 256:512], scalar1=mv[:, 0:1],
                                                          scalar2=lnr[:, 0:1], op0=ALU.subtract, op1=ALU.mult),
                     r=[t_gbuf, t_ln], w=[t_ln1])
                S.op(pool, lambda: nc.gpsimd.tensor_tensor(out=ln2[:], in0=ln1[:], in1=sgug_bc[:], op=ALU.mult),
                     r=[t_ln1, t_par], w=[t_ln2])
                S.op(pool, lambda: nc.gpsimd.tensor_tensor(out=vn[:], in0=ln2[:], in1=sgub_bc[:], op=ALU.add),
                     r=[t_ln2, t_par], w=[t_vn])
                bk2 = nb(ALLB)
                for g in range(4):
                    S.op(pe, lambda g=g: nc.tensor.matmul(ps[bk2][:, g * 64:(g + 1) * 64], lhsT=WcT[:, g, :],
                                                          rhs=vn[:, g * 64:(g + 1) * 64], start=(g == 0), stop=(g == 3),
                                                          skip_group_check=True), r=[t_vn, t_par], w=[t_ps[bk2]])
                for g in range(4):
                    S.op(dve, lambda g=g: nc.vector.scalar_tensor_tensor(
                        out=ybt[:, g * 64:(g + 1) * 64], in0=ps[bk2][:, g * 64:(g + 1) * 64], scalar=bs[:, g:g + 1],
                        in1=gbuf[:, g * 64:(g + 1) * 64], op0=ALU.add, op1=ALU.mult),
                        r=[t_ps[bk2], t_par, t_gbuf], w=[t_ybt] if g == 0 else [], wa=[] if g == 0 else [t_ybt])
                for c in range(2):
                    S.op(pe, lambda c=c: nc.tensor.transpose(psT[:, c * 128:(c + 1) * 128], ybt[:, c * 128:(c + 1) * 128],
                                                            ident_bf[:]), r=[t_ybt, t_cst], w=[t_psT])
                evac_copy(yT[:, 1, :, b * 128:(b + 1) * 128], psT[:, 0:256].rearrange("p (c t) -> p c t", c=2),
                          r=[t_psT], wa=[t_yT[1]])
            chk('B2')
            fm = [(FM_AQ, 512, [("qa", 0), ("qa", 1), ("ka", 0), ("ka", 1)]),
                  (FM_IQ, 512, [("qi", 0), ("qi", 1), ("qc", 0), ("qc", 1)]),
                  (FM_CK, 256, [("kc", 0), ("kc", 1)])]
            for (c0, cw, dests) in fm:
                s = load_w(win_bf[l][:, :, c0:c0 + cw].rearrange("k p c -> p k c"), 8, cw, t_prep[l])
                for ci, (kind, c) in enumerate(dests):
                    bk = nb(ALLB)
                    for kc in range(8):
                        S.op(pe, lambda kc=kc: nc.tensor.matmul(ps[bk][:, 0:512], lhsT=wbuf[s][:, kc, ci * 128:(ci + 1) * 128],
                                                                rhs=hT[:, kc, :], start=(kc == 0), stop=(kc == 7)),
                             r=[t_hT, t_wbuf[s]], w=[t_ps[bk]])
                    if kind == "qa": evac_copy(QaT[:, c, :], ps[bk][:, :], r=[t_ps[bk]], wa=[t_QaT])
                    elif kind == "qi": evac_copy(QiT[:, c, :], ps[bk][:, :], r=[t_ps[bk]], wa=[t_QiT])
                    elif kind == "qc": evac_copy(QcT[:, c, :], ps[bk][:, :], r=[t_ps[bk]], wa=[t_QcT])
                    elif kind == "ka": evac_copy(KaT[:, c, j * 512:(j + 1) * 512], ps[bk][:, :], r=[t_ps[bk]], wa=[t_KaT[j]])
                    elif kind == "kc": evac_copy(KcT[:, c, j * 512:(j + 1) * 512], ps[bk][:, :], r=[t_ps[bk]], wa=[t_KcT[j]])

            chk('B3')
            for qb in range(4):
                gb = 4 * j + qb
                ncols = (gb + 1) * 128
                for kg in range((ncols + 511) // 512):
                    c0 = kg * 512; cw = min(512, ncols - c0)
                    for h in range(4):
                        bk = nb(ALLB)
                        rs = slice((h % 2) * 64, (h % 2) * 64 + 64)
                        S.op(pe, lambda: nc.tensor.matmul(ps[bk][:, 0:cw], lhsT=QiT[rs, h // 2, qb * 128:(qb + 1) * 128],
                                                          rhs=KiT[rs, c0:c0 + cw], start=True, stop=True),
                             r=[t_QiT] + t_KiT[0:j + 1], w=[t_ps[bk]])
                        S.op(act, lambda: nc.scalar.activation(out=ps[bk][:, 0:cw], in_=ps[bk][:, 0:cw], func=AF.Relu,
                                                               scale=absw[:, qb * 4 + h:qb * 4 + h + 1]),
                             r=[t_iw[qb]], w=[t_ps[bk]])
                        in1 = ramp[:, c0:c0 + cw] if h == 0 else acc[:, c0:c0 + cw]
                        S.op(dve, lambda: nc.vector.scalar_tensor_tensor(
                            out=acc[:, c0:c0 + cw], in0=ps[bk][:, 0:cw], scalar=sgn[:, qb * 4 + h:qb * 4 + h + 1], in1=in1,
                            op0=ALU.mult, op1=ALU.add), r=[t_ps[bk], t_iw[qb], t_ramp], w=[t_acc])
                S.op(dve, lambda: nc.vector.tensor_tensor(out=acc[:, gb * 128:(gb + 1) * 128],
                                                          in0=acc[:, gb * 128:(gb + 1) * 128], in1=causal_big, op=ALU.add),
                     r=[t_cst], w=[t_acc])
                if gb >= 2:
                    S.op(dve, lambda: nc.vector.memset(mid[:], 0.0), w=[t_bis])
                    for k in range(NBIS):
                        ck = RNG / (2.0 ** k)
                        S.op(dve, lambda: nc.vector.tensor_scalar(out=mb[:, qb, 0:ncols], in0=acc[:, 0:ncols],
                                                                  scalar1=mid[:, 0:1], scalar2=None, op0=ALU.is_ge,
                                                                  op1=ALU.add, accum_out=cntt[:]),
                             r=[t_acc], w=[t_mb[qb], t_bis])
                        S.op(dve, lambda: nc.vector.tensor_scalar(out=ttt[:], in0=cntt[:], scalar1=255.5, scalar2=ck,
                                                                  op0=ALU.is_ge, op1=ALU.mult), w=[t_bis])
                        S.op(dve, lambda: nc.vector.scalar_tensor_tensor(out=mid[:], in0=ttt[:], scalar=-ck / 2.0,
                                                                         in1=mid[:], op0=ALU.add, op1=ALU.add), w=[t_bis])
                    cK = RNG / (2.0 ** NBIS)
                    S.op(dve, lambda: nc.vector.tensor_scalar(out=thr[:], in0=mid[:], scalar1=-cK, scalar2=None,
                                                              op0=ALU.add), w=[t_bis])
                else:
                    S.op(dve, lambda: nc.vector.memset(thr[:], -RNG), w=[t_bis])
                S.op(pool, lambda: nc.gpsimd.tensor_scalar(out=mb[:, qb, 0:ncols], in0=acc[:, 0:ncols], scalar1=thr[:, 0:1],
                                                           scalar2=MASKV, op0=ALU.is_lt, op1=ALU.mult),
                     r=[t_acc, t_bis], w=[t_mb[qb]])

            chk('C')
            def attention(kind, comp):
                if kind == "a":
                    KT, QT, VE, tK, tQ, tV = KaT, QaT, Vae, t_KaT, t_QaT, t_Vae
                    scale = 64 ** -0.5
                else:
                    KT, QT, VE, tK, tQ, tV = KcT, QcT, Vce, t_KcT, t_QcT, t_Vce
                    scale = 32 ** -0.5
                steps = [(h, kb) for h in range(4) for kb in range(nkb)]
                banks = {}
                LA = 2

                def qk(i):
                    h, kb = steps[i]
                    r0 = max(kb - 4 * j, 0); c0 = r0 * 128
                    bk = nb((4, 5, 6)); banks[i] = bk
                    if kind == "a":
                        rs = slice((h % 2) * 64, (h % 2) * 64 + 64); ch = h // 2; tp = None
                    else:
                        idx = h * 2 + comp
                        rs = slice((idx % 4) * 32, (idx % 4) * 32 + 32); ch = idx // 4; tp = ((idx % 4) * 32, 0)
                    kw = {} if tp is None else {"tile_position": tp}
                    S.op(pe, lambda: nc.tensor.matmul(ps[bk][:, c0:512], lhsT=KT[rs, ch, kb * 128:(kb + 1) * 128],
                                                      rhs=QT[rs, ch, c0:512], start=True, stop=False, **kw),
                         r=[tQ, tK[kb // 4]], w=[t_ps[bk]])
                    if kind == "a":
                        for qb in range(r0, 4):
                            S.op(pe, lambda qb=qb: nc.tensor.matmul(ps[bk][:, qb * 128:(qb + 1) * 128],
                                                                    lhsT=mb[:, qb, kb * 128:(kb + 1) * 128], rhs=ident_bf[:],
                                                                    start=False, stop=(qb == 3), skip_group_check=True),
                                 r=[t_mb[qb], t_cst], w=[t_ps[bk]])
                    else:
                        if kb >= 4 * j:
                            S.op(pe, lambda: nc.tensor.matmul(ps[bk][:, r0 * 128:(r0 + 1) * 128], lhsT=cmb_bf[:],
                                                              rhs=ident_bf[:], start=False, stop=True,
                                                              skip_group_check=True), r=[t_cst], w=[t_ps[bk]])
                    pi = i % NPT
                    S.op(act, lambda: nc.scalar.activation(out=PT[pi][:, c0:512], in_=ps[bk][:, c0:512], func=AF.Exp,
                                                           scale=scale), r=[t_ps[bk]], w=[t_PT[pi]])

                def pv(i):
                    h, kb = steps[i]
                    r0 = max(kb - 4 * j, 0)
                    pi = i % NPT
                    for qb in range(r0, 4):
                        S.op(pe, lambda qb=qb: nc.tensor.matmul(ps[qb][:, h * 65:(h + 1) * 65],
                                                                lhsT=PT[pi][:, qb * 128:(qb + 1) * 128],
                                                                rhs=VE[:, kb, h * 65:(h + 1) * 65],
                                                                start=(h == 0 and kb == 0), stop=(kb == 4 * j + qb),
                                                                skip_group_check=True),
                             r=[t_PT[pi], tV[kb // 4]], w=[t_ps[qb]])
                n = len(steps)
                for i in range(min(LA, n)): qk(i)
                for i in range(n):
                    if i + LA < n: qk(i + LA)
                    pv(i)

            def out_views(qb):
                o4 = ps[qb][:, 0:260].rearrange("p (h e) -> p h e", h=4)
                return o4[:, :, 0:64], o4[:, :, 64:65]

            def transpose_to_yT(src, t_src, br, qb):
                for c in range(2):
                    S.op(pe, lambda c=c: nc.tensor.transpose(psT[:, c * 128:(c + 1) * 128], src[:, c * 128:(c + 1) * 128],
                                                            ident_bf[:]), r=[t_src, t_cst], w=[t_psT])
                evac_copy(yT[:, br, :, qb * 128:(qb + 1) * 128], psT[:, 0:256].rearrange("p (c t) -> p c t", c=2),
                          r=[t_psT], wa=[t_yT[br]])

            attention("a", 0)
            for qb in range(4):
                ov, dv = out_views(qb)
                S.op(dve, lambda: nc.vector.reciprocal(out=rden[:].rearrange("p (h o) -> p h o", o=1), in_=dv),
                     r=[t_ps[qb]], w=[t_rden])
                S.op(dve, lambda: nc.vector.tensor_tensor(out=ybt[:].rearrange("p (h e) -> p h e", h=4), in0=ov,
                                                          in1=rden[:].rearrange("p (h o) -> p h o", o=1).to_broadcast([128, 4, 64]),
                                                          op=ALU.mult), r=[t_ps[qb], t_rden], w=[t_ybt])
                transpose_to_yT(ybt, t_ybt, 0, qb)
            chk('D')
            attention("c", 0)
            for qb in range(4):
                ov, dv = out_views(qb)
                S.op(dve, lambda: nc.vector.reciprocal(out=rden[:].rearrange("p (h o) -> p h o", o=1), in_=dv),
                     r=[t_ps[qb]], w=[t_rden])
                S.op(dve, lambda: nc.vector.tensor_tensor(out=o1[:, qb, :].rearrange("p (h e) -> p h e", h=4), in0=ov,
                                                          in1=rden[:].rearrange("p (h o) -> p h o", o=1).to_broadcast([128, 4, 64]),
                                                          op=ALU.mult), r=[t_ps[qb], t_rden], w=[t_o1[qb]])
            attention("c", 1)
            for qb in range(4):
                ov, dv = out_views(qb)
                S.op(dve, lambda: nc.vector.reciprocal(out=rden[:].rearrange("p (h o) -> p h o", o=1), in_=dv),
                     r=[t_ps[qb]], w=[t_rden])
                S.op(dve, lambda: nc.vector.tensor_scalar(out=rden[:], in0=rden[:], scalar1=neglam[:, 0:1], scalar2=None,
                                                          op0=ALU.mult), r=[t_par], w=[t_rden])
                S.op(dve, lambda: nc.vector.tensor_tensor(out=oo[:].rearrange("p (h e) -> p h e", h=4), in0=ov,
                                                          in1=rden[:].rearrange("p (h o) -> p h o", o=1).to_broadcast([128, 4, 64]),
                                                          op=ALU.mult), r=[t_ps[qb], t_rden], w=[t_oo])
                S.op(pool, lambda: nc.gpsimd.tensor_tensor(out=oo[:], in0=oo[:], in1=o1[:, qb, :], op=ALU.add),
                     r=[t_o1[qb]], w=[t_oo])
                S.op(pool, lambda: nc.gpsimd.tensor_tensor(out=sq[:], in0=oo[:], in1=oo[:], op=ALU.mult), r=[t_oo], w=[t_sq])
                S.op(dve, lambda: nc.vector.tensor_reduce(out=ssc[:], in_=sq[:].rearrange("p (h e) -> p h e", h=4), axis=AX.X,
                                                          op=ALU.add), r=[t_sq], w=[t_ssc])
                S.op(dve, lambda: nc.vector.tensor_scalar(out=ssc[:], in0=ssc[:], scalar1=1.0 / 64.0, scalar2=EPS,
                                                          op0=ALU.mult, op1=ALU.add), w=[t_ssc])
                S.op(pool, lambda: nc.gpsimd.tensor_tensor(out=ssc[:], in0=ssc[:], in1=neghalf[:, 0:1].to_broadcast([128, 4]),
                                                           op=ALU.pow), r=[t_cst], w=[t_ssc])
                S.op(dve, lambda: nc.vector.tensor_tensor(out=sq[:].rearrange("p (h e) -> p h e", h=4),
                                                          in0=oo[:].rearrange("p (h e) -> p h e", h=4),
                                                          in1=ssc[:].rearrange("p (h o) -> p h o", o=1).to_broadcast([128, 4, 64]),
                                                          op=ALU.mult), r=[t_oo, t_ssc], w=[t_sq])
                S.op(pool, lambda: nc.gpsimd.tensor_tensor(out=ybt[:].rearrange("p (h e) -> p h e", h=4),
                                                           in0=sq[:].rearrange("p (h e) -> p h e", h=4),
                                                           in1=gsub[:].rearrange("p (o e) -> p o e", o=1).to_broadcast([128, 4, 64]),
                                                           op=ALU.mult), r=[t_sq, t_par], w=[t_ybt])
                transpose_to_yT(ybt, t_ybt, 2, qb)

            chk('E')
            for fc in range(8):
                if fc % 4 == 0:
                    S.dma(wbr_sb[:], wbr_bf[l][:, :, (fc // 4) * 512:(fc // 4 + 1) * 512].rearrange("k p c -> p k c"),
                          r=[t_prep[l]], w=[t_wbr])
                sg = load_w(win_bf[l][:, :, FM_G + fc * 384:FM_G + (fc + 1) * 384].rearrange("k p c -> p k c"), 8, 384,
                            t_prep[l])
                for i in range(3):
                    bkg = nb(ALLB)
                    for kc in range(8):
                        S.op(pe, lambda kc=kc: nc.tensor.matmul(ps[bkg][:, 0:512], lhsT=wbuf[sg][:, kc, i * 128:(i + 1) * 128],
                                                                rhs=hT[:, kc, :], start=(kc == 0), stop=(kc == 7)),
                             r=[t_hT, t_wbuf[sg]], w=[t_ps[bkg]])
                    S.op(act, lambda: nc.scalar.activation(out=sig[i], in_=ps[bkg][:, :], func=AF.Sigmoid),
                         r=[t_ps[bkg]], w=[t_sig[i]])
                    bkz = nb(ALLB)
                    for c in range(2):
                        S.op(pe, lambda c=c: nc.tensor.matmul(ps[bkz][:, 0:512],
                                                              lhsT=wbr_sb[:, i * 2 + c, (fc % 4) * 128:(fc % 4 + 1) * 128],
                                                              rhs=yT[:, i, c, :], start=(c == 0), stop=(c == 1)),
                             r=[t_yT[i], t_wbr], w=[t_ps[bkz]])
                    di = 0 if i == 0 else 1
                    S.op(dve, lambda: nc.vector.tensor_tensor(out=tm[di], in0=ps[bkz][:, :], in1=sig[i], op=ALU.mult),
                         r=[t_ps[bkz], t_sig[i]], w=[t_tm[di]])
                    if i == 1:
                        S.op(pool, lambda: nc.gpsimd.tensor_tensor(out=tm[0], in0=tm[0], in1=tm[1], op=ALU.add),
                             r=[t_tm[1]], w=[t_tm[0]])
                    if i == 2:
                        S.op(pool, lambda: nc.gpsimd.tensor_tensor(out=mergedT[:, fc, :], in0=tm[0], in1=tm[1],
                                                                   op=ALU.add), r=[t_tm[0], t_tm[1]], wa=[t_mT])
            for half in range(2):
                s = load_w(wout_bf[l][:, :, half * 512:(half + 1) * 512].rearrange("k p c -> p k c"), 8, 512, t_prep[l])
                for b in range(4):
                    bk = nb(ALLB)
                    for kc in range(8):
                        S.op(pe, lambda kc=kc: nc.tensor.matmul(ps[bk][:, 0:512], lhsT=mergedT[:, kc, b * 128:(b + 1) * 128],
                                                                rhs=wbuf[s][:, kc, :], start=(kc == 0), stop=(kc == 7)),
                             r=[t_mT, t_wbuf[s]], w=[t_ps[bk]])
                    S.op(dve, lambda: nc.vector.tensor_tensor(out=xb[:, b, half * 512:(half + 1) * 512], in0=ps[bk][:, :],
                                                              in1=xb[:, b, half * 512:(half + 1) * 512], op=ALU.add),
                         r=[t_ps[bk]], w=[t_xb[b]])

            chk('F')
            for b in range(4):
                rmsnorm_to_hT(b)
            ri = 0
            for ffh in range(2):
                for grp in range(4):
                    cb = (ffh * 4 + grp) * 512
                    s = load_w(wff1_bf[l][:, :, cb:cb + 512].rearrange("k p c -> p k c"), 8, 512, t_prep[l])
                    for cc in range(4):
                        bk = nb((4, 5, 6))
                        for kc in range(8):
                            S.op(pe, lambda kc=kc: nc.tensor.matmul(ps[bk][:, 0:512], lhsT=wbuf[s][:, kc, cc * 128:(cc + 1) * 128],
                                                                    rhs=hT[:, kc, :], start=(kc == 0), stop=(kc == 7)),
                                 r=[t_hT, t_wbuf[s]], w=[t_ps[bk]])
                        ri ^= 1
                        S.op(act, lambda: nc.scalar.activation(out=rt[ri], in_=ps[bk][:, :], func=AF.Relu),
                             r=[t_ps[bk]], w=[t_rt[ri]])
                        S.op(pool, lambda: nc.gpsimd.tensor_tensor(out=aT[:, grp * 4 + cc, :], in0=rt[ri], in1=rt[ri],
                                                                   op=ALU.mult), r=[t_rt[ri]], wa=[t_aT])
                for half in range(2):
                    for wg in range(2):
                        k0 = ffh * 16 + wg * 8
                        s = load_w(wff2_bf[l][k0:k0 + 8, :, half * 512:(half + 1) * 512].rearrange("k p c -> p k c"), 8, 512,
                                   t_prep[l])
                        for b in range(4):
                            for c in range(8):
                                S.op(pe, lambda c=c: nc.tensor.matmul(ps[b][:, 0:512],
                                                                      lhsT=aT[:, wg * 8 + c, b * 128:(b + 1) * 128],
                                                                      rhs=wbuf[s][:, c, :], start=(wg == 0 and c == 0),
                                                                      stop=(wg == 1 and c == 7)),
                                     r=[t_aT, t_wbuf[s]], w=[t_ps[b]])
                    for b in range(4):
                        S.op(dve, lambda: nc.vector.tensor_tensor(out=xb[:, b, half * 512:(half + 1) * 512], in0=ps[b][:, :],
                                                                  in1=xb[:, b, half * 512:(half + 1) * 512], op=ALU.add),
                             r=[t_ps[b]], w=[t_xb[b]])
            chk('G')
            if last:
                S.dma(gfin, fin_g[0:1, :].partition_broadcast(128), r=[t_in], w=[t_gfin])
            for b in range(4):
                gb = 4 * j + b
                if last:
                    S.op(act, lambda: nc.scalar.activation(out=hn[:], in_=xb[:, b, :], func=AF.Square,
                                                           accum_out=ss[:, b:b + 1]), r=[t_xb[b]], w=[t_hn, t_ss[b]])
                    S.op(dve, lambda: nc.vector.tensor_scalar(out=ms[:, b:b + 1], in0=ss[:, b:b + 1], scalar1=1.0 / D,
                                                              scalar2=EPS, op0=ALU.mult, op1=ALU.add), r=[t_ss[b]], w=[t_ms[b]])
                    S.op(pool, lambda: nc.gpsimd.tensor_tensor(out=rstd[:, b:b + 1], in0=ms[:, b:b + 1], in1=neghalf[:, 0:1],
                                                               op=ALU.pow), r=[t_ms[b], t_cst], w=[t_rstd[b]])
                    S.op(dve, lambda: nc.vector.scalar_tensor_tensor(out=xb[:, b, :], in0=xb[:, b, :], scalar=rstd[:, b:b + 1],
                                                                     in1=gfin, op0=ALU.mult, op1=ALU.mult),
                         r=[t_rstd[b], t_gfin], w=[t_xb[b]])
                    S.dma(out[gb * 128:(gb + 1) * 128, :], xb[:, b, :], r=[t_xb[b]])
                else:
                    S.dma(xs[gb * 128:(gb + 1) * 128, :], xb[:, b, :], r=[t_xb[b]], wa=[t_xs[j]])

    except _Stop:
        pass
    S.barrier()
    es.close()
    return nc, S, dbg_outs


def make_consts():
    t = np.arange(128)[:, None]; s = np.arange(128)[None, :]
    ident = np.eye(128, dtype=np.float32)
    causal_big = np.where(s <= t, 0.0, -1e30).astype(np.float32)
    cmb = np.where(s <= t, 0.0, MASKV).astype(np.float32)
    tril = (s <= t).astype(np.float32)
    consts = np.concatenate([ident, causal_big, cmb, tril], axis=1).astype(np.float32)
    bits = (np.arange(SEQ) + (27 << 7)).astype(np.uint16)
    rampv = -(bits.view(ml_dtypes.bfloat16).astype(np.float32))[None, :]
    return np.ascontiguousarray(consts), np.ascontiguousarray(rampv.astype(np.float32))


_CACHE = {}


def kernel(**inputs):
    if "nc" not in _CACHE:
        _CACHE["nc"] = build_nc()[0]
    nc = _CACHE["nc"]
    consts, rampv = make_consts()
    shared = {}
    for k, v in inputs.items():
        if k == "x": continue
        a = np.ascontiguousarray(np.asarray(v, dtype=np.float32))
        if k == "final_norm_g": a = a.reshape(1, D)
        shared[k] = a
    shared["consts"] = consts; shared["rampv"] = rampv
    x = np.asarray(inputs["x"], dtype=np.float32)
    in_maps = []
    for c in range(8):
        m = dict(shared); m["x"] = np.ascontiguousarray(x[c]); in_maps.append(m)
    res = run_bass_kernel_spmd(nc, in_maps, core_ids=list(range(8)))
    return np.stack([np.asarray(res.results[c]["out"], dtype=np.float32) for c in range(8)], axis=0)
```

```python
import math, contextlib
import numpy as np
import ml_dtypes
import concourse.bass as bass
import concourse.mybir as mybir
from concourse.bass_utils import run_bass_kernel_spmd

F32 = mybir.dt.float32; BF16 = mybir.dt.bfloat16; U8 = mybir.dt.uint8
AF = mybir.ActivationFunctionType; ALU = mybir.AluOpType; AX = mybir.AxisListType

D = 1024; SEQ = 4096; NL = 2; NIN = 5444; DFF = 4096
EPS = 1e-6
NBIS = 24
RNG = 32.0
MASKV = -30000.0
FM_AQ, FM_AK, FM_IQ, FM_CQ, FM_CK, FM_G = 0, 256, 512, 768, 1024, 1280
TM_AV, TM_IK, TM_IW, TM_CV, TM_BUV = 4352, 4608, 4672, 4676, 4932


class Trk:
    __slots__ = ("w", "r", "excl")

    def __init__(self, excl=False):
        self.w = {}; self.r = {}; self.excl = excl


class Eng:
    def __init__(self, key, h, sem, inorder=False):
        self.key = key; self.h = h; self.sem = sem; self.count = 0; self.seen = {}; self.inorder = inorder


class Sync:
    def __init__(self, nc, es, ndma=24):
        self.nc = nc
        self.sems = {}

        def mk(key, h, inorder=False):
            s = es.enter_context(nc.semaphore("s_" + key))
            self.sems[key] = s
            return Eng(key, h, s, inorder)
        self.pe = mk("pe", nc.tensor, True)
        self.act = mk("act", nc.scalar)
        self.dve = mk("dve", nc.vector)
        self.pool = mk("pool", nc.gpsimd)
        self.sp = Eng("sp", nc.sync, None)
        self.dsem = []
        for k in range(ndma):
            s = es.enter_context(nc.semaphore("s_d%d" % k))
            self.sems[("d", k)] = s
            self.dsem.append([s, 0])
        self.dnext = 0
        self.ninstr = 0

    def _waits(self, eng, r, w, wa):
        waits = {}
        for t in r:
            for k, v in t.w.items():
                if v > waits.get(k, 0): waits[k] = v
        for t in w:
            for k, v in t.w.items():
                if v > waits.get(k, 0): waits[k] = v
            for k, v in t.r.items():
                if v > waits.get(k, 0): waits[k] = v
        for t in wa:
            for k, v in t.r.items():
                if v > waits.get(k, 0): waits[k] = v
        for k, v in waits.items():
            if k == eng.key and eng.inorder: continue
            if eng.seen.get(k, 0) >= v: continue
            eng.h.wait_ge(self.sems[k], v)
            eng.seen[k] = v

    def op(self, eng, fn, r=(), w=(), wa=()):
        if any(t.excl for t in r):
            w = list(w) + [t for t in r if t.excl]
            r = [t for t in r if not t.excl]
        self._waits(eng, r, w, wa)
        ins = fn()
        eng.count += 1
        ins.then_inc(eng.sem, 1)
        self.ninstr += 1
        for t in r: t.r[eng.key] = eng.count
        for t in w: t.w[eng.key] = eng.count
        for t in wa: t.w[eng.key] = eng.count
        return ins

    def dma(self, out, in_, r=(), w=(), wa=(), **kw):
        q = self.sp
        self._waits(q, r, w, wa)
        k = self.dnext; self.dnext = (self.dnext + 1) % len(self.dsem)
        s, c = self.dsem[k]
        key = ("d", k)
        if c > 0 and q.seen.get(key, 0) < c:
            q.h.wait_ge(s, c); q.seen[key] = c
        q.h.dma_start(out=out, in_=in_, **kw).then_inc(s, 16)
        c += 16
        self.dsem[k][1] = c
        self.ninstr += 1
        for t in r: t.r[key] = c
        for t in w: t.w[key] = c
        for t in wa: t.w[key] = c

    def barrier(self):
        engs = [self.pe, self.act, self.dve, self.pool, self.sp]
        for e in engs:
            for o in [self.pe, self.act, self.dve, self.pool]:
                if o is e or o.count == 0: continue
                if e.seen.get(o.key, 0) < o.count:
                    e.h.wait_ge(o.sem, o.count); e.seen[o.key] = o.count
            for k, (s, c) in enumerate(self.dsem):
                if c and e.seen.get(("d", k), 0) < c:
                    e.h.wait_ge(s, c); e.seen[("d", k)] = c


class _Stop(Exception):
    pass


def build_nc(n_layers=NL, n_tiles=8, dbg=False, stop=None):
    nc = bass.Bass("TRN2", target_bir_lowering=False, dynamic_dma_scratch_size=512)
    es = contextlib.ExitStack()
    S = Sync(nc, es)

    def din(name, shape):
        return nc.dram_tensor(name, list(shape), F32, kind="ExternalInput").ap()
    x_in = din("x", [SEQ, D])
    attn_norm_g = din("attn_norm_g", [NL, D]); w_in = din("w_in", [NL, D, NIN])
    idx_g = din("idx_k_norm_g", [NL, 64]); idx_b = din("idx_k_norm_b", [NL, 64])
    sgu_g = din("sgu_norm_g", [NL, 256]); sgu_b = din("sgu_norm_b", [NL, 256])
    sgu_w = din("sgu_w_s", [NL, 4, 128, 128]); sgu_bs = din("sgu_b_s", [NL, 4, 128])
    dlam = din("diff_lambda", [NL, 4, 32]); subln = din("diff_subln_g", [NL, 64])
    wbrs = [din("w_branch_a", [NL, 256, D]), din("w_branch_b", [NL, 256, D]), din("w_branch_c", [NL, 256, D])]
    w_out = din("w_out", [NL, D, D]); mlp_norm_g = din("mlp_norm_g", [NL, D])
    w_ff1 = din("w_ff1", [NL, D, DFF]); w_ff2 = din("w_ff2", [NL, DFF, D])
    fin_g = din("final_norm_g", [1, D])
    consts = din("consts", [128, 512]); rampv = din("rampv", [1, SEQ])
    out = nc.dram_tensor("out", [SEQ, D], F32, kind="ExternalOutput").ap()

    def dscr(name, shape, dt):
        return nc.dram_tensor(name, list(shape), dt, kind="Internal").ap()
    win_bf = [dscr("win_bf%d" % l, [8, 128, NIN], BF16) for l in range(NL)]
    wff1_bf = [dscr("wff1_bf%d" % l, [8, 128, DFF], BF16) for l in range(NL)]
    wff2_bf = [dscr("wff2_bf%d" % l, [32, 128, D], BF16) for l in range(NL)]
    wout_bf = [dscr("wout_bf%d" % l, [8, 128, D], BF16) for l in range(NL)]
    wbr_bf = [dscr("wbr_bf%d" % l, [6, 128, D], BF16) for l in range(NL)]
    xs = dscr("xs", [SEQ, D], F32)
    t_prep = [Trk() for _ in range(NL)]
    t_xs = [Trk() for _ in range(8)]
    t_in = Trk()

    def sb(name, shape, dt, stack=es):
        return stack.enter_context(nc.sbuf_tensor(name, list(shape), dt))

    pe, act, dve, pool = S.pe, S.act, S.dve, S.pool

    cst = sb("cst", [128, 512], F32); t_cst = Trk()
    ident_bf = sb("ident_bf", [128, 128], BF16)
    cmb_bf = sb("cmb_bf", [128, 128], BF16)
    neghalf = sb("neghalf", [128, 1], F32)
    gpre = sb("gpre", [128, 2 * NL * 8], F32); t_gpre = Trk()
    S.dma(cst[:], consts[:, :], r=[t_in], w=[t_cst])
    causal_big = cst[:, 128:256]
    tril = cst[:, 384:512]
    S.op(dve, lambda: nc.vector.tensor_copy(out=ident_bf[:], in_=cst[:, 0:128]), r=[t_cst], w=[t_cst])
    S.op(dve, lambda: nc.vector.tensor_copy(out=cmb_bf[:], in_=cst[:, 256:384]), r=[t_cst], w=[t_cst])
    S.op(pool, lambda: nc.gpsimd.memset(neghalf[:], -0.5), w=[t_cst])
    for l in range(NL):
        S.dma(gpre[:, l * 16:l * 16 + 8], attn_norm_g[l].rearrange("(k p) -> p k", p=128), r=[t_in], wa=[t_gpre],
              allow_slow_non_contiguous=True)
        S.dma(gpre[:, l * 16 + 8:l * 16 + 16], mlp_norm_g[l].rearrange("(k p) -> p k", p=128), r=[t_in], wa=[t_gpre],
              allow_slow_non_contiguous=True)

    with contextlib.ExitStack() as pes:
        NB = 3
        pin = [sb("pin%d" % i, [128, 1024], F32, pes) for i in range(NB)]
        pout = [sb("pout%d" % i, [128, 1024], BF16, pes) for i in range(NB)]
        t_pin = [Trk() for _ in range(NB)]; t_pout = [Trk() for _ in range(NB)]
        cnt = [0]

        def piece(l, src_ap, dst_ap, w, gcol, dst_view=None):
            i = cnt[0] % NB; cnt[0] += 1
            S.dma(pin[i][:, 0:w], src_ap, r=[t_in], w=[t_pin[i]])
            e = [dve, act, pool][cnt[0] % 3]
            if gcol is None:
                if e is act:
                    fn = lambda: nc.scalar.copy(out=pout[i][:, 0:w], in_=pin[i][:, 0:w])
                else:
                    fn = lambda: e.h.tensor_copy(out=pout[i][:, 0:w], in_=pin[i][:, 0:w])
            else:
                g = gpre[:, gcol:gcol + 1]
                if e is act:
                    fn = lambda: nc.scalar.activation(out=pout[i][:, 0:w], in_=pin[i][:, 0:w], func=AF.Copy, scale=g)
                else:
                    fn = lambda: e.h.tensor_scalar(out=pout[i][:, 0:w], in0=pin[i][:, 0:w], scalar1=g, scalar2=None,
                                                   op0=ALU.mult)
            S.op(e, fn, r=[t_pin[i], t_gpre], w=[t_pout[i]])
            src = pout[i][:, 0:w] if dst_view is None else dst_view(pout[i])
            S.dma(dst_ap, src, r=[t_pout[i]], wa=[t_prep[l]])

        segs = [(0, FM_AQ, 512), (768, FM_IQ, 256), (1604, FM_CQ, 512), (512, TM_AV, 256), (1024, TM_IK, 68),
                (2116, TM_CV, 256), (1092, TM_BUV, 512)]
        for l in range(n_layers):
            for kc in range(8):
                rows = slice(kc * 128, (kc + 1) * 128)
                for (s0, d0, w) in segs:
                    piece(l, w_in[l, rows, s0:s0 + w], win_bf[l][kc, :, d0:d0 + w], w, l * 16 + kc)
                for i in range(3):
                    dst = win_bf[l][kc, :, FM_G:FM_G + 3072].rearrange("p (f i c) -> p f i c", f=8, i=3)[:, :, i, :]
                    piece(l, w_in[l, rows, 2372 + i * 1024:2372 + (i + 1) * 1024], dst, 1024, l * 16 + kc,
                          dst_view=lambda t: t[:, 0:1024].rearrange("p (f c) -> p f c", f=8))
                for c in range(4):
                    piece(l, w_ff1[l, rows, c * 1024:(c + 1) * 1024], wff1_bf[l][kc, :, c * 1024:(c + 1) * 1024], 1024,
                          l * 16 + 8 + kc)
                piece(l, w_out[l, rows, :], wout_bf[l][kc, :, :], 1024, None)
            for kc in range(32):
                piece(l, w_ff2[l, kc * 128:(kc + 1) * 128, :], wff2_bf[l][kc, :, :], 1024, None)
            for i in range(3):
                for c in range(2):
                    piece(l, wbrs[i][l, c * 128:(c + 1) * 128, :], wbr_bf[l][i * 2 + c, :, :], 1024, None)
        S.barrier()
    if stop == 'prep':
        S.barrier(); es.close(); return nc, S, {}

    ramp = sb("ramp", [128, SEQ], BF16); t_ramp = Trk()
    KaT = sb("KaT", [128, 2, SEQ], BF16); t_KaT = [Trk() for _ in range(8)]
    KcT = sb("KcT", [128, 2, SEQ], BF16); t_KcT = [Trk() for _ in range(8)]
    KiT = sb("KiT", [128, SEQ], BF16); t_KiT = [Trk() for _ in range(8)]
    Vae = sb("Vae", [128, 32, 260], BF16); t_Vae = [Trk() for _ in range(8)]
    Vce = sb("Vce", [128, 32, 260], BF16); t_Vce = [Trk() for _ in range(8)]
    xb = sb("xb", [128, 4, D], F32); t_xb = [Trk() for _ in range(4)]
    hT = sb("hT", [128, 8, 512], BF16); t_hT = Trk()
    QaT = sb("QaT", [128, 2, 512], BF16); t_QaT = Trk()
    QiT = sb("QiT", [128, 2, 512], BF16); t_QiT = Trk()
    QcT = sb("QcT", [128, 2, 512], BF16); t_QcT = Trk()
    acc = sb("acc", [128, SEQ], F32); t_acc = Trk()
    aT = acc[:, :].bitcast(BF16).rearrange("p (c t) -> p c t", c=16); t_aT = t_acc
    o1 = acc[:, 0:1024].rearrange("p (q e) -> p q e", q=4); t_o1 = [t_acc] * 4
    mb = sb("mb", [128, 4, SEQ], BF16); t_mb = [Trk() for _ in range(4)]
    mergedT = mb[:, 0, :].rearrange("p (k t) -> p k t", k=8); t_mT = t_mb[0]
    gfin = mb[:, 1, :].bitcast(F32)[:, 0:D]; t_gfin = t_mb[1]
    yT = sb("yT", [128, 3, 2, 512], BF16); t_yT = [Trk() for _ in range(3)]
    NW = 2
    wbuf = [sb("wbuf%d" % i, [128, 8, 512], BF16) for i in range(NW)]; t_wbuf = [Trk() for _ in range(NW)]
    wbr_sb = sb("wbr_sb", [128, 6, 512], BF16); t_wbr = Trk()
    NPT = 3
    PT = [sb("PT%d" % i, [128, 512], BF16) for i in range(NPT)]; t_PT = [Trk() for _ in range(NPT)]
    RR = sb("RR", [128, 5, 512], F32); t_RR = [Trk() for _ in range(5)]
    sig = [RR[:, i, :] for i in range(3)]; t_sig = t_RR[0:3]
    tm = [RR[:, 3 + i, :] for i in range(2)]; t_tm = t_RR[3:5]
    rt = [RR[:, i, :] for i in range(2)]; t_rt = t_RR[0:2]
    gbuf = RR[:, 2, :]; t_gbuf = t_RR[2]
    hn = sb("hn", [128, D], BF16); t_hn = Trk()
    ss = sb("ss", [128, 4], F32); t_ss = [Trk() for _ in range(4)]
    ms = sb("ms", [128, 4], F32); t_ms = [Trk() for _ in range(4)]
    rstd = sb("rstd", [128, 4], F32); t_rstd = [Trk() for _ in range(4)]
    absw = sb("absw", [128, 16], F32); sgn = sb("sgn", [128, 16], F32); wsc = sb("wsc", [128, 16], F32)
    t_iw = [Trk() for _ in range(4)]
    st6 = sb("st6", [128, 6], F32); mv = sb("mv", [128, 2], F32); lnr = sb("lnr", [128, 2], F32); t_ln = Trk()
    ln1 = sb("ln1", [128, 256], F32); ln2 = sb("ln2", [128, 256], F32); t_ln1 = Trk(); t_ln2 = Trk()
    knd = sb("knd", [128, 128], BF16); t_knd = Trk()
    vn = sb("vn", [128, 256], BF16); t_vn = Trk()
    ybt = sb("ybt", [128, 256], BF16); t_ybt = Trk()
    mid = sb("mid", [128, 1], F32); cntt = sb("cntt", [128, 1], F32); ttt = sb("ttt", [128, 1], F32)
    thr = sb("thr", [128, 1], F32); t_bis = Trk(); t_mid = Trk(); t_bisa = Trk()
    sneg = sb("sneg", [128, 1], F32)
    rden = sb("rden", [128, 4], F32); t_rden = Trk()
    sq = sb("sq", [128, 256], F32); t_sq = Trk()
    oo = sb("oo", [128, 256], F32); t_oo = Trk()
    ssc = sb("ssc", [128, 4], F32); t_ssc = Trk()
    idxg_bc = sb("idxg_bc", [128, 64], F32); idxb_bc = sb("idxb_bc", [128, 64], F32)
    sgug_bc = sb("sgug_bc", [128, 256], F32); sgub_bc = sb("sgub_bc", [128, 256], F32)
    wsf = sb("wsf", [128, 4, 128], F32); wsb = sb("wsb", [128, 4, 128], BF16); WcT = sb("WcT", [128, 4, 128], BF16)
    bs = sb("bs", [128, 4], F32)
    lamt = sb("lamt", [128, 128], F32); lamp = sb("lamp", [128, 64], F32); lam2 = sb("lam2", [128, 2], F32)
    neglam = sb("neglam", [128, 1], F32)
    gsub = sb("gsub", [128, 64], F32)
    t_par = Trk()

    ps = [es.enter_context(nc.psum_tensor("ps%d" % i, [128, 512], F32)) for i in range(7)]
    psT = es.enter_context(nc.psum_tensor("psT", [128, 1024], BF16))
    t_ps = [Trk(True) for _ in range(7)]; t_psT = Trk(True)
    rot = {"i": 0}

    def nb(lst=(4, 5, 6)):
        rot["i"] += 1
        return lst[rot["i"] % len(lst)]
    ALLB = (0, 1, 2, 3, 4, 5, 6)
    wrot = {"i": 0}

    def wslot():
        wrot["i"] += 1
        return wrot["i"] % NW
    evrot = {"i": 0}

    def evac_copy(out_ap, in_ap, r, w=(), wa=()):
        evrot["i"] += 1
        if evrot["i"] % 2:
            S.op(act, lambda: nc.scalar.copy(out=out_ap, in_=in_ap), r=r, w=w, wa=wa)
        else:
            S.op(dve, lambda: nc.vector.tensor_copy(out=out_ap, in_=in_ap), r=r, w=w, wa=wa)

    S.op(pool, lambda: nc.gpsimd.memset(Vae[:], 1.0), w=t_Vae)
    S.op(pool, lambda: nc.gpsimd.memset(Vce[:], 1.0), w=t_Vce)
    for q in range(4):
        S.dma(acc[:, q * 1024:(q + 1) * 1024], rampv[0:1, q * 1024:(q + 1) * 1024].partition_broadcast(128), r=[t_in],
              wa=[t_acc])
    S.op(dve, lambda: nc.vector.tensor_copy(out=ramp[:], in_=acc[:]), r=[t_acc], w=[t_ramp])

    dbg_outs = {}

    chkc = {}

    def chk(tag):
        chkc[tag] = chkc.get(tag, 0) + 1
        if stop == tag or stop == "%s:%d" % (tag, chkc[tag]):
            raise _Stop()

    def dump(name, ap, trks, shape, dt=F32):
        if not dbg: return
        d = nc.dram_tensor("dbg_" + name, list(shape), dt, kind="ExternalOutput").ap()
        S.dma(d, ap, r=trks)
        dbg_outs[name] = shape

    def rmsnorm_to_hT(b):
        S.op(act, lambda: nc.scalar.activation(out=hn[:], in_=xb[:, b, :], func=AF.Square, accum_out=ss[:, b:b + 1]),
             r=[t_xb[b]], w=[t_hn, t_ss[b]])
        S.op(dve, lambda: nc.vector.tensor_scalar(out=ms[:, b:b + 1], in0=ss[:, b:b + 1], scalar1=1.0 / D, scalar2=EPS,
                                                  op0=ALU.mult, op1=ALU.add), r=[t_ss[b]], w=[t_ms[b]])
        S.op(pool, lambda: nc.gpsimd.tensor_tensor(out=rstd[:, b:b + 1], in0=ms[:, b:b + 1], in1=neghalf[:, 0:1],
                                                   op=ALU.pow), r=[t_ms[b], t_cst], w=[t_rstd[b]])
        S.op(dve, lambda: nc.vector.tensor_scalar(out=hn[:], in0=xb[:, b, :], scalar1=rstd[:, b:b + 1], scalar2=None,
                                                  op0=ALU.mult), r=[t_xb[b], t_rstd[b]], w=[t_hn])
        for kc in range(8):
            S.op(pe, lambda kc=kc: nc.tensor.transpose(psT[:, kc * 128:(kc + 1) * 128], hn[:, kc * 128:(kc + 1) * 128],
                                                      ident_bf[:]), r=[t_hn, t_cst], w=[t_psT])
        evac_copy(hT[:, :, b * 128:(b + 1) * 128], psT[:, :].rearrange("p (k t) -> p k t", k=8), r=[t_psT], wa=[t_hT])

    def load_w(src_ap, nk, cw, prep_t):
        s = wslot()
        S.dma(wbuf[s][:, 0:nk, 0:cw], src_ap, r=[prep_t], w=[t_wbuf[s]])
        return s

    def layernorm_stats(in_ap):
        S.op(dve, lambda: nc.vector.bn_stats(out=st6[:], in_=in_ap[0]), r=in_ap[1], w=[t_ln])
        chk('LN1')
        S.op(dve, lambda: nc.vector.bn_aggr(out=mv[:], in_=st6[:]), r=[t_ln], w=[t_ln])
        chk('LN2')
        S.op(dve, lambda: nc.vector.tensor_scalar(out=lnr[:, 1:2], in0=mv[:, 1:2], scalar1=EPS, scalar2=None, op0=ALU.add),
             r=[t_ln], w=[t_ln])
        chk('LN3')
        S.op(pool, lambda: nc.gpsimd.tensor_tensor(out=lnr[:, 0:1], in0=lnr[:, 1:2], in1=neghalf[:, 0:1], op=ALU.pow),
             r=[t_ln, t_cst], w=[t_ln])

    try:
      for l in range(n_layers):
        lambda_init = 0.8 - 0.6 * math.exp(-0.3 * l)
        last = (l == n_layers - 1)
        xsrc = x_in if l == 0 else xs
        S.dma(idxg_bc[:], idx_g[l:l + 1, :].partition_broadcast(128), r=[t_in], w=[t_par])
        S.dma(idxb_bc[:], idx_b[l:l + 1, :].partition_broadcast(128), r=[t_in], wa=[t_par])
        S.dma(sgug_bc[:], sgu_g[l:l + 1, :].partition_broadcast(128), r=[t_in], wa=[t_par])
        S.dma(sgub_bc[:], sgu_b[l:l + 1, :].partition_broadcast(128), r=[t_in], wa=[t_par])
        S.dma(wsf[:], sgu_w[l].rearrange("g t s -> t g s"), r=[t_in], wa=[t_par])
        S.dma(bs[:], sgu_bs[l].rearrange("g t -> t g"), r=[t_in], wa=[t_par], allow_slow_non_contiguous=True)
        S.dma(lamt[:], dlam[l:l + 1].rearrange("o a b -> o (a b)").partition_broadcast(128), r=[t_in], wa=[t_par])
        S.dma(gsub[:], subln[l:l + 1, :].partition_broadcast(128), r=[t_in], wa=[t_par])
        for g in range(4):
            S.op(dve, lambda g=g: nc.vector.tensor_tensor(out=wsb[:, g, :], in0=wsf[:, g, :], in1=tril, op=ALU.mult),
                 r=[t_par, t_cst], w=[t_par])
        for g in range(4):
            S.op(pe, lambda g=g: nc.tensor.transpose(psT[:, g * 128:(g + 1) * 128], wsb[:, g, :], ident_bf[:]),
                 r=[t_par, t_cst], w=[t_psT])
        S.op(dve, lambda: nc.vector.tensor_copy(out=WcT[:], in_=psT[:, 0:512].rearrange("p (g t) -> p g t", g=4)),
             r=[t_psT], w=[t_par])
        lt4 = lamt[:].rearrange("p (a b) -> p a b", a=4)
        S.op(dve, lambda: nc.vector.tensor_tensor(out=lamp[:].rearrange("p (a b) -> p a b", a=2), in0=lt4[:, 0:4:2, :],
                                                  in1=lt4[:, 1:4:2, :], op=ALU.mult), r=[t_par], w=[t_par])
        S.op(dve, lambda: nc.vector.tensor_reduce(out=lam2[:], in_=lamp[:].rearrange("p (a b) -> p a b", a=2), axis=AX.X,
                                                  op=ALU.add), r=[t_par], w=[t_par])
        S.op(act, lambda: nc.scalar.activation(out=lam2[:], in_=lam2[:], func=AF.Exp), r=[t_par], w=[t_par])
        S.op(dve, lambda: nc.vector.tensor_tensor(out=neglam[:], in0=lam2[:, 1:2], in1=lam2[:, 0:1], op=ALU.subtract),
             r=[t_par], w=[t_par])
        S.op(dve, lambda: nc.vector.tensor_scalar(out=neglam[:], in0=neglam[:], scalar1=-lambda_init, scalar2=None,
                                                  op0=ALU.add), r=[t_par], w=[t_par])
        S.op(dve, lambda: nc.vector.tensor_scalar(out=gsub[:], in0=gsub[:], scalar1=(1.0 - lambda_init), scalar2=None,
                                                  op0=ALU.mult), r=[t_par], w=[t_par])

        chk('par')
        for j in range(n_tiles):
            nkb = 4 * j + 4
            for b in range(4):
                gb = 4 * j + b
                S.dma(xb[:, b, :], xsrc[gb * 128:(gb + 1) * 128, :], r=[t_in if l == 0 else t_xs[j]], w=[t_xb[b]])
            for b in range(4):
                rmsnorm_to_hT(b)

            chk('A')
            s1 = load_w(win_bf[l][:, :, TM_AV:TM_AV + 324].rearrange("k p c -> p k c"), 8, 324, t_prep[l])
            s2 = load_w(win_bf[l][:, :, TM_CV:TM_CV + 256].rearrange("k p c -> p k c"), 8, 256, t_prep[l])
            for b in range(4):
                gb = 4 * j + b
                bk = nb(ALLB)
                for kc in range(8):
                    S.op(pe, lambda kc=kc: nc.tensor.matmul(ps[bk][:, 0:324], lhsT=hT[:, kc, b * 128:(b + 1) * 128],
                                                            rhs=wbuf[s1][:, kc, 0:324], start=(kc == 0), stop=(kc == 7)),
                         r=[t_hT, t_wbuf[s1]], w=[t_ps[bk]])
                chk('B1a')
                evac_copy(Vae[:, gb, :].rearrange("p (h e) -> p h e", h=4)[:, :, 0:64],
                          ps[bk][:, 0:256].rearrange("p (h e) -> p h e", h=4), r=[t_ps[bk]], wa=[t_Vae[j]])
                chk('B1b')
                S.op(act, lambda: nc.scalar.copy(out=ln2[:, 64:128], in_=ps[bk][:, 256:320]), r=[t_ps[bk]], w=[t_ln2])
                chk('LN0')
                layernorm_stats((ln2[:, 64:128], [t_ln2]))
                chk('B1b1')
                S.op(dve, lambda: nc.vector.tensor_scalar(out=ln1[:, 0:64], in0=ln2[:, 64:128], scalar1=mv[:, 0:1],
                                                          scalar2=lnr[:, 0:1], op0=ALU.subtract, op1=ALU.mult),
                     r=[t_ln2, t_ln], w=[t_ln1])
                chk('B1b2')
                S.op(dve, lambda: nc.vector.tensor_tensor(out=ln2[:, 0:64], in0=ln1[:, 0:64], in1=idxg_bc[:], op=ALU.mult),
                     r=[t_ln1, t_par], w=[t_ln2])
                chk('B1b3')
                S.op(dve, lambda: nc.vector.tensor_tensor(out=knd[:, 0:64], in0=ln2[:, 0:64], in1=idxb_bc[:], op=ALU.add),
                     r=[t_ln2, t_par], w=[t_knd])
                S.op(dve, lambda: nc.vector.tensor_tensor(out=knd[:, 64:128], in0=ln2[:, 0:64], in1=idxb_bc[:], op=ALU.add),
                     r=[t_ln2, t_par], wa=[t_knd])
                chk('B1c0')
                S.op(pe, lambda: nc.tensor.transpose(psT[:, 0:128], knd[:], ident_bf[:]), r=[t_knd, t_cst], w=[t_psT])
                evac_copy(KiT[:, gb * 128:(gb + 1) * 128], psT[:, 0:128], r=[t_psT], wa=[t_KiT[j]])
                chk('B1c')
                S.op(dve, lambda: nc.vector.tensor_scalar(out=wsc[:, b * 4:b * 4 + 4], in0=ps[bk][:, 320:324],
                                                          scalar1=1.0 / 16.0, scalar2=None, op0=ALU.mult),
                     r=[t_ps[bk]], w=[t_iw[b]])
                S.op(dve, lambda: nc.vector.scalar_tensor_tensor(out=absw[:, b * 4:b * 4 + 4], in0=wsc[:, b * 4:b * 4 + 4],
                                                                 scalar=-1.0, in1=wsc[:, b * 4:b * 4 + 4], op0=ALU.mult,
                                                                 op1=ALU.max), r=[t_iw[b]], w=[t_iw[b]])
                S.op(dve, lambda: nc.vector.tensor_scalar(out=sgn[:, b * 4:b * 4 + 4], in0=wsc[:, b * 4:b * 4 + 4],
                                                          scalar1=0.0, scalar2=2.0, op0=ALU.is_ge, op1=ALU.mult),
                     r=[t_iw[b]], w=[t_iw[b]])
                S.op(dve, lambda: nc.vector.tensor_scalar(out=sgn[:, b * 4:b * 4 + 4], in0=sgn[:, b * 4:b * 4 + 4],
                                                          scalar1=-1.0, scalar2=None, op0=ALU.add),
                     r=[t_iw[b]], w=[t_iw[b]])
                chk('B1d')
                bk2 = nb(ALLB)
                for kc in range(8):
                    S.op(pe, lambda kc=kc: nc.tensor.matmul(ps[bk2][:, 0:256], lhsT=hT[:, kc, b * 128:(b + 1) * 128],
                                                            rhs=wbuf[s2][:, kc, 0:256], start=(kc == 0), stop=(kc == 7)),
                         r=[t_hT, t_wbuf[s2]], w=[t_ps[bk2]])
                evac_copy(Vce[:, gb, :].rearrange("p (h e) -> p h e", h=4)[:, :, 0:64],
                          ps[bk2][:, 0:256].rearrange("p (h e) -> p h e", h=4), r=[t_ps[bk2]], wa=[t_Vce[j]])
            chk('B1')
            chk('B1e')
            s3 = load_w(win_bf[l][:, :, TM_BUV:TM_BUV + 512].rearrange("k p c -> p k c"), 8, 512, t_prep[l])
            for b in range(4):
                bk = nb(ALLB)
                for kc in range(8):
                    S.op(pe, lambda kc=kc: nc.tensor.matmul(ps[bk][:, 0:512], lhsT=hT[:, kc, b * 128:(b + 1) * 128],
                                                            rhs=wbuf[s3][:, kc, 0:512], start=(kc == 0), stop=(kc == 7)),
                         r=[t_hT, t_wbuf[s3]], w=[t_ps[bk]])
                S.op(act, lambda: nc.scalar.activation(out=gbuf, in_=ps[bk][:, 0:512], func=AF.Gelu_apprx_tanh),
                     r=[t_ps[bk]], w=[t_gbuf])
                layernorm_stats((gbuf[:, 256:512], [t_gbuf]))
                S.op(dve, lambda: nc.vector.tensor_scalar(out=ln1[:], in0=gbuf[:, 256:512], scalar1=mv[:, 0:1],
                                                          scalar2=lnr[:, 0:1], op0=ALU.subtract, op1=ALU.mult),
                     r=[t_gbuf, t_ln], w=[t_ln1])
                S.op(pool, lambda: nc.gpsimd.tensor_tensor(out=ln2[:], in0=ln1[:], in1=sgug_bc[:], op=ALU.mult),
                     r=[t_ln1, t_par], w=[t_ln2])
                S.op(pool, lambda: nc.gpsimd.tensor_tensor(out=vn[:], in0=ln2[:], in1=sgub_bc[:], op=ALU.add),
                     r=[t_ln2, t_par], w=[t_vn])
                bk2 = nb(ALLB)
                for g in range(4):
                    S.op(pe, lambda g=g: nc.tensor.matmul(ps[bk2][:, g * 64:(g + 1) * 64], lhsT=WcT[:, g, :],
                                                          rhs=vn[:, g * 64:(g + 1) * 64], start=(g == 0), stop=(g == 3),
                                                          skip_group_check=True), r=[t_vn, t_par], w=[t_ps[bk2]])
                for g in range(4):
                    S.op(dve, lambda g=g: nc.vector.scalar_tensor_tensor(
                        out=ybt[:, g * 64:(g + 1) * 64], in0=ps[bk2][:, g * 64:(g + 1) * 64], scalar=bs[:, g:g + 1],
                        in1=gbuf[:, g * 64:(g + 1) * 64], op0=ALU.add, op1=ALU.mult),
                        r=[t_ps[bk2], t_par, t_gbuf], w=[t_ybt] if g == 0 else [], wa=[] if g == 0 else [t_ybt])
                for c in range(2):
                    S.op(pe, lambda c=c: nc.tensor.transpose(psT[:, c * 128:(c + 1) * 128], ybt[:, c * 128:(c + 1) * 128],
                                                            ident_bf[:]), r=[t_ybt, t_cst], w=[t_psT])
                evac_copy(yT[:, 1, :, b * 128:(b + 1) * 128], psT[:, 0:256].rearrange("p (c t) -> p c t", c=2),
                          r=[t_psT], wa=[t_yT[1]])
            chk('B2')
            fm = [(FM_AQ, 512, [("qa", 0), ("qa", 1), ("ka", 0), ("ka", 1)]),
                  (FM_IQ, 512, [("qi", 0), ("qi", 1), ("qc", 0), ("qc", 1)]),
                  (FM_CK, 256, [("kc", 0), ("kc", 1)])]
            for (c0, cw, dests) in fm:
                s = load_w(win_bf[l][:, :, c0:c0 + cw].rearrange("k p c -> p k c"), 8, cw, t_prep[l])
                for ci, (kind, c) in enumerate(dests):
                    bk = nb(ALLB)
                    for kc in range(8):
                        S.op(pe, lambda kc=kc: nc.tensor.matmul(ps[bk][:, 0:512], lhsT=wbuf[s][:, kc, ci * 128:(ci + 1) * 128],
                                                                rhs=hT[:, kc, :], start=(kc == 0), stop=(kc == 7)),
                             r=[t_hT, t_wbuf[s]], w=[t_ps[bk]])
                    if kind == "qa": evac_copy(QaT[:, c, :], ps[bk][:, :], r=[t_ps[bk]], wa=[t_QaT])
                    elif kind == "qi": evac_copy(QiT[:, c, :], ps[bk][:, :], r=[t_ps[bk]], wa=[t_QiT])
                    elif kind == "qc": evac_copy(QcT[:, c, :], ps[bk][:, :], r=[t_ps[bk]], wa=[t_QcT])
                    elif kind == "ka": evac_copy(KaT[:, c, j * 512:(j + 1) * 512], ps[bk][:, :], r=[t_ps[bk]], wa=[t_KaT[j]])
                    elif kind == "kc": evac_copy(KcT[:, c, j * 512:(j + 1) * 512], ps[bk][:, :], r=[t_ps[bk]], wa=[t_KcT[j]])

            chk('B3')
            for qb in range(4):
                gb = 4 * j + qb
                ncols = (gb + 1) * 128
                for kg in range((ncols + 511) // 512):
                    c0 = kg * 512; cw = min(512, ncols - c0)
                    for h in range(4):
                        bk = nb(ALLB)
                        rs = slice((h % 2) * 64, (h % 2) * 64 + 64)
                        S.op(pe, lambda: nc.tensor.matmul(ps[bk][:, 0:cw], lhsT=QiT[rs, h // 2, qb * 128:(qb + 1) * 128],
                                                          rhs=KiT[rs, c0:c0 + cw], start=True, stop=True),
                             r=[t_QiT] + t_KiT[0:j + 1], w=[t_ps[bk]])
                        S.op(act, lambda: nc.scalar.activation(out=ps[bk][:, 0:cw], in_=ps[bk][:, 0:cw], func=AF.Relu,
                                                               scale=absw[:, qb * 4 + h:qb * 4 + h + 1]),
                             r=[t_iw[qb]], w=[t_ps[bk]])
                        in1 = ramp[:, c0:c0 + cw] if h == 0 else acc[:, c0:c0 + cw]
                        S.op(dve, lambda: nc.vector.scalar_tensor_tensor(
                            out=acc[:, c0:c0 + cw], in0=ps[bk][:, 0:cw], scalar=sgn[:, qb * 4 + h:qb * 4 + h + 1], in1=in1,
                            op0=ALU.mult, op1=ALU.add), r=[t_ps[bk], t_iw[qb], t_ramp], w=[t_acc])
                S.op(dve, lambda: nc.vector.tensor_tensor(out=acc[:, gb * 128:(gb + 1) * 128],
                                                          in0=acc[:, gb * 128:(gb + 1) * 128], in1=causal_big, op=ALU.add),
                     r=[t_cst], w=[t_acc])
                if gb >= 2:
                    n1 = (int(ncols * 0.40) // 64) * 64; n2 = ncols - n1
                    S.op(dve, lambda: nc.vector.memset(mid[:], 0.0), w=[t_bis, t_mid])
                    for k in range(NBIS):
                        ck = RNG / (2.0 ** k)
                        S.op(dve, lambda: nc.vector.tensor_scalar(out=mb[:, qb, 0:n1], in0=acc[:, 0:n1],
                                                                  scalar1=mid[:, 0:1], scalar2=None, op0=ALU.is_ge,
                                                                  op1=ALU.add, accum_out=cntt[:]),
                             r=[t_acc, t_mid], w=[t_bis], wa=[t_mb[qb]])
                        S.op(act, lambda: nc.scalar.activation(out=mb[:, qb, n1:ncols], in_=acc[:, n1:ncols], func=AF.Sign,
                                                               scale=-1.0, bias=mid[:, 0:1], accum_out=sneg[:]),
                             r=[t_acc, t_mid], w=[t_bisa], wa=[t_mb[qb]])
                        S.op(dve, lambda: nc.vector.scalar_tensor_tensor(out=ttt[:], in0=cntt[:], scalar=2.0, in1=sneg[:],
                                                                         op0=ALU.mult, op1=ALU.subtract),
                             r=[t_bisa], w=[t_bis])
                        S.op(dve, lambda: nc.vector.tensor_scalar(out=ttt[:], in0=ttt[:], scalar1=512.0 - n2 - 0.5,
                                                                  scalar2=ck, op0=ALU.is_ge, op1=ALU.mult), w=[t_bis])
                        S.op(dve, lambda: nc.vector.scalar_tensor_tensor(out=mid[:], in0=ttt[:], scalar=-ck / 2.0,
                                                                         in1=mid[:], op0=ALU.add, op1=ALU.add),
                             w=[t_bis, t_mid])
                    cK = RNG / (2.0 ** NBIS)
                    S.op(dve, lambda: nc.vector.tensor_scalar(out=thr[:], in0=mid[:], scalar1=-cK, scalar2=None,
                                                              op0=ALU.add), w=[t_bis])
                else:
                    S.op(dve, lambda: nc.vector.memset(thr[:], -RNG), w=[t_bis])
                S.op(pool, lambda: nc.gpsimd.tensor_scalar(out=mb[:, qb, 0:ncols], in0=acc[:, 0:ncols], scalar1=thr[:, 0:1],
                                                           scalar2=MASKV, op0=ALU.is_lt, op1=ALU.mult),
                     r=[t_acc, t_bis], w=[t_mb[qb]])

            chk('C')
            def attention(kind, comp):
                if kind == "a":
                    KT, QT, VE, tK, tQ, tV = KaT, QaT, Vae, t_KaT, t_QaT, t_Vae
                    scale = 64 ** -0.5
                else:
                    KT, QT, VE, tK, tQ, tV = KcT, QcT, Vce, t_KcT, t_QcT, t_Vce
                    scale = 32 ** -0.5
                steps = [(h, kb) for h in range(4) for kb in range(nkb)]
                banks = {}
                LA = 2

                def qk(i):
                    h, kb = steps[i]
                    r0 = max(kb - 4 * j, 0); c0 = r0 * 128
                    bk = nb((4, 5, 6)); banks[i] = bk
                    if kind == "a":
                        rs = slice((h % 2) * 64, (h % 2) * 64 + 64); ch = h // 2; tp = None
                    else:
                        idx = h * 2 + comp
                        rs = slice((idx % 4) * 32, (idx % 4) * 32 + 32); ch = idx // 4; tp = ((idx % 4) * 32, 0)
                    kw = {} if tp is None else {"tile_position": tp}
                    S.op(pe, lambda: nc.tensor.matmul(ps[bk][:, c0:512], lhsT=KT[rs, ch, kb * 128:(kb + 1) * 128],
                                                      rhs=QT[rs, ch, c0:512], start=True, stop=False, **kw),
                         r=[tQ, tK[kb // 4]], w=[t_ps[bk]])
                    if kind == "a":
                        for qb in range(r0, 4):
                            S.op(pe, lambda qb=qb: nc.tensor.matmul(ps[bk][:, qb * 128:(qb + 1) * 128],
                                                                    lhsT=mb[:, qb, kb * 128:(kb + 1) * 128], rhs=ident_bf[:],
                                                                    start=False, stop=(qb == 3), skip_group_check=True),
                                 r=[t_mb[qb], t_cst], w=[t_ps[bk]])
                    else:
                        if kb >= 4 * j:
                            S.op(pe, lambda: nc.tensor.matmul(ps[bk][:, r0 * 128:(r0 + 1) * 128], lhsT=cmb_bf[:],
                                                              rhs=ident_bf[:], start=False, stop=True,
                                                              skip_group_check=True), r=[t_cst], w=[t_ps[bk]])
                    pi = i % NPT
                    S.op(act, lambda: nc.scalar.activation(out=PT[pi][:, c0:512], in_=ps[bk][:, c0:512], func=AF.Exp,
                                                           scale=scale), r=[t_ps[bk]], w=[t_PT[pi]])

                def pv(i):
                    h, kb = steps[i]
                    r0 = max(kb - 4 * j, 0)
                    pi = i % NPT
                    for qb in range(r0, 4):
                        S.op(pe, lambda qb=qb: nc.tensor.matmul(ps[qb][:, h * 65:(h + 1) * 65],
                                                                lhsT=PT[pi][:, qb * 128:(qb + 1) * 128],
                                                                rhs=VE[:, kb, h * 65:(h + 1) * 65],
                                                                start=(h == 0 and kb == 0), stop=(kb == 4 * j + qb),
                                                                skip_group_check=True),
                             r=[t_PT[pi], tV[kb // 4]], w=[t_ps[qb]])
                n = len(steps)
                for i in range(min(LA, n)): qk(i)
                for i in range(n):
                    if i + LA < n: qk(i + LA)
                    pv(i)

            def out_views(qb):
                o4 = ps[qb][:, 0:260].rearrange("p (h e) -> p h e", h=4)
                return o4[:, :, 0:64], o4[:, :, 64:65]

            def transpose_to_yT(src, t_src, br, qb):
                for c in range(2):
                    S.op(pe, lambda c=c: nc.tensor.transpose(psT[:, c * 128:(c + 1) * 128], src[:, c * 128:(c + 1) * 128],
                                                            ident_bf[:]), r=[t_src, t_cst], w=[t_psT])
                evac_copy(yT[:, br, :, qb * 128:(qb + 1) * 128], psT[:, 0:256].rearrange("p (c t) -> p c t", c=2),
                          r=[t_psT], wa=[t_yT[br]])

            attention("a", 0)
            for qb in range(4):
                ov, dv = out_views(qb)
                S.op(dve, lambda: nc.vector.reciprocal(out=rden[:].rearrange("p (h o) -> p h o", o=1), in_=dv),
                     r=[t_ps[qb]], w=[t_rden])
                S.op(dve, lambda: nc.vector.tensor_tensor(out=ybt[:].rearrange("p (h e) -> p h e", h=4), in0=ov,
                                                          in1=rden[:].rearrange("p (h o) -> p h o", o=1).to_broadcast([128, 4, 64]),
                                                          op=ALU.mult), r=[t_ps[qb], t_rden], w=[t_ybt])
                transpose_to_yT(ybt, t_ybt, 0, qb)
            chk('D')
            attention("c", 0)
            for qb in range(4):
                ov, dv = out_views(qb)
                S.op(dve, lambda: nc.vector.reciprocal(out=rden[:].rearrange("p (h o) -> p h o", o=1), in_=dv),
                     r=[t_ps[qb]], w=[t_rden])
                S.op(dve, lambda: nc.vector.tensor_tensor(out=o1[:, qb, :].rearrange("p (h e) -> p h e", h=4), in0=ov,
                                                          in1=rden[:].rearrange("p (h o) -> p h o", o=1).to_broadcast([128, 4, 64]),
                                                          op=ALU.mult), r=[t_ps[qb], t_rden], w=[t_o1[qb]])
            attention("c", 1)
            for qb in range(4):
                ov, dv = out_views(qb)
                S.op(dve, lambda: nc.vector.reciprocal(out=rden[:].rearrange("p (h o) -> p h o", o=1), in_=dv),
                     r=[t_ps[qb]], w=[t_rden])
                S.op(dve, lambda: nc.vector.tensor_scalar(out=rden[:], in0=rden[:], scalar1=neglam[:, 0:1], scalar2=None,
                                                          op0=ALU.mult), r=[t_par], w=[t_rden])
                S.op(dve, lambda: nc.vector.tensor_tensor(out=oo[:].rearrange("p (h e) -> p h e", h=4), in0=ov,
                                                          in1=rden[:].rearrange("p (h o) -> p h o", o=1).to_broadcast([128, 4, 64]),
                                                          op=ALU.mult), r=[t_ps[qb], t_rden], w=[t_oo])
                S.op(pool, lambda: nc.gpsimd.tensor_tensor(out=oo[:], in0=oo[:], in1=o1[:, qb, :], op=ALU.add),
                     r=[t_o1[qb]], w=[t_oo])
                S.op(pool, lambda: nc.gpsimd.tensor_tensor(out=sq[:], in0=oo[:], in1=oo[:], op=ALU.mult), r=[t_oo], w=[t_sq])
                S.op(dve, lambda: nc.vector.tensor_reduce(out=ssc[:], in_=sq[:].rearrange("p (h e) -> p h e", h=4), axis=AX.X,
                                                          op=ALU.add), r=[t_sq], w=[t_ssc])
                S.op(dve, lambda: nc.vector.tensor_scalar(out=ssc[:], in0=ssc[:], scalar1=1.0 / 64.0, scalar2=EPS,
                                                          op0=ALU.mult, op1=ALU.add), w=[t_ssc])
                S.op(pool, lambda: nc.gpsimd.tensor_tensor(out=ssc[:], in0=ssc[:], in1=neghalf[:, 0:1].to_broadcast([128, 4]),
                                                           op=ALU.pow), r=[t_cst], w=[t_ssc])
                S.op(dve, lambda: nc.vector.tensor_tensor(out=sq[:].rearrange("p (h e) -> p h e", h=4),
                                                          in0=oo[:].rearrange("p (h e) -> p h e", h=4),
                                                          in1=ssc[:].rearrange("p (h o) -> p h o", o=1).to_broadcast([128, 4, 64]),
                                                          op=ALU.mult), r=[t_oo, t_ssc], w=[t_sq])
                S.op(pool, lambda: nc.gpsimd.tensor_tensor(out=ybt[:].rearrange("p (h e) -> p h e", h=4),
                                                           in0=sq[:].rearrange("p (h e) -> p h e", h=4),
                                                           in1=gsub[:].rearrange("p (o e) -> p o e", o=1).to_broadcast([128, 4, 64]),
                                                           op=ALU.mult), r=[t_sq, t_par], w=[t_ybt])
                transpose_to_yT(ybt, t_ybt, 2, qb)

            chk('E')
            for fc in range(8):
                if fc % 4 == 0:
                    S.dma(wbr_sb[:], wbr_bf[l][:, :, (fc // 4) * 512:(fc // 4 + 1) * 512].rearrange("k p c -> p k c"),
                          r=[t_prep[l]], w=[t_wbr])
                sg = load_w(win_bf[l][:, :, FM_G + fc * 384:FM_G + (fc + 1) * 384].rearrange("k p c -> p k c"), 8, 384,
                            t_prep[l])
                for i in range(3):
                    bkg = nb(ALLB)
                    for kc in range(8):
                        S.op(pe, lambda kc=kc: nc.tensor.matmul(ps[bkg][:, 0:512], lhsT=wbuf[sg][:, kc, i * 128:(i + 1) * 128],
                                                                rhs=hT[:, kc, :], start=(kc == 0), stop=(kc == 7)),
                             r=[t_hT, t_wbuf[sg]], w=[t_ps[bkg]])
                    S.op(act, lambda: nc.scalar.activation(out=sig[i], in_=ps[bkg][:, :], func=AF.Sigmoid),
                         r=[t_ps[bkg]], w=[t_sig[i]])
                    bkz = nb(ALLB)
                    for c in range(2):
                        S.op(pe, lambda c=c: nc.tensor.matmul(ps[bkz][:, 0:512],
                                                              lhsT=wbr_sb[:, i * 2 + c, (fc % 4) * 128:(fc % 4 + 1) * 128],
                                                              rhs=yT[:, i, c, :], start=(c == 0), stop=(c == 1)),
                             r=[t_yT[i], t_wbr], w=[t_ps[bkz]])
                    di = 0 if i == 0 else 1
                    S.op(dve, lambda: nc.vector.tensor_tensor(out=tm[di], in0=ps[bkz][:, :], in1=sig[i], op=ALU.mult),
                         r=[t_ps[bkz], t_sig[i]], w=[t_tm[di]])
                    if i == 1:
                        S.op(pool, lambda: nc.gpsimd.tensor_tensor(out=tm[0], in0=tm[0], in1=tm[1], op=ALU.add),
                             r=[t_tm[1]], w=[t_tm[0]])
                    if i == 2:
                        S.op(pool, lambda: nc.gpsimd.tensor_tensor(out=mergedT[:, fc, :], in0=tm[0], in1=tm[1],
                                                                   op=ALU.add), r=[t_tm[0], t_tm[1]], wa=[t_mT])
            for half in range(2):
                s = load_w(wout_bf[l][:, :, half * 512:(half + 1) * 512].rearrange("k p c -> p k c"), 8, 512, t_prep[l])
                for b in range(4):
                    bk = nb(ALLB)
                    for kc in range(8):
                        S.op(pe, lambda kc=kc: nc.tensor.matmul(ps[bk][:, 0:512], lhsT=mergedT[:, kc, b * 128:(b + 1) * 128],
                                                                rhs=wbuf[s][:, kc, :], start=(kc == 0), stop=(kc == 7)),
                             r=[t_mT, t_wbuf[s]], w=[t_ps[bk]])
                    S.op(dve, lambda: nc.vector.tensor_tensor(out=xb[:, b, half * 512:(half + 1) * 512], in0=ps[bk][:, :],
                                                              in1=xb[:, b, half * 512:(half + 1) * 512], op=ALU.add),
                         r=[t_ps[bk]], w=[t_xb[b]])

            chk('F')
            for b in range(4):
                rmsnorm_to_hT(b)
            ri = 0
            for ffh in range(2):
                for grp in range(4):
                    cb = (ffh * 4 + grp) * 512
                    s = load_w(wff1_bf[l][:, :, cb:cb + 512].rearrange("k p c -> p k c"), 8, 512, t_prep[l])
                    for cc in range(4):
                        bk = nb((4, 5, 6))
                        for kc in range(8):
                            S.op(pe, lambda kc=kc: nc.tensor.matmul(ps[bk][:, 0:512], lhsT=wbuf[s][:, kc, cc * 128:(cc + 1) * 128],
                                                                    rhs=hT[:, kc, :], start=(kc == 0), stop=(kc == 7)),
                                 r=[t_hT, t_wbuf[s]], w=[t_ps[bk]])
                        ri ^= 1
                        S.op(act, lambda: nc.scalar.activation(out=rt[ri], in_=ps[bk][:, :], func=AF.Relu),
                             r=[t_ps[bk]], w=[t_rt[ri]])
                        S.op(pool, lambda: nc.gpsimd.tensor_tensor(out=aT[:, grp * 4 + cc, :], in0=rt[ri], in1=rt[ri],
                                                                   op=ALU.mult), r=[t_rt[ri]], wa=[t_aT])
                for half in range(2):
                    for wg in range(2):
                        k0 = ffh * 16 + wg * 8
                        s = load_w(wff2_bf[l][k0:k0 + 8, :, half * 512:(half + 1) * 512].rearrange("k p c -> p k c"), 8, 512,
                                   t_prep[l])
                        for b in range(4):
                            for c in range(8):
                                S.op(pe, lambda c=c: nc.tensor.matmul(ps[b][:, 0:512],
                                                                      lhsT=aT[:, wg * 8 + c, b * 128:(b + 1) * 128],
                                                                      rhs=wbuf[s][:, c, :], start=(wg == 0 and c == 0),
                                                                      stop=(wg == 1 and c == 7)),
                                     r=[t_aT, t_wbuf[s]], w=[t_ps[b]])
                    for b in range(4):
                        S.op(dve, lambda: nc.vector.tensor_tensor(out=xb[:, b, half * 512:(half + 1) * 512], in0=ps[b][:, :],
                                                                  in1=xb[:, b, half * 512:(half + 1) * 512], op=ALU.add),
                             r=[t_ps[b]], w=[t_xb[b]])
            chk('G')
            if last:
                S.dma(gfin, fin_g[0:1, :].partition_broadcast(128), r=[t_in], w=[t_gfin])
            for b in range(4):
                gb = 4 * j + b
                if last:
                    S.op(act, lambda: nc.scalar.activation(out=hn[:], in_=xb[:, b, :], func=AF.Square,
                                                           accum_out=ss[:, b:b + 1]), r=[t_xb[b]], w=[t_hn, t_ss[b]])
                    S.op(dve, lambda: nc.vector.tensor_scalar(out=ms[:, b:b + 1], in0=ss[:, b:b + 1], scalar1=1.0 / D,
                                                              scalar2=EPS, op0=ALU.mult, op1=ALU.add), r=[t_ss[b]], w=[t_ms[b]])
                    S.op(pool, lambda: nc.gpsimd.tensor_tensor(out=rstd[:, b:b + 1], in0=ms[:, b:b + 1], in1=neghalf[:, 0:1],
                                                               op=ALU.pow), r=[t_ms[b], t_cst], w=[t_rstd[b]])
                    S.op(dve, lambda: nc.vector.scalar_tensor_tensor(out=xb[:, b, :], in0=xb[:, b, :], scalar=rstd[:, b:b + 1],
                                                                     in1=gfin, op0=ALU.mult, op1=ALU.mult),
                         r=[t_rstd[b], t_gfin], w=[t_xb[b]])
                    S.dma(out[gb * 128:(gb + 1) * 128, :], xb[:, b, :], r=[t_xb[b]])
                else:
                    S.dma(xs[gb * 128:(gb + 1) * 128, :], xb[:, b, :], r=[t_xb[b]], wa=[t_xs[j]])

    except _Stop:
        pass
    S.barrier()
    es.close()
    return nc, S, dbg_outs


def make_consts():
    t = np.arange(128)[:, None]; s = np.arange(128)[None, :]
    ident = np.eye(128, dtype=np.float32)
    causal_big = np.where(s <= t, 0.0, -1e30).astype(np.float32)
    cmb = np.where(s <= t, 0.0, MASKV).astype(np.float32)
    tril = (s <= t).astype(np.float32)
    consts = np.concatenate([ident, causal_big, cmb, tril], axis=1).astype(np.float32)
    bits = (np.arange(SEQ) + (27 << 7)).astype(np.uint16)
    rampv = -(bits.view(ml_dtypes.bfloat16).astype(np.float32))[None, :]
    return np.ascontiguousarray(consts), np.ascontiguousarray(rampv.astype(np.float32))


_CACHE = {}


def kernel(**inputs):
    if "nc" not in _CACHE:
        _CACHE["nc"] = build_nc()[0]
    nc = _CACHE["nc"]
    consts, rampv = make_consts()
    shared = {}
    for k, v in inputs.items():
        if k == "x": continue
        a = np.ascontiguousarray(np.asarray(v, dtype=np.float32))
        if k == "final_norm_g": a = a.reshape(1, D)
        shared[k] = a
    shared["consts"] = consts; shared["rampv"] = rampv
    x = np.asarray(inputs["x"], dtype=np.float32)
    in_maps = []
    for c in range(8):
        m = dict(shared); m["x"] = np.ascontiguousarray(x[c]); in_maps.append(m)
    res = run_bass_kernel_spmd(nc, in_maps, core_ids=list(range(8)))
    return np.stack([np.asarray(res.results[c]["out"], dtype=np.float32) for c in range(8)], axis=0)
```

```python
import math, contextlib
import numpy as np
import ml_dtypes
import concourse.bass as bass
import concourse.mybir as mybir
from concourse.bass_utils import run_bass_kernel_spmd

F32 = mybir.dt.float32; BF16 = mybir.dt.bfloat16; U8 = mybir.dt.uint8
AF = mybir.ActivationFunctionType; ALU = mybir.AluOpType; AX = mybir.AxisListType

D = 1024; SEQ = 4096; NL = 2; NIN = 5444; DFF = 4096
EPS = 1e-6
NBIS = 24
RNG = 32.0
MASKV = -30000.0
FM_AQ, FM_AK, FM_IQ, FM_CQ, FM_CK, FM_G = 0, 256, 512, 768, 1024, 1280
TM_AV, TM_IK, TM_IW, TM_CV, TM_BUV = 4352, 4608, 4672, 4676, 4932


class Trk:
    __slots__ = ("w", "r", "excl")

    def __init__(self, excl=False):
        self.w = {}; self.r = {}; self.excl = excl


class Eng:
    def __init__(self, key, h, sem, inorder=False):
        self.key = key; self.h = h; self.sem = sem; self.count = 0; self.seen = {}; self.inorder = inorder


class Sync:
    def __init__(self, nc, es, ndma=24):
        self.nc = nc
        self.sems = {}

        def mk(key, h, inorder=False):
            s = es.enter_context(nc.semaphore("s_" + key))
            self.sems[key] = s
            return Eng(key, h, s, inorder)
        self.pe = mk("pe", nc.tensor, True)
        self.act = mk("act", nc.scalar)
        self.dve = mk("dve", nc.vector)
        self.pool = mk("pool", nc.gpsimd)
        self.sp = Eng("sp", nc.sync, None)
        self.dsem = []
        for k in range(ndma):
            s = es.enter_context(nc.semaphore("s_d%d" % k))
            self.sems[("d", k)] = s
            self.dsem.append([s, 0])
        self.dnext = 0
        self.ninstr = 0

    def _waits(self, eng, r, w, wa):
        waits = {}
        for t in r:
            for k, v in t.w.items():
                if v > waits.get(k, 0): waits[k] = v
        for t in w:
            for k, v in t.w.items():
                if v > waits.get(k, 0): waits[k] = v
            for k, v in t.r.items():
                if v > waits.get(k, 0): waits[k] = v
        for t in wa:
            for k, v in t.r.items():
                if v > waits.get(k, 0): waits[k] = v
        for k, v in waits.items():
            if k == eng.key and eng.inorder: continue
            if eng.seen.get(k, 0) >= v: continue
            eng.h.wait_ge(self.sems[k], v)
            eng.seen[k] = v

    def op(self, eng, fn, r=(), w=(), wa=()):
        if any(t.excl for t in r):
            w = list(w) + [t for t in r if t.excl]
            r = [t for t in r if not t.excl]
        self._waits(eng, r, w, wa)
        ins = fn()
        eng.count += 1
        ins.then_inc(eng.sem, 1)
        self.ninstr += 1
        for t in r: t.r[eng.key] = eng.count
        for t in w: t.w[eng.key] = eng.count
        for t in wa: t.w[eng.key] = eng.count
        return ins

    def dma(self, out, in_, r=(), w=(), wa=(), **kw):
        q = self.sp
        self._waits(q, r, w, wa)
        k = self.dnext; self.dnext = (self.dnext + 1) % len(self.dsem)
        s, c = self.dsem[k]
        key = ("d", k)
        if c > 0 and q.seen.get(key, 0) < c:
            q.h.wait_ge(s, c); q.seen[key] = c
        q.h.dma_start(out=out, in_=in_, **kw).then_inc(s, 16)
        c += 16
        self.dsem[k][1] = c
        self.ninstr += 1
        for t in r: t.r[key] = c
        for t in w: t.w[key] = c
        for t in wa: t.w[key] = c

    def barrier(self):
        engs = [self.pe, self.act, self.dve, self.pool, self.sp]
        for e in engs:
            for o in [self.pe, self.act, self.dve, self.pool]:
                if o is e or o.count == 0: continue
                if e.seen.get(o.key, 0) < o.count:
                    e.h.wait_ge(o.sem, o.count); e.seen[o.key] = o.count
            for k, (s, c) in enumerate(self.dsem):
                if c and e.seen.get(("d", k), 0) < c:
                    e.h.wait_ge(s, c); e.seen[("d", k)] = c


class _Stop(Exception):
    pass


def build_nc(n_layers=NL, n_tiles=8, dbg=False, stop=None):
    nc = bass.Bass("TRN2", target_bir_lowering=False, dynamic_dma_scratch_size=512)
    es = contextlib.ExitStack()
    S = Sync(nc, es)

    def din(name, shape):
        return nc.dram_tensor(name, list(shape), F32, kind="ExternalInput").ap()
    x_in = din("x", [SEQ, D])
    attn_norm_g = din("attn_norm_g", [NL, D]); w_in = din("w_in", [NL, D, NIN])
    idx_g = din("idx_k_norm_g", [NL, 64]); idx_b = din("idx_k_norm_b", [NL, 64])
    sgu_g = din("sgu_norm_g", [NL, 256]); sgu_b = din("sgu_norm_b", [NL, 256])
    sgu_w = din("sgu_w_s", [NL, 4, 128, 128]); sgu_bs = din("sgu_b_s", [NL, 4, 128])
    dlam = din("diff_lambda", [NL, 4, 32]); subln = din("diff_subln_g", [NL, 64])
    wbrs = [din("w_branch_a", [NL, 256, D]), din("w_branch_b", [NL, 256, D]), din("w_branch_c", [NL, 256, D])]
    w_out = din("w_out", [NL, D, D]); mlp_norm_g = din("mlp_norm_g", [NL, D])
    w_ff1 = din("w_ff1", [NL, D, DFF]); w_ff2 = din("w_ff2", [NL, DFF, D])
    fin_g = din("final_norm_g", [1, D])
    consts = din("consts", [128, 512]); rampv = din("rampv", [1, SEQ])
    out = nc.dram_tensor("out", [SEQ, D], F32, kind="ExternalOutput").ap()

    def dscr(name, shape, dt):
        return nc.dram_tensor(name, list(shape), dt, kind="Internal").ap()
    win_bf = [dscr("win_bf%d" % l, [8, 128, NIN], BF16) for l in range(NL)]
    wff1_bf = [dscr("wff1_bf%d" % l, [8, 128, DFF], BF16) for l in range(NL)]
    wff2_bf = [dscr("wff2_bf%d" % l, [32, 128, D], BF16) for l in range(NL)]
    wout_bf = [dscr("wout_bf%d" % l, [8, 128, D], BF16) for l in range(NL)]
    wbr_bf = [dscr("wbr_bf%d" % l, [6, 128, D], BF16) for l in range(NL)]
    xs = dscr("xs", [SEQ, D], F32)
    t_prep = [Trk() for _ in range(NL)]
    t_xs = [Trk() for _ in range(8)]
    t_in = Trk()

    def sb(name, shape, dt, stack=es):
        return stack.enter_context(nc.sbuf_tensor(name, list(shape), dt))

    pe, act, dve, pool = S.pe, S.act, S.dve, S.pool

    cst = sb("cst", [128, 512], F32); t_cst = Trk()
    ident_bf = sb("ident_bf", [128, 128], BF16)
    cmb_bf = sb("cmb_bf", [128, 128], BF16)
    neghalf = sb("neghalf", [128, 1], F32)
    gpre = sb("gpre", [128, 2 * NL * 8], F32); t_gpre = Trk()
    S.dma(cst[:], consts[:, :], r=[t_in], w=[t_cst])
    causal_big = cst[:, 128:256]
    tril = cst[:, 384:512]
    S.op(dve, lambda: nc.vector.tensor_copy(out=ident_bf[:], in_=cst[:, 0:128]), r=[t_cst], w=[t_cst])
    S.op(dve, lambda: nc.vector.tensor_copy(out=cmb_bf[:], in_=cst[:, 256:384]), r=[t_cst], w=[t_cst])
    S.op(pool, lambda: nc.gpsimd.memset(neghalf[:], -0.5), w=[t_cst])
    for l in range(NL):
        S.dma(gpre[:, l * 16:l * 16 + 8], attn_norm_g[l].rearrange("(k p) -> p k", p=128), r=[t_in], wa=[t_gpre],
              allow_slow_non_contiguous=True)
        S.dma(gpre[:, l * 16 + 8:l * 16 + 16], mlp_norm_g[l].rearrange("(k p) -> p k", p=128), r=[t_in], wa=[t_gpre],
              allow_slow_non_contiguous=True)

    with contextlib.ExitStack() as pes:
        NB = 3
        pin = [sb("pin%d" % i, [128, 1024], F32, pes) for i in range(NB)]
        pout = [sb("pout%d" % i, [128, 1024], BF16, pes) for i in range(NB)]
        t_pin = [Trk() for _ in range(NB)]; t_pout = [Trk() for _ in range(NB)]
        cnt = [0]

        def piece(l, src_ap, dst_ap, w, gcol, dst_view=None):
            i = cnt[0] % NB; cnt[0] += 1
            S.dma(pin[i][:, 0:w], src_ap, r=[t_in], w=[t_pin[i]])
            e = [dve, act][cnt[0] % 2]
            if gcol is None:
                if e is act:
                    fn = lambda: nc.scalar.copy(out=pout[i][:, 0:w], in_=pin[i][:, 0:w])
                else:
                    fn = lambda: e.h.tensor_copy(out=pout[i][:, 0:w], in_=pin[i][:, 0:w])
            else:
                g = gpre[:, gcol:gcol + 1]
                if e is act:
                    fn = lambda: nc.scalar.activation(out=pout[i][:, 0:w], in_=pin[i][:, 0:w], func=AF.Copy, scale=g)
                else:
                    fn = lambda: e.h.tensor_scalar(out=pout[i][:, 0:w], in0=pin[i][:, 0:w], scalar1=g, scalar2=None,
                                                   op0=ALU.mult)
            S.op(e, fn, r=[t_pin[i], t_gpre], w=[t_pout[i]])
            src = pout[i][:, 0:w] if dst_view is None else dst_view(pout[i])
            S.dma(dst_ap, src, r=[t_pout[i]], wa=[t_prep[l]])

        segs = [(0, FM_AQ, 512), (768, FM_IQ, 256), (1604, FM_CQ, 512), (512, TM_AV, 256), (1024, TM_IK, 68),
                (2116, TM_CV, 256), (1092, TM_BUV, 512)]
        for l in range(n_layers):
            for kc in range(8):
                rows = slice(kc * 128, (kc + 1) * 128)
                for (s0, d0, w) in segs:
                    piece(l, w_in[l, rows, s0:s0 + w], win_bf[l][kc, :, d0:d0 + w], w, l * 16 + kc)
                for i in range(3):
                    dst = win_bf[l][kc, :, FM_G:FM_G + 3072].rearrange("p (f i c) -> p f i c", f=8, i=3)[:, :, i, :]
                    piece(l, w_in[l, rows, 2372 + i * 1024:2372 + (i + 1) * 1024], dst, 1024, l * 16 + kc,
                          dst_view=lambda t: t[:, 0:1024].rearrange("p (f c) -> p f c", f=8))
                for c in range(4):
                    piece(l, w_ff1[l, rows, c * 1024:(c + 1) * 1024], wff1_bf[l][kc, :, c * 1024:(c + 1) * 1024], 1024,
                          l * 16 + 8 + kc)
                piece(l, w_out[l, rows, :], wout_bf[l][kc, :, :], 1024, None)
            for kc in range(32):
                piece(l, w_ff2[l, kc * 128:(kc + 1) * 128, :], wff2_bf[l][kc, :, :], 1024, None)
            for i in range(3):
                for c in range(2):
                    piece(l, wbrs[i][l, c * 128:(c + 1) * 128, :], wbr_bf[l][i * 2 + c, :, :], 1024, None)
        S.barrier()
    if stop == 'prep':
        S.barrier(); es.close(); return nc, S, {}

    ramp = sb("ramp", [128, SEQ], BF16); t_ramp = Trk()
    KaT = sb("KaT", [128, 2, SEQ], BF16); t_KaT = [Trk() for _ in range(8)]
    KcT = sb("KcT", [128, 2, SEQ], BF16); t_KcT = [Trk() for _ in range(8)]
    KiT = sb("KiT", [128, SEQ], BF16); t_KiT = [Trk() for _ in range(8)]
    Vae = sb("Vae", [128, 32, 260], BF16); t_Vae = [Trk() for _ in range(8)]
    Vce = sb("Vce", [128, 32, 260], BF16); t_Vce = [Trk() for _ in range(8)]
    xb = sb("xb", [128, 4, D], F32); t_xb = [Trk() for _ in range(4)]
    hT = sb("hT", [128, 8, 512], BF16); t_hT = Trk()
    QaT = sb("QaT", [128, 2, 512], BF16); t_QaT = Trk()
    QiT = sb("QiT", [128, 2, 512], BF16); t_QiT = Trk()
    QcT = sb("QcT", [128, 2, 512], BF16); t_QcT = Trk()
    acc = sb("acc", [128, SEQ], F32); t_acc = Trk()
    aT = acc[:, :].bitcast(BF16).rearrange("p (c t) -> p c t", c=16); t_aT = t_acc
    o1 = acc[:, 0:1024].rearrange("p (q e) -> p q e", q=4); t_o1 = [t_acc] * 4
    mb = sb("mb", [128, 4, SEQ], BF16); t_mb = [Trk() for _ in range(4)]
    mergedT = mb[:, 0, :].rearrange("p (k t) -> p k t", k=8); t_mT = t_mb[0]
    gfin = mb[:, 1, :].bitcast(F32)[:, 0:D]; t_gfin = t_mb[1]
    yT = sb("yT", [128, 3, 2, 512], BF16); t_yT = [Trk() for _ in range(3)]
    NW = 2
    wbuf = [sb("wbuf%d" % i, [128, 8, 512], BF16) for i in range(NW)]; t_wbuf = [Trk() for _ in range(NW)]
    wbr_sb = sb("wbr_sb", [128, 6, 512], BF16); t_wbr = Trk()
    NPT = 3
    PT = [sb("PT%d" % i, [128, 512], BF16) for i in range(NPT)]; t_PT = [Trk() for _ in range(NPT)]
    RR = sb("RR", [128, 5, 512], F32); t_RR = [Trk() for _ in range(5)]
    sig = [RR[:, i, :] for i in range(3)]; t_sig = t_RR[0:3]
    tm = [RR[:, 3 + i, :] for i in range(2)]; t_tm = t_RR[3:5]
    rt = [RR[:, i, :] for i in range(2)]; t_rt = t_RR[0:2]
    gbuf = RR[:, 2, :]; t_gbuf = t_RR[2]
    hn = sb("hn", [128, D], BF16); t_hn = Trk()
    ss = sb("ss", [128, 4], F32); t_ss = [Trk() for _ in range(4)]
    ms = sb("ms", [128, 4], F32); t_ms = [Trk() for _ in range(4)]
    rstd = sb("rstd", [128, 4], F32); t_rstd = [Trk() for _ in range(4)]
    absw = sb("absw", [128, 16], F32); sgn = sb("sgn", [128, 16], F32); wsc = sb("wsc", [128, 16], F32)
    t_iw = [Trk() for _ in range(4)]
    st6 = sb("st6", [128, 6], F32); mv = sb("mv", [128, 2], F32); lnr = sb("lnr", [128, 2], F32); t_ln = Trk()
    ln1 = sb("ln1", [128, 256], F32); ln2 = sb("ln2", [128, 256], F32); t_ln1 = Trk(); t_ln2 = Trk()
    knd = sb("knd", [128, 128], BF16); t_knd = Trk()
    vn = sb("vn", [128, 256], BF16); t_vn = Trk()
    ybt = sb("ybt", [128, 256], BF16); t_ybt = Trk()
    mid = sb("mid", [128, 1], F32); cntt = sb("cntt", [128, 1], F32); ttt = sb("ttt", [128, 1], F32)
    thr = sb("thr", [128, 1], F32); t_bis = Trk(); t_mid = Trk(); t_bisa = Trk()
    sneg = sb("sneg", [128, 1], F32)
    rden = sb("rden", [128, 4], F32); t_rden = Trk()
    sq = sb("sq", [128, 256], F32); t_sq = Trk()
    oo = sb("oo", [128, 256], F32); t_oo = Trk()
    ssc = sb("ssc", [128, 4], F32); t_ssc = Trk()
    idxg_bc = sb("idxg_bc", [128, 64], F32); idxb_bc = sb("idxb_bc", [128, 64], F32)
    sgug_bc = sb("sgug_bc", [128, 256], F32); sgub_bc = sb("sgub_bc", [128, 256], F32)
    wsf = sb("wsf", [128, 4, 128], F32); wsb = sb("wsb", [128, 4, 128], BF16); WcT = sb("WcT", [128, 4, 128], BF16)
    bs = sb("bs", [128, 4], F32)
    lamt = sb("lamt", [128, 128], F32); lamp = sb("lamp", [128, 64], F32); lam2 = sb("lam2", [128, 2], F32)
    neglam = sb("neglam", [128, 1], F32)
    gsub = sb("gsub", [128, 64], F32)
    t_par = Trk()

    ps = [es.enter_context(nc.psum_tensor("ps%d" % i, [128, 512], F32)) for i in range(7)]
    psT = es.enter_context(nc.psum_tensor("psT", [128, 1024], BF16))
    t_ps = [Trk(True) for _ in range(7)]; t_psT = Trk(True)
    rot = {"i": 0}

    def nb(lst=(4, 5, 6)):
        rot["i"] += 1
        return lst[rot["i"] % len(lst)]
    ALLB = (0, 1, 2, 3, 4, 5, 6)
    wrot = {"i": 0}

    def wslot():
        wrot["i"] += 1
        return wrot["i"] % NW
    evrot = {"i": 0}

    def evac_copy(out_ap, in_ap, r, w=(), wa=()):
        evrot["i"] += 1
        if evrot["i"] % 2:
            S.op(act, lambda: nc.scalar.copy(out=out_ap, in_=in_ap), r=r, w=w, wa=wa)
        else:
            S.op(dve, lambda: nc.vector.tensor_copy(out=out_ap, in_=in_ap), r=r, w=w, wa=wa)

    S.op(pool, lambda: nc.gpsimd.memset(Vae[:], 1.0), w=t_Vae)
    S.op(pool, lambda: nc.gpsimd.memset(Vce[:], 1.0), w=t_Vce)
    for q in range(4):
        S.dma(acc[:, q * 1024:(q + 1) * 1024], rampv[0:1, q * 1024:(q + 1) * 1024].partition_broadcast(128), r=[t_in],
              wa=[t_acc])
    S.op(dve, lambda: nc.vector.tensor_copy(out=ramp[:], in_=acc[:]), r=[t_acc], w=[t_ramp])

    dbg_outs = {}

    chkc = {}

    marks = []

    def chk(tag):
        chkc[tag] = chkc.get(tag, 0) + 1
        marks.append((tag, chkc[tag], S.pe.count, S.act.count, S.dve.count, S.pool.count))
        if stop == tag or stop == "%s:%d" % (tag, chkc[tag]):
            raise _Stop()

    def dump(name, ap, trks, shape, dt=F32):
        if not dbg: return
        d = nc.dram_tensor("dbg_" + name, list(shape), dt, kind="ExternalOutput").ap()
        S.dma(d, ap, r=trks)
        dbg_outs[name] = shape

    def rmsnorm_to_hT(b):
        S.op(act, lambda: nc.scalar.activation(out=hn[:], in_=xb[:, b, :], func=AF.Square, accum_out=ss[:, b:b + 1]),
             r=[t_xb[b]], w=[t_hn, t_ss[b]])
        S.op(dve, lambda: nc.vector.tensor_scalar(out=ms[:, b:b + 1], in0=ss[:, b:b + 1], scalar1=1.0 / D, scalar2=EPS,
                                                  op0=ALU.mult, op1=ALU.add), r=[t_ss[b]], w=[t_ms[b]])
        S.op(pool, lambda: nc.gpsimd.tensor_tensor(out=rstd[:, b:b + 1], in0=ms[:, b:b + 1], in1=neghalf[:, 0:1],
                                                   op=ALU.pow), r=[t_ms[b], t_cst], w=[t_rstd[b]])
        S.op(dve, lambda: nc.vector.tensor_scalar(out=hn[:], in0=xb[:, b, :], scalar1=rstd[:, b:b + 1], scalar2=None,
                                                  op0=ALU.mult), r=[t_xb[b], t_rstd[b]], w=[t_hn])
        for kc in range(8):
            S.op(pe, lambda kc=kc: nc.tensor.transpose(psT[:, kc * 128:(kc + 1) * 128], hn[:, kc * 128:(kc + 1) * 128],
                                                      ident_bf[:]), r=[t_hn, t_cst], w=[t_psT])
        evac_copy(hT[:, :, b * 128:(b + 1) * 128], psT[:, :].rearrange("p (k t) -> p k t", k=8), r=[t_psT], wa=[t_hT])

    def load_w(src_ap, nk, cw, prep_t):
        s = wslot()
        S.dma(wbuf[s][:, 0:nk, 0:cw], src_ap, r=[prep_t], w=[t_wbuf[s]])
        return s

    def layernorm_stats(in_ap):
        S.op(dve, lambda: nc.vector.bn_stats(out=st6[:], in_=in_ap[0]), r=in_ap[1], w=[t_ln])
        chk('LN1')
        S.op(dve, lambda: nc.vector.bn_aggr(out=mv[:], in_=st6[:]), r=[t_ln], w=[t_ln])
        chk('LN2')
        S.op(dve, lambda: nc.vector.tensor_scalar(out=lnr[:, 1:2], in0=mv[:, 1:2], scalar1=EPS, scalar2=None, op0=ALU.add),
             r=[t_ln], w=[t_ln])
        chk('LN3')
        S.op(pool, lambda: nc.gpsimd.tensor_tensor(out=lnr[:, 0:1], in0=lnr[:, 1:2], in1=neghalf[:, 0:1], op=ALU.pow),
             r=[t_ln, t_cst], w=[t_ln])

    try:
      for l in range(n_layers):
        lambda_init = 0.8 - 0.6 * math.exp(-0.3 * l)
        last = (l == n_layers - 1)
        xsrc = x_in if l == 0 else xs
        S.dma(idxg_bc[:], idx_g[l:l + 1, :].partition_broadcast(128), r=[t_in], w=[t_par])
        S.dma(idxb_bc[:], idx_b[l:l + 1, :].partition_broadcast(128), r=[t_in], wa=[t_par])
        S.dma(sgug_bc[:], sgu_g[l:l + 1, :].partition_broadcast(128), r=[t_in], wa=[t_par])
        S.dma(sgub_bc[:], sgu_b[l:l + 1, :].partition_broadcast(128), r=[t_in], wa=[t_par])
        S.dma(wsf[:], sgu_w[l].rearrange("g t s -> t g s"), r=[t_in], wa=[t_par])
        S.dma(bs[:], sgu_bs[l].rearrange("g t -> t g"), r=[t_in], wa=[t_par], allow_slow_non_contiguous=True)
        S.dma(lamt[:], dlam[l:l + 1].rearrange("o a b -> o (a b)").partition_broadcast(128), r=[t_in], wa=[t_par])
        S.dma(gsub[:], subln[l:l + 1, :].partition_broadcast(128), r=[t_in], wa=[t_par])
        for g in range(4):
            S.op(dve, lambda g=g: nc.vector.tensor_tensor(out=wsb[:, g, :], in0=wsf[:, g, :], in1=tril, op=ALU.mult),
                 r=[t_par, t_cst], w=[t_par])
        for g in range(4):
            S.op(pe, lambda g=g: nc.tensor.transpose(psT[:, g * 128:(g + 1) * 128], wsb[:, g, :], ident_bf[:]),
                 r=[t_par, t_cst], w=[t_psT])
        S.op(dve, lambda: nc.vector.tensor_copy(out=WcT[:], in_=psT[:, 0:512].rearrange("p (g t) -> p g t", g=4)),
             r=[t_psT], w=[t_par])
        lt4 = lamt[:].rearrange("p (a b) -> p a b", a=4)
        S.op(dve, lambda: nc.vector.tensor_tensor(out=lamp[:].rearrange("p (a b) -> p a b", a=2), in0=lt4[:, 0:4:2, :],
                                                  in1=lt4[:, 1:4:2, :], op=ALU.mult), r=[t_par], w=[t_par])
        S.op(dve, lambda: nc.vector.tensor_reduce(out=lam2[:], in_=lamp[:].rearrange("p (a b) -> p a b", a=2), axis=AX.X,
                                                  op=ALU.add), r=[t_par], w=[t_par])
        S.op(act, lambda: nc.scalar.activation(out=lam2[:], in_=lam2[:], func=AF.Exp), r=[t_par], w=[t_par])
        S.op(dve, lambda: nc.vector.tensor_tensor(out=neglam[:], in0=lam2[:, 1:2], in1=lam2[:, 0:1], op=ALU.subtract),
             r=[t_par], w=[t_par])
        S.op(dve, lambda: nc.vector.tensor_scalar(out=neglam[:], in0=neglam[:], scalar1=-lambda_init, scalar2=None,
                                                  op0=ALU.add), r=[t_par], w=[t_par])
        S.op(dve, lambda: nc.vector.tensor_scalar(out=gsub[:], in0=gsub[:], scalar1=(1.0 - lambda_init), scalar2=None,
                                                  op0=ALU.mult), r=[t_par], w=[t_par])

        chk('par')
        for j in range(n_tiles):
            nkb = 4 * j + 4
            for b in range(4):
                gb = 4 * j + b
                S.dma(xb[:, b, :], xsrc[gb * 128:(gb + 1) * 128, :], r=[t_in if l == 0 else t_xs[j]], w=[t_xb[b]])
            for b in range(4):
                rmsnorm_to_hT(b)

            chk('A')
            s1 = load_w(win_bf[l][:, :, TM_AV:TM_AV + 324].rearrange("k p c -> p k c"), 8, 324, t_prep[l])
            s2 = load_w(win_bf[l][:, :, TM_CV:TM_CV + 256].rearrange("k p c -> p k c"), 8, 256, t_prep[l])
            for b in range(4):
                gb = 4 * j + b
                bk = nb(ALLB)
                for kc in range(8):
                    S.op(pe, lambda kc=kc: nc.tensor.matmul(ps[bk][:, 0:324], lhsT=hT[:, kc, b * 128:(b + 1) * 128],
                                                            rhs=wbuf[s1][:, kc, 0:324], start=(kc == 0), stop=(kc == 7)),
                         r=[t_hT, t_wbuf[s1]], w=[t_ps[bk]])
                chk('B1a')
                evac_copy(Vae[:, gb, :].rearrange("p (h e) -> p h e", h=4)[:, :, 0:64],
                          ps[bk][:, 0:256].rearrange("p (h e) -> p h e", h=4), r=[t_ps[bk]], wa=[t_Vae[j]])
                chk('B1b')
                S.op(act, lambda: nc.scalar.copy(out=ln2[:, 64:128], in_=ps[bk][:, 256:320]), r=[t_ps[bk]], w=[t_ln2])
                chk('LN0')
                layernorm_stats((ln2[:, 64:128], [t_ln2]))
                chk('B1b1')
                S.op(dve, lambda: nc.vector.tensor_scalar(out=ln1[:, 0:64], in0=ln2[:, 64:128], scalar1=mv[:, 0:1],
                                                          scalar2=lnr[:, 0:1], op0=ALU.subtract, op1=ALU.mult),
                     r=[t_ln2, t_ln], w=[t_ln1])
                chk('B1b2')
                S.op(dve, lambda: nc.vector.tensor_tensor(out=ln2[:, 0:64], in0=ln1[:, 0:64], in1=idxg_bc[:], op=ALU.mult),
                     r=[t_ln1, t_par], w=[t_ln2])
                chk('B1b3')
                S.op(dve, lambda: nc.vector.tensor_tensor(out=knd[:, 0:64], in0=ln2[:, 0:64], in1=idxb_bc[:], op=ALU.add),
                     r=[t_ln2, t_par], w=[t_knd])
                S.op(dve, lambda: nc.vector.tensor_tensor(out=knd[:, 64:128], in0=ln2[:, 0:64], in1=idxb_bc[:], op=ALU.add),
                     r=[t_ln2, t_par], wa=[t_knd])
                chk('B1c0')
                S.op(pe, lambda: nc.tensor.transpose(psT[:, 0:128], knd[:], ident_bf[:]), r=[t_knd, t_cst], w=[t_psT])
                evac_copy(KiT[:, gb * 128:(gb + 1) * 128], psT[:, 0:128], r=[t_psT], wa=[t_KiT[j]])
                chk('B1c')
                S.op(dve, lambda: nc.vector.tensor_scalar(out=wsc[:, b * 4:b * 4 + 4], in0=ps[bk][:, 320:324],
                                                          scalar1=1.0 / 16.0, scalar2=None, op0=ALU.mult),
                     r=[t_ps[bk]], w=[t_iw[b]])
                S.op(dve, lambda: nc.vector.scalar_tensor_tensor(out=absw[:, b * 4:b * 4 + 4], in0=wsc[:, b * 4:b * 4 + 4],
                                                                 scalar=-1.0, in1=wsc[:, b * 4:b * 4 + 4], op0=ALU.mult,
                                                                 op1=ALU.max), r=[t_iw[b]], w=[t_iw[b]])
                S.op(dve, lambda: nc.vector.tensor_scalar(out=sgn[:, b * 4:b * 4 + 4], in0=wsc[:, b * 4:b * 4 + 4],
                                                          scalar1=0.0, scalar2=2.0, op0=ALU.is_ge, op1=ALU.mult),
                     r=[t_iw[b]], w=[t_iw[b]])
                S.op(dve, lambda: nc.vector.tensor_scalar(out=sgn[:, b * 4:b * 4 + 4], in0=sgn[:, b * 4:b * 4 + 4],
                                                          scalar1=-1.0, scalar2=None, op0=ALU.add),
                     r=[t_iw[b]], w=[t_iw[b]])
                chk('B1d')
                bk2 = nb(ALLB)
                for kc in range(8):
                    S.op(pe, lambda kc=kc: nc.tensor.matmul(ps[bk2][:, 0:256], lhsT=hT[:, kc, b * 128:(b + 1) * 128],
                                                            rhs=wbuf[s2][:, kc, 0:256], start=(kc == 0), stop=(kc == 7)),
                         r=[t_hT, t_wbuf[s2]], w=[t_ps[bk2]])
                evac_copy(Vce[:, gb, :].rearrange("p (h e) -> p h e", h=4)[:, :, 0:64],
                          ps[bk2][:, 0:256].rearrange("p (h e) -> p h e", h=4), r=[t_ps[bk2]], wa=[t_Vce[j]])
            chk('B1')
            chk('B1e')
            s3 = load_w(win_bf[l][:, :, TM_BUV:TM_BUV + 512].rearrange("k p c -> p k c"), 8, 512, t_prep[l])
            for b in range(4):
                bk = nb(ALLB)
                for kc in range(8):
                    S.op(pe, lambda kc=kc: nc.tensor.matmul(ps[bk][:, 0:512], lhsT=hT[:, kc, b * 128:(b + 1) * 128],
                                                            rhs=wbuf[s3][:, kc, 0:512], start=(kc == 0), stop=(kc == 7)),
                         r=[t_hT, t_wbuf[s3]], w=[t_ps[bk]])
                S.op(act, lambda: nc.scalar.activation(out=gbuf, in_=ps[bk][:, 0:512], func=AF.Gelu_apprx_tanh),
                     r=[t_ps[bk]], w=[t_gbuf])
                layernorm_stats((gbuf[:, 256:512], [t_gbuf]))
                S.op(dve, lambda: nc.vector.tensor_scalar(out=ln1[:], in0=gbuf[:, 256:512], scalar1=mv[:, 0:1],
                                                          scalar2=lnr[:, 0:1], op0=ALU.subtract, op1=ALU.mult),
                     r=[t_gbuf, t_ln], w=[t_ln1])
                S.op(pool, lambda: nc.gpsimd.tensor_tensor(out=ln2[:], in0=ln1[:], in1=sgug_bc[:], op=ALU.mult),
                     r=[t_ln1, t_par], w=[t_ln2])
                S.op(pool, lambda: nc.gpsimd.tensor_tensor(out=vn[:], in0=ln2[:], in1=sgub_bc[:], op=ALU.add),
                     r=[t_ln2, t_par], w=[t_vn])
                bk2 = nb(ALLB)
                for g in range(4):
                    S.op(pe, lambda g=g: nc.tensor.matmul(ps[bk2][:, g * 64:(g + 1) * 64], lhsT=WcT[:, g, :],
                                                          rhs=vn[:, g * 64:(g + 1) * 64], start=(g == 0), stop=(g == 3),
                                                          skip_group_check=True), r=[t_vn, t_par], w=[t_ps[bk2]])
                for g in range(4):
                    S.op(dve, lambda g=g: nc.vector.scalar_tensor_tensor(
                        out=ybt[:, g * 64:(g + 1) * 64], in0=ps[bk2][:, g * 64:(g + 1) * 64], scalar=bs[:, g:g + 1],
                        in1=gbuf[:, g * 64:(g + 1) * 64], op0=ALU.add, op1=ALU.mult),
                        r=[t_ps[bk2], t_par, t_gbuf], w=[t_ybt] if g == 0 else [], wa=[] if g == 0 else [t_ybt])
                for c in range(2):
                    S.op(pe, lambda c=c: nc.tensor.transpose(psT[:, c * 128:(c + 1) * 128], ybt[:, c * 128:(c + 1) * 128],
                                                            ident_bf[:]), r=[t_ybt, t_cst], w=[t_psT])
                evac_copy(yT[:, 1, :, b * 128:(b + 1) * 128], psT[:, 0:256].rearrange("p (c t) -> p c t", c=2),
                          r=[t_psT], wa=[t_yT[1]])
            chk('B2')
            fm = [(FM_AQ, 512, [("qa", 0), ("qa", 1), ("ka", 0), ("ka", 1)]),
                  (FM_IQ, 512, [("qi", 0), ("qi", 1), ("qc", 0), ("qc", 1)]),
                  (FM_CK, 256, [("kc", 0), ("kc", 1)])]
            for (c0, cw, dests) in fm:
                s = load_w(win_bf[l][:, :, c0:c0 + cw].rearrange("k p c -> p k c"), 8, cw, t_prep[l])
                for ci, (kind, c) in enumerate(dests):
                    bk = nb(ALLB)
                    for kc in range(8):
                        S.op(pe, lambda kc=kc: nc.tensor.matmul(ps[bk][:, 0:512], lhsT=wbuf[s][:, kc, ci * 128:(ci + 1) * 128],
                                                                rhs=hT[:, kc, :], start=(kc == 0), stop=(kc == 7)),
                             r=[t_hT, t_wbuf[s]], w=[t_ps[bk]])
                    if kind == "qa": evac_copy(QaT[:, c, :], ps[bk][:, :], r=[t_ps[bk]], wa=[t_QaT])
                    elif kind == "qi": evac_copy(QiT[:, c, :], ps[bk][:, :], r=[t_ps[bk]], wa=[t_QiT])
                    elif kind == "qc": evac_copy(QcT[:, c, :], ps[bk][:, :], r=[t_ps[bk]], wa=[t_QcT])
                    elif kind == "ka": evac_copy(KaT[:, c, j * 512:(j + 1) * 512], ps[bk][:, :], r=[t_ps[bk]], wa=[t_KaT[j]])
                    elif kind == "kc": evac_copy(KcT[:, c, j * 512:(j + 1) * 512], ps[bk][:, :], r=[t_ps[bk]], wa=[t_KcT[j]])

            chk('B3')
            for qb in range(4):
                gb = 4 * j + qb
                ncols = (gb + 1) * 128
                for kg in range((ncols + 511) // 512):
                    c0 = kg * 512; cw = min(512, ncols - c0)
                    for h in range(4):
                        bk = nb(ALLB)
                        rs = slice((h % 2) * 64, (h % 2) * 64 + 64)
                        S.op(pe, lambda: nc.tensor.matmul(ps[bk][:, 0:cw], lhsT=QiT[rs, h // 2, qb * 128:(qb + 1) * 128],
                                                          rhs=KiT[rs, c0:c0 + cw], start=True, stop=True),
                             r=[t_QiT] + t_KiT[0:j + 1], w=[t_ps[bk]])
                        S.op(act, lambda: nc.scalar.activation(out=ps[bk][:, 0:cw], in_=ps[bk][:, 0:cw], func=AF.Relu,
                                                               scale=absw[:, qb * 4 + h:qb * 4 + h + 1]),
                             r=[t_iw[qb]], w=[t_ps[bk]])
                        in1 = ramp[:, c0:c0 + cw] if h == 0 else acc[:, c0:c0 + cw]
                        S.op(dve, lambda: nc.vector.scalar_tensor_tensor(
                            out=acc[:, c0:c0 + cw], in0=ps[bk][:, 0:cw], scalar=sgn[:, qb * 4 + h:qb * 4 + h + 1], in1=in1,
                            op0=ALU.mult, op1=ALU.add), r=[t_ps[bk], t_iw[qb], t_ramp], w=[t_acc])
                S.op(dve, lambda: nc.vector.tensor_tensor(out=acc[:, gb * 128:(gb + 1) * 128],
                                                          in0=acc[:, gb * 128:(gb + 1) * 128], in1=causal_big, op=ALU.add),
                     r=[t_cst], w=[t_acc])
                if gb >= 2:
                    n1 = (int(ncols * 0.40) // 64) * 64; n2 = ncols - n1
                    S.op(dve, lambda: nc.vector.memset(mid[:], 0.0), w=[t_bis, t_mid])
                    for k in range(NBIS):
                        ck = RNG / (2.0 ** k)
                        S.op(dve, lambda: nc.vector.tensor_scalar(out=mb[:, qb, 0:n1], in0=acc[:, 0:n1],
                                                                  scalar1=mid[:, 0:1], scalar2=None, op0=ALU.is_ge,
                                                                  op1=ALU.add, accum_out=cntt[:]),
                             r=[t_acc, t_mid], w=[t_bis], wa=[t_mb[qb]])
                        S.op(act, lambda: nc.scalar.activation(out=mb[:, qb, n1:ncols], in_=acc[:, n1:ncols], func=AF.Sign,
                                                               scale=-1.0, bias=mid[:, 0:1], accum_out=sneg[:]),
                             r=[t_acc, t_mid], w=[t_bisa], wa=[t_mb[qb]])
                        S.op(dve, lambda: nc.vector.scalar_tensor_tensor(out=ttt[:], in0=cntt[:], scalar=2.0, in1=sneg[:],
                                                                         op0=ALU.mult, op1=ALU.subtract),
                             r=[t_bisa], w=[t_bis])
                        S.op(dve, lambda: nc.vector.tensor_scalar(out=ttt[:], in0=ttt[:], scalar1=512.0 - n2 - 0.5,
                                                                  scalar2=ck, op0=ALU.is_ge, op1=ALU.mult), w=[t_bis])
                        S.op(dve, lambda: nc.vector.scalar_tensor_tensor(out=mid[:], in0=ttt[:], scalar=-ck / 2.0,
                                                                         in1=mid[:], op0=ALU.add, op1=ALU.add),
                             w=[t_bis, t_mid])
                    cK = RNG / (2.0 ** NBIS)
                    S.op(dve, lambda: nc.vector.tensor_scalar(out=thr[:], in0=mid[:], scalar1=-cK, scalar2=None,
                                                              op0=ALU.add), w=[t_bis])
                else:
                    S.op(dve, lambda: nc.vector.memset(thr[:], -RNG), w=[t_bis])
                S.op(dve, lambda: nc.vector.tensor_scalar(out=mb[:, qb, 0:ncols], in0=acc[:, 0:ncols], scalar1=thr[:, 0:1],
                                                          scalar2=MASKV, op0=ALU.is_lt, op1=ALU.mult),
                     r=[t_acc, t_bis], w=[t_mb[qb]])

            chk('C')
            def attention(kind, comp):
                if kind == "a":
                    KT, QT, VE, tK, tQ, tV = KaT, QaT, Vae, t_KaT, t_QaT, t_Vae
                    scale = 64 ** -0.5
                else:
                    KT, QT, VE, tK, tQ, tV = KcT, QcT, Vce, t_KcT, t_QcT, t_Vce
                    scale = 32 ** -0.5
                steps = [(h, kb) for h in range(4) for kb in range(nkb)]
                banks = {}
                LA = 2

                def qk(i):
                    h, kb = steps[i]
                    r0 = max(kb - 4 * j, 0); c0 = r0 * 128
                    bk = nb((4, 5, 6)); banks[i] = bk
                    if kind == "a":
                        rs = slice((h % 2) * 64, (h % 2) * 64 + 64); ch = h // 2; tp = None
                    else:
                        idx = h * 2 + comp
                        rs = slice((idx % 4) * 32, (idx % 4) * 32 + 32); ch = idx // 4; tp = ((idx % 4) * 32, 0)
                    kw = {} if tp is None else {"tile_position": tp}
                    S.op(pe, lambda: nc.tensor.matmul(ps[bk][:, c0:512], lhsT=KT[rs, ch, kb * 128:(kb + 1) * 128],
                                                      rhs=QT[rs, ch, c0:512], start=True, stop=False, **kw),
                         r=[tQ, tK[kb // 4]], w=[t_ps[bk]])
                    if kind == "a":
                        for qb in range(r0, 4):
                            S.op(pe, lambda qb=qb: nc.tensor.matmul(ps[bk][:, qb * 128:(qb + 1) * 128],
                                                                    lhsT=mb[:, qb, kb * 128:(kb + 1) * 128], rhs=ident_bf[:],
                                                                    start=False, stop=(qb == 3), skip_group_check=True),
                                 r=[t_mb[qb], t_cst], w=[t_ps[bk]])
                    else:
                        if kb >= 4 * j:
                            S.op(pe, lambda: nc.tensor.matmul(ps[bk][:, r0 * 128:(r0 + 1) * 128], lhsT=cmb_bf[:],
                                                              rhs=ident_bf[:], start=False, stop=True,
                                                              skip_group_check=True), r=[t_cst], w=[t_ps[bk]])
                    pi = i % NPT
                    S.op(act, lambda: nc.scalar.activation(out=PT[pi][:, c0:512], in_=ps[bk][:, c0:512], func=AF.Exp,
                                                           scale=scale), r=[t_ps[bk]], w=[t_PT[pi]])

                def pv(i):
                    h, kb = steps[i]
                    r0 = max(kb - 4 * j, 0)
                    pi = i % NPT
                    for qb in range(r0, 4):
                        S.op(pe, lambda qb=qb: nc.tensor.matmul(ps[qb][:, h * 65:(h + 1) * 65],
                                                                lhsT=PT[pi][:, qb * 128:(qb + 1) * 128],
                                                                rhs=VE[:, kb, h * 65:(h + 1) * 65],
                                                                start=(h == 0 and kb == 0), stop=(kb == 4 * j + qb),
                                                                skip_group_check=True),
                             r=[t_PT[pi], tV[kb // 4]], w=[t_ps[qb]])
                n = len(steps)
                for i in range(min(LA, n)): qk(i)
                for i in range(n):
                    if i + LA < n: qk(i + LA)
                    pv(i)

            def out_views(qb):
                o4 = ps[qb][:, 0:260].rearrange("p (h e) -> p h e", h=4)
                return o4[:, :, 0:64], o4[:, :, 64:65]

            def transpose_to_yT(src, t_src, br, qb):
                for c in range(2):
                    S.op(pe, lambda c=c: nc.tensor.transpose(psT[:, c * 128:(c + 1) * 128], src[:, c * 128:(c + 1) * 128],
                                                            ident_bf[:]), r=[t_src, t_cst], w=[t_psT])
                evac_copy(yT[:, br, :, qb * 128:(qb + 1) * 128], psT[:, 0:256].rearrange("p (c t) -> p c t", c=2),
                          r=[t_psT], wa=[t_yT[br]])

            attention("a", 0)
            for qb in range(4):
                ov, dv = out_views(qb)
                S.op(dve, lambda: nc.vector.reciprocal(out=rden[:].rearrange("p (h o) -> p h o", o=1), in_=dv),
                     r=[t_ps[qb]], w=[t_rden])
                S.op(dve, lambda: nc.vector.tensor_tensor(out=ybt[:].rearrange("p (h e) -> p h e", h=4), in0=ov,
                                                          in1=rden[:].rearrange("p (h o) -> p h o", o=1).to_broadcast([128, 4, 64]),
                                                          op=ALU.mult), r=[t_ps[qb], t_rden], w=[t_ybt])
                transpose_to_yT(ybt, t_ybt, 0, qb)
            chk('D')
            attention("c", 0)
            for qb in range(4):
                ov, dv = out_views(qb)
                S.op(dve, lambda: nc.vector.reciprocal(out=rden[:].rearrange("p (h o) -> p h o", o=1), in_=dv),
                     r=[t_ps[qb]], w=[t_rden])
                S.op(dve, lambda: nc.vector.tensor_tensor(out=o1[:, qb, :].rearrange("p (h e) -> p h e", h=4), in0=ov,
                                                          in1=rden[:].rearrange("p (h o) -> p h o", o=1).to_broadcast([128, 4, 64]),
                                                          op=ALU.mult), r=[t_ps[qb], t_rden], w=[t_o1[qb]])
            attention("c", 1)
            for qb in range(4):
                ov, dv = out_views(qb)
                S.op(dve, lambda: nc.vector.reciprocal(out=rden[:].rearrange("p (h o) -> p h o", o=1), in_=dv),
                     r=[t_ps[qb]], w=[t_rden])
                S.op(dve, lambda: nc.vector.tensor_scalar(out=rden[:], in0=rden[:], scalar1=neglam[:, 0:1], scalar2=None,
                                                          op0=ALU.mult), r=[t_par], w=[t_rden])
                S.op(dve, lambda: nc.vector.tensor_tensor(out=oo[:].rearrange("p (h e) -> p h e", h=4), in0=ov,
                                                          in1=rden[:].rearrange("p (h o) -> p h o", o=1).to_broadcast([128, 4, 64]),
                                                          op=ALU.mult), r=[t_ps[qb], t_rden], w=[t_oo])
                S.op(pool, lambda: nc.gpsimd.tensor_tensor(out=oo[:], in0=oo[:], in1=o1[:, qb, :], op=ALU.add),
                     r=[t_o1[qb]], w=[t_oo])
                S.op(pool, lambda: nc.gpsimd.tensor_tensor(out=sq[:], in0=oo[:], in1=oo[:], op=ALU.mult), r=[t_oo], w=[t_sq])
                S.op(dve, lambda: nc.vector.tensor_reduce(out=ssc[:], in_=sq[:].rearrange("p (h e) -> p h e", h=4), axis=AX.X,
                                                          op=ALU.add), r=[t_sq], w=[t_ssc])
                S.op(dve, lambda: nc.vector.tensor_scalar(out=ssc[:], in0=ssc[:], scalar1=1.0 / 64.0, scalar2=EPS,
                                                          op0=ALU.mult, op1=ALU.add), w=[t_ssc])
                S.op(pool, lambda: nc.gpsimd.tensor_tensor(out=ssc[:], in0=ssc[:], in1=neghalf[:, 0:1].to_broadcast([128, 4]),
                                                           op=ALU.pow), r=[t_cst], w=[t_ssc])
                S.op(dve, lambda: nc.vector.tensor_tensor(out=sq[:].rearrange("p (h e) -> p h e", h=4),
                                                          in0=oo[:].rearrange("p (h e) -> p h e", h=4),
                                                          in1=ssc[:].rearrange("p (h o) -> p h o", o=1).to_broadcast([128, 4, 64]),
                                                          op=ALU.mult), r=[t_oo, t_ssc], w=[t_sq])
                S.op(pool, lambda: nc.gpsimd.tensor_tensor(out=ybt[:].rearrange("p (h e) -> p h e", h=4),
                                                           in0=sq[:].rearrange("p (h e) -> p h e", h=4),
                                                           in1=gsub[:].rearrange("p (o e) -> p o e", o=1).to_broadcast([128, 4, 64]),
                                                           op=ALU.mult), r=[t_sq, t_par], w=[t_ybt])
                transpose_to_yT(ybt, t_ybt, 2, qb)

            chk('E')
            for fc in range(8):
                if fc % 4 == 0:
                    S.dma(wbr_sb[:], wbr_bf[l][:, :, (fc // 4) * 512:(fc // 4 + 1) * 512].rearrange("k p c -> p k c"),
                          r=[t_prep[l]], w=[t_wbr])
                sg = load_w(win_bf[l][:, :, FM_G + fc * 384:FM_G + (fc + 1) * 384].rearrange("k p c -> p k c"), 8, 384,
                            t_prep[l])
                for i in range(3):
                    bkg = nb(ALLB)
                    for kc in range(8):
                        S.op(pe, lambda kc=kc: nc.tensor.matmul(ps[bkg][:, 0:512], lhsT=wbuf[sg][:, kc, i * 128:(i + 1) * 128],
                                                                rhs=hT[:, kc, :], start=(kc == 0), stop=(kc == 7)),
                             r=[t_hT, t_wbuf[sg]], w=[t_ps[bkg]])
                    S.op(act, lambda: nc.scalar.activation(out=sig[i], in_=ps[bkg][:, :], func=AF.Sigmoid),
                         r=[t_ps[bkg]], w=[t_sig[i]])
                    bkz = nb(ALLB)
                    for c in range(2):
                        S.op(pe, lambda c=c: nc.tensor.matmul(ps[bkz][:, 0:512],
                                                              lhsT=wbr_sb[:, i * 2 + c, (fc % 4) * 128:(fc % 4 + 1) * 128],
                                                              rhs=yT[:, i, c, :], start=(c == 0), stop=(c == 1)),
                             r=[t_yT[i], t_wbr], w=[t_ps[bkz]])
                    di = 0 if i == 0 else 1
                    S.op(dve, lambda: nc.vector.tensor_tensor(out=tm[di], in0=ps[bkz][:, :], in1=sig[i], op=ALU.mult),
                         r=[t_ps[bkz], t_sig[i]], w=[t_tm[di]])
                    if i == 1:
                        S.op(pool, lambda: nc.gpsimd.tensor_tensor(out=tm[0], in0=tm[0], in1=tm[1], op=ALU.add),
                             r=[t_tm[1]], w=[t_tm[0]])
                    if i == 2:
                        S.op(pool, lambda: nc.gpsimd.tensor_tensor(out=mergedT[:, fc, :], in0=tm[0], in1=tm[1],
                                                                   op=ALU.add), r=[t_tm[0], t_tm[1]], wa=[t_mT])
            for half in range(2):
                s = load_w(wout_bf[l][:, :, half * 512:(half + 1) * 512].rearrange("k p c -> p k c"), 8, 512, t_prep[l])
                for b in range(4):
                    bk = nb(ALLB)
                    for kc in range(8):
                        S.op(pe, lambda kc=kc: nc.tensor.matmul(ps[bk][:, 0:512], lhsT=mergedT[:, kc, b * 128:(b + 1) * 128],
                                                                rhs=wbuf[s][:, kc, :], start=(kc == 0), stop=(kc == 7)),
                             r=[t_mT, t_wbuf[s]], w=[t_ps[bk]])
                    S.op(dve, lambda: nc.vector.tensor_tensor(out=xb[:, b, half * 512:(half + 1) * 512], in0=ps[bk][:, :],
                                                              in1=xb[:, b, half * 512:(half + 1) * 512], op=ALU.add),
                         r=[t_ps[bk]], w=[t_xb[b]])

            chk('F')
            for b in range(4):
                rmsnorm_to_hT(b)
            ri = 0
            for ffh in range(2):
                for grp in range(4):
                    cb = (ffh * 4 + grp) * 512
                    s = load_w(wff1_bf[l][:, :, cb:cb + 512].rearrange("k p c -> p k c"), 8, 512, t_prep[l])
                    for cc in range(4):
                        bk = nb((4, 5, 6))
                        for kc in range(8):
                            S.op(pe, lambda kc=kc: nc.tensor.matmul(ps[bk][:, 0:512], lhsT=wbuf[s][:, kc, cc * 128:(cc + 1) * 128],
                                                                    rhs=hT[:, kc, :], start=(kc == 0), stop=(kc == 7)),
                                 r=[t_hT, t_wbuf[s]], w=[t_ps[bk]])
                        ri ^= 1
                        S.op(act, lambda: nc.scalar.activation(out=rt[ri], in_=ps[bk][:, :], func=AF.Relu),
                             r=[t_ps[bk]], w=[t_rt[ri]])
                        S.op(pool, lambda: nc.gpsimd.tensor_tensor(out=aT[:, grp * 4 + cc, :], in0=rt[ri], in1=rt[ri],
                                                                   op=ALU.mult), r=[t_rt[ri]], wa=[t_aT])
                for half in range(2):
                    for wg in range(2):
                        k0 = ffh * 16 + wg * 8
                        s = load_w(wff2_bf[l][k0:k0 + 8, :, half * 512:(half + 1) * 512].rearrange("k p c -> p k c"), 8, 512,
                                   t_prep[l])
                        for b in range(4):
                            for c in range(8):
                                S.op(pe, lambda c=c: nc.tensor.matmul(ps[b][:, 0:512],
                                                                      lhsT=aT[:, wg * 8 + c, b * 128:(b + 1) * 128],
                                                                      rhs=wbuf[s][:, c, :], start=(wg == 0 and c == 0),
                                                                      stop=(wg == 1 and c == 7)),
                                     r=[t_aT, t_wbuf[s]], w=[t_ps[b]])
                    for b in range(4):
                        S.op(dve, lambda: nc.vector.tensor_tensor(out=xb[:, b, half * 512:(half + 1) * 512], in0=ps[b][:, :],
                                                                  in1=xb[:, b, half * 512:(half + 1) * 512], op=ALU.add),
                             r=[t_ps[b]], w=[t_xb[b]])
            chk('G')
            if last:
                S.dma(gfin, fin_g[0:1, :].partition_broadcast(128), r=[t_in], w=[t_gfin])
            for b in range(4):
                gb = 4 * j + b
                if last:
                    S.op(act, lambda: nc.scalar.activation(out=hn[:], in_=xb[:, b, :], func=AF.Square,
                                                           accum_out=ss[:, b:b + 1]), r=[t_xb[b]], w=[t_hn, t_ss[b]])
                    S.op(dve, lambda: nc.vector.tensor_scalar(out=ms[:, b:b + 1], in0=ss[:, b:b + 1], scalar1=1.0 / D,
                                                              scalar2=EPS, op0=ALU.mult, op1=ALU.add), r=[t_ss[b]], w=[t_ms[b]])
                    S.op(pool, lambda: nc.gpsimd.tensor_tensor(out=rstd[:, b:b + 1], in0=ms[:, b:b + 1], in1=neghalf[:, 0:1],
                                                               op=ALU.pow), r=[t_ms[b], t_cst], w=[t_rstd[b]])
                    S.op(dve, lambda: nc.vector.scalar_tensor_tensor(out=xb[:, b, :], in0=xb[:, b, :], scalar=rstd[:, b:b + 1],
                                                                     in1=gfin, op0=ALU.mult, op1=ALU.mult),
                         r=[t_rstd[b], t_gfin], w=[t_xb[b]])
                    S.dma(out[gb * 128:(gb + 1) * 128, :], xb[:, b, :], r=[t_xb[b]])
                else:
                    S.dma(xs[gb * 128:(gb + 1) * 128, :], xb[:, b, :], r=[t_xb[b]], wa=[t_xs[j]])

    except _Stop:
        pass
    S.barrier()
    es.close()
    S.marks = marks
    return nc, S, dbg_outs


def make_consts():
    t = np.arange(128)[:, None]; s = np.arange(128)[None, :]
    ident = np.eye(128, dtype=np.float32)
    causal_big = np.where(s <= t, 0.0, -1e30).astype(np.float32)
    cmb = np.where(s <= t, 0.0, MASKV).astype(np.float32)
    tril = (s <= t).astype(np.float32)
    consts = np.concatenate([ident, causal_big, cmb, tril], axis=1).astype(np.float32)
    bits = (np.arange(SEQ) + (27 << 7)).astype(np.uint16)
    rampv = -(bits.view(ml_dtypes.bfloat16).astype(np.float32))[None, :]
    return np.ascontiguousarray(consts), np.ascontiguousarray(rampv.astype(np.float32))


_CACHE = {}


def kernel(**inputs):
    if "nc" not in _CACHE:
        _CACHE["nc"] = build_nc()[0]
    nc = _CACHE["nc"]
    consts, rampv = make_consts()
    shared = {}
    for k, v in inputs.items():
        if k == "x": continue
        a = np.ascontiguousarray(np.asarray(v, dtype=np.float32))
        if k == "final_norm_g": a = a.reshape(1, D)
        shared[k] = a
    shared["consts"] = consts; shared["rampv"] = rampv
    x = np.asarray(inputs["x"], dtype=np.float32)
    in_maps = []
    for c in range(8):
        m = dict(shared); m["x"] = np.ascontiguousarray(x[c]); in_maps.append(m)
    res = run_bass_kernel_spmd(nc, in_maps, core_ids=list(range(8)))
    return np.stack([np.asarray(res.results[c]["out"], dtype=np.float32) for c in range(8)], axis=0)
```

```python
import math, contextlib
import numpy as np
import ml_dtypes
import concourse.bass as bass
import concourse.mybir as mybir
from concourse.bass_utils import run_bass_kernel_spmd

F32 = mybir.dt.float32; BF16 = mybir.dt.bfloat16; U8 = mybir.dt.uint8
AF = mybir.ActivationFunctionType; ALU = mybir.AluOpType; AX = mybir.AxisListType

D = 1024; SEQ = 4096; NL = 2; NIN = 5444; DFF = 4096
EPS = 1e-6
NBIS = 24
RNG = 32.0
MASKV = -30000.0
FM_AQ, FM_AK, FM_IQ, FM_CQ, FM_CK, FM_G = 0, 256, 512, 768, 1024, 1280
TM_AV, TM_IK, TM_IW, TM_CV, TM_BUV = 4352, 4608, 4672, 4676, 4932


class Trk:
    __slots__ = ("w", "r", "excl")

    def __init__(self, excl=False):
        self.w = {}; self.r = {}; self.excl = excl


class Eng:
    def __init__(self, key, h, sem, inorder=False):
        self.key = key; self.h = h; self.sem = sem; self.count = 0; self.seen = {}; self.inorder = inorder


class Sync:
    def __init__(self, nc, es, ndma=24):
        self.nc = nc
        self.sems = {}

        def mk(key, h, inorder=False):
            s = es.enter_context(nc.semaphore("s_" + key))
            self.sems[key] = s
            return Eng(key, h, s, inorder)
        self.pe = mk("pe", nc.tensor, True)
        self.act = mk("act", nc.scalar)
        self.dve = mk("dve", nc.vector)
        self.pool = mk("pool", nc.gpsimd)
        self.sp = Eng("sp", nc.sync, None)
        self.dsem = []
        for k in range(ndma):
            s = es.enter_context(nc.semaphore("s_d%d" % k))
            self.sems[("d", k)] = s
            self.dsem.append([s, 0])
        self.dnext = 0
        self.ninstr = 0

    def _waits(self, eng, r, w, wa):
        waits = {}
        for t in r:
            for k, v in t.w.items():
                if v > waits.get(k, 0): waits[k] = v
        for t in w:
            for k, v in t.w.items():
                if v > waits.get(k, 0): waits[k] = v
            for k, v in t.r.items():
                if v > waits.get(k, 0): waits[k] = v
        for t in wa:
            for k, v in t.r.items():
                if v > waits.get(k, 0): waits[k] = v
        for k, v in waits.items():
            if k == eng.key and eng.inorder: continue
            if eng.seen.get(k, 0) >= v: continue
            eng.h.wait_ge(self.sems[k], v)
            eng.seen[k] = v

    def op(self, eng, fn, r=(), w=(), wa=()):
        if any(t.excl for t in r):
            w = list(w) + [t for t in r if t.excl]
            r = [t for t in r if not t.excl]
        self._waits(eng, r, w, wa)
        ins = fn()
        eng.count += 1
        ins.then_inc(eng.sem, 1)
        self.ninstr += 1
        for t in r: t.r[eng.key] = eng.count
        for t in w: t.w[eng.key] = eng.count
        for t in wa: t.w[eng.key] = eng.count
        return ins

    def dma(self, out, in_, r=(), w=(), wa=(), **kw):
        q = self.sp
        self._waits(q, r, w, wa)
        k = self.dnext; self.dnext = (self.dnext + 1) % len(self.dsem)
        s, c = self.dsem[k]
        key = ("d", k)
        if c > 0 and q.seen.get(key, 0) < c:
            q.h.wait_ge(s, c); q.seen[key] = c
        q.h.dma_start(out=out, in_=in_, **kw).then_inc(s, 16)
        c += 16
        self.dsem[k][1] = c
        self.ninstr += 1
        for t in r: t.r[key] = c
        for t in w: t.w[key] = c
        for t in wa: t.w[key] = c

    def barrier(self):
        engs = [self.pe, self.act, self.dve, self.pool, self.sp]
        for e in engs:
            for o in [self.pe, self.act, self.dve, self.pool]:
                if o is e or o.count == 0: continue
                if e.seen.get(o.key, 0) < o.count:
                    e.h.wait_ge(o.sem, o.count); e.seen[o.key] = o.count
            for k, (s, c) in enumerate(self.dsem):
                if c and e.seen.get(("d", k), 0) < c:
                    e.h.wait_ge(s, c); e.seen[("d", k)] = c


class _Stop(Exception):
    pass


def build_nc(n_layers=NL, n_tiles=8, dbg=False, stop=None):
    nc = bass.Bass("TRN2", target_bir_lowering=False, dynamic_dma_scratch_size=512)
    es = contextlib.ExitStack()
    S = Sync(nc, es)

    def din(name, shape):
        return nc.dram_tensor(name, list(shape), F32, kind="ExternalInput").ap()
    x_in = din("x", [SEQ, D])
    attn_norm_g = din("attn_norm_g", [NL, D]); w_in = din("w_in", [NL, D, NIN])
    idx_g = din("idx_k_norm_g", [NL, 64]); idx_b = din("idx_k_norm_b", [NL, 64])
    sgu_g = din("sgu_norm_g", [NL, 256]); sgu_b = din("sgu_norm_b", [NL, 256])
    sgu_w = din("sgu_w_s", [NL, 4, 128, 128]); sgu_bs = din("sgu_b_s", [NL, 4, 128])
    dlam = din("diff_lambda", [NL, 4, 32]); subln = din("diff_subln_g", [NL, 64])
    wbrs = [din("w_branch_a", [NL, 256, D]), din("w_branch_b", [NL, 256, D]), din("w_branch_c", [NL, 256, D])]
    w_out = din("w_out", [NL, D, D]); mlp_norm_g = din("mlp_norm_g", [NL, D])
    w_ff1 = din("w_ff1", [NL, D, DFF]); w_ff2 = din("w_ff2", [NL, DFF, D])
    fin_g = din("final_norm_g", [1, D])
    consts = din("consts", [128, 512]); rampv = din("rampv", [1, SEQ])
    out = nc.dram_tensor("out", [SEQ, D], F32, kind="ExternalOutput").ap()

    def dscr(name, shape, dt):
        return nc.dram_tensor(name, list(shape), dt, kind="Internal").ap()
    win_bf = [dscr("win_bf%d" % l, [8, 128, NIN], BF16) for l in range(NL)]
    wff1_bf = [dscr("wff1_bf%d" % l, [8, 128, DFF], BF16) for l in range(NL)]
    wff2_bf = [dscr("wff2_bf%d" % l, [32, 128, D], BF16) for l in range(NL)]
    wout_bf = [dscr("wout_bf%d" % l, [8, 128, D], BF16) for l in range(NL)]
    wbr_bf = [dscr("wbr_bf%d" % l, [6, 128, D], BF16) for l in range(NL)]
    xs = dscr("xs", [SEQ, D], F32)
    t_prep = [Trk() for _ in range(NL)]
    t_xs = [Trk() for _ in range(8)]
    t_in = Trk()

    def sb(name, shape, dt, stack=es):
        return stack.enter_context(nc.sbuf_tensor(name, list(shape), dt))

    pe, act, dve, pool = S.pe, S.act, S.dve, S.pool

    cst = sb("cst", [128, 512], F32); t_cst = Trk()
    ident_bf = sb("ident_bf", [128, 128], BF16)
    cmb_bf = sb("cmb_bf", [128, 128], BF16)
    neghalf = sb("neghalf", [128, 1], F32)
    gpre = sb("gpre", [128, 2 * NL * 8], F32); t_gpre = Trk()
    S.dma(cst[:], consts[:, :], r=[t_in], w=[t_cst])
    causal_big = cst[:, 128:256]
    tril = cst[:, 384:512]
    S.op(dve, lambda: nc.vector.tensor_copy(out=ident_bf[:], in_=cst[:, 0:128]), r=[t_cst], w=[t_cst])
    S.op(dve, lambda: nc.vector.tensor_copy(out=cmb_bf[:], in_=cst[:, 256:384]), r=[t_cst], w=[t_cst])
    S.op(pool, lambda: nc.gpsimd.memset(neghalf[:], -0.5), w=[t_cst])
    for l in range(NL):
        S.dma(gpre[:, l * 16:l * 16 + 8], attn_norm_g[l].rearrange("(k p) -> p k", p=128), r=[t_in], wa=[t_gpre],
              allow_slow_non_contiguous=True)
        S.dma(gpre[:, l * 16 + 8:l * 16 + 16], mlp_norm_g[l].rearrange("(k p) -> p k", p=128), r=[t_in], wa=[t_gpre],
              allow_slow_non_contiguous=True)

    with contextlib.ExitStack() as pes:
        NB = 3
        pin = [sb("pin%d" % i, [128, 1024], F32, pes) for i in range(NB)]
        pout = [sb("pout%d" % i, [128, 1024], BF16, pes) for i in range(NB)]
        t_pin = [Trk() for _ in range(NB)]; t_pout = [Trk() for _ in range(NB)]
        cnt = [0]

        def piece(l, src_ap, dst_ap, w, gcol, dst_view=None):
            i = cnt[0] % NB; cnt[0] += 1
            S.dma(pin[i][:, 0:w], src_ap, r=[t_in], w=[t_pin[i]])
            e = [dve, act][cnt[0] % 2]
            if gcol is None:
                if e is act:
                    fn = lambda: nc.scalar.copy(out=pout[i][:, 0:w], in_=pin[i][:, 0:w])
                else:
                    fn = lambda: e.h.tensor_copy(out=pout[i][:, 0:w], in_=pin[i][:, 0:w])
            else:
                g = gpre[:, gcol:gcol + 1]
                if e is act:
                    fn = lambda: nc.scalar.activation(out=pout[i][:, 0:w], in_=pin[i][:, 0:w], func=AF.Copy, scale=g)
                else:
                    fn = lambda: e.h.tensor_scalar(out=pout[i][:, 0:w], in0=pin[i][:, 0:w], scalar1=g, scalar2=None,
                                                   op0=ALU.mult)
            S.op(e, fn, r=[t_pin[i], t_gpre], w=[t_pout[i]])
            src = pout[i][:, 0:w] if dst_view is None else dst_view(pout[i])
            S.dma(dst_ap, src, r=[t_pout[i]], wa=[t_prep[l]])

        segs = [(0, FM_AQ, 512), (768, FM_IQ, 256), (1604, FM_CQ, 512), (512, TM_AV, 256), (1024, TM_IK, 68),
                (2116, TM_CV, 256), (1092, TM_BUV, 512)]
        for l in range(n_layers):
            for kc in range(8):
                rows = slice(kc * 128, (kc + 1) * 128)
                for (s0, d0, w) in segs:
                    piece(l, w_in[l, rows, s0:s0 + w], win_bf[l][kc, :, d0:d0 + w], w, l * 16 + kc)
                for i in range(3):
                    dst = win_bf[l][kc, :, FM_G:FM_G + 3072].rearrange("p (f i c) -> p f i c", f=8, i=3)[:, :, i, :]
                    piece(l, w_in[l, rows, 2372 + i * 1024:2372 + (i + 1) * 1024], dst, 1024, l * 16 + kc,
                          dst_view=lambda t: t[:, 0:1024].rearrange("p (f c) -> p f c", f=8))
                for c in range(4):
                    piece(l, w_ff1[l, rows, c * 1024:(c + 1) * 1024], wff1_bf[l][kc, :, c * 1024:(c + 1) * 1024], 1024,
                          l * 16 + 8 + kc)
                piece(l, w_out[l, rows, :], wout_bf[l][kc, :, :], 1024, None)
            for kc in range(32):
                piece(l, w_ff2[l, kc * 128:(kc + 1) * 128, :], wff2_bf[l][kc, :, :], 1024, None)
            for i in range(3):
                for c in range(2):
                    piece(l, wbrs[i][l, c * 128:(c + 1) * 128, :], wbr_bf[l][i * 2 + c, :, :], 1024, None)
        S.barrier()
    if stop == 'prep':
        S.barrier(); es.close(); return nc, S, {}

    ramp = sb("ramp", [128, SEQ], BF16); t_ramp = Trk()
    KaT = sb("KaT", [128, 2, SEQ], BF16); t_KaT = [Trk() for _ in range(8)]
    KcT = sb("KcT", [128, 2, SEQ], BF16); t_KcT = [Trk() for _ in range(8)]
    KiT = sb("KiT", [128, SEQ], BF16); t_KiT = [Trk() for _ in range(8)]
    Vae = sb("Vae", [128, 32, 260], BF16); t_Vae = [Trk() for _ in range(8)]
    Vce = sb("Vce", [128, 32, 260], BF16); t_Vce = [Trk() for _ in range(8)]
    xb = sb("xb", [128, 4, D], F32); t_xb = [Trk() for _ in range(4)]
    hT = sb("hT", [128, 8, 512], BF16); t_hT = Trk()
    QaT = sb("QaT", [128, 2, 512], BF16); t_QaT = Trk()
    QiT = sb("QiT", [128, 2, 512], BF16); t_QiT = Trk()
    QcT = sb("QcT", [128, 2, 512], BF16); t_QcT = Trk()
    acc = sb("acc", [128, SEQ], F32); t_acc = Trk()
    aT = acc[:, :].bitcast(BF16).rearrange("p (c t) -> p c t", c=16); t_aT = t_acc
    mb = sb("mb", [128, 4, SEQ], BF16); t_mb = [Trk() for _ in range(4)]
    mergedT = mb[:, 0, :].rearrange("p (k t) -> p k t", k=8); t_mT = t_mb[0]
    gfin = mb[:, 1, :].bitcast(F32)[:, 0:D]; t_gfin = t_mb[1]
    yT = sb("yT", [128, 3, 2, 512], BF16); t_yT = [Trk() for _ in range(3)]
    NW = 2
    wbuf = [sb("wbuf%d" % i, [128, 8, 512], BF16) for i in range(NW)]; t_wbuf = [Trk() for _ in range(NW)]
    wbr_sb = sb("wbr_sb", [128, 6, 512], BF16); t_wbr = Trk()
    NPT = 3
    PT = [sb("PT%d" % i, [128, 512], BF16) for i in range(NPT)]; t_PT = [Trk() for _ in range(NPT)]
    RR = sb("RR", [128, 5, 512], F32); t_RR = [Trk() for _ in range(5)]
    sig = [RR[:, i, :] for i in range(3)]; t_sig = t_RR[0:3]
    tm = [RR[:, 3 + i, :] for i in range(2)]; t_tm = t_RR[3:5]
    rt = [RR[:, i, :] for i in range(2)]; t_rt = t_RR[0:2]
    gbuf = RR[:, 2, :]; t_gbuf = t_RR[2]
    o1 = RR[:, 0:2, :].rearrange("p a (b e) -> p (a b) e", b=2); t_o1 = [t_RR[0], t_RR[0], t_RR[1], t_RR[1]]
    hn = sb("hn", [128, D], BF16); t_hn = Trk()
    ss = sb("ss", [128, 4], F32); t_ss = [Trk() for _ in range(4)]
    ms = sb("ms", [128, 4], F32); t_ms = [Trk() for _ in range(4)]
    rstd = sb("rstd", [128, 4], F32); t_rstd = [Trk() for _ in range(4)]
    absw = sb("absw", [128, 16], F32); sgn = sb("sgn", [128, 16], F32); wsc = sb("wsc", [128, 16], F32)
    t_iw = [Trk() for _ in range(4)]
    st6 = sb("st6", [128, 6], F32); mv = sb("mv", [128, 2], F32); lnr = sb("lnr", [128, 2], F32); t_ln = Trk()
    ln1 = sb("ln1", [128, 256], F32); ln2 = sb("ln2", [128, 256], F32); t_ln1 = Trk(); t_ln2 = Trk()
    knd = sb("knd", [128, 128], BF16); t_knd = Trk()
    vn = sb("vn", [128, 256], BF16); t_vn = Trk()
    ybt = sb("ybt", [128, 256], BF16); t_ybt = Trk()
    mid = sb("mid", [128, 1], F32); cntt = sb("cntt", [128, 1], F32); ttt = sb("ttt", [128, 1], F32)
    thr = sb("thr", [128, 1], F32); t_bis = Trk(); t_mid = Trk(); t_bisa = Trk()
    sneg = sb("sneg", [128, 1], F32)
    rden = sb("rden", [128, 4], F32); t_rden = Trk()
    sq = sb("sq", [128, 256], F32); t_sq = Trk()
    oo = sb("oo", [128, 256], F32); t_oo = Trk()
    ssc = sb("ssc", [128, 4], F32); t_ssc = Trk()
    idxg_bc = sb("idxg_bc", [128, 64], F32); idxb_bc = sb("idxb_bc", [128, 64], F32)
    sgug_bc = sb("sgug_bc", [128, 256], F32); sgub_bc = sb("sgub_bc", [128, 256], F32)
    wsf = sb("wsf", [128, 4, 128], F32); wsb = sb("wsb", [128, 4, 128], BF16); WcT = sb("WcT", [128, 4, 128], BF16)
    bs = sb("bs", [128, 4], F32)
    lamt = sb("lamt", [128, 128], F32); lamp = sb("lamp", [128, 64], F32); lam2 = sb("lam2", [128, 2], F32)
    neglam = sb("neglam", [128, 1], F32)
    gsub = sb("gsub", [128, 64], F32)
    t_par = Trk()

    ps = [es.enter_context(nc.psum_tensor("ps%d" % i, [128, 512], F32)) for i in range(7)]
    psT = es.enter_context(nc.psum_tensor("psT", [128, 1024], BF16))
    t_ps = [Trk(True) for _ in range(7)]; t_psT = Trk(True)
    rot = {"i": 0}

    def nb(lst=(4, 5, 6)):
        rot["i"] += 1
        return lst[rot["i"] % len(lst)]
    ALLB = (0, 1, 2, 3, 4, 5, 6)
    wrot = {"i": 0}

    def wslot():
        wrot["i"] += 1
        return wrot["i"] % NW
    evrot = {"i": 0}

    def evac_copy(out_ap, in_ap, r, w=(), wa=()):
        evrot["i"] += 1
        if evrot["i"] % 2:
            S.op(act, lambda: nc.scalar.copy(out=out_ap, in_=in_ap), r=r, w=w, wa=wa)
        else:
            S.op(dve, lambda: nc.vector.tensor_copy(out=out_ap, in_=in_ap), r=r, w=w, wa=wa)

    S.op(pool, lambda: nc.gpsimd.memset(Vae[:], 1.0), w=t_Vae)
    S.op(pool, lambda: nc.gpsimd.memset(Vce[:], 1.0), w=t_Vce)
    for q in range(4):
        S.dma(acc[:, q * 1024:(q + 1) * 1024], rampv[0:1, q * 1024:(q + 1) * 1024].partition_broadcast(128), r=[t_in],
              wa=[t_acc])
    S.op(dve, lambda: nc.vector.tensor_copy(out=ramp[:], in_=acc[:]), r=[t_acc], w=[t_ramp])

    dbg_outs = {}

    chkc = {}

    marks = []

    def chk(tag):
        chkc[tag] = chkc.get(tag, 0) + 1
        marks.append((tag, chkc[tag], S.pe.count, S.act.count, S.dve.count, S.pool.count))
        if stop == tag or stop == "%s:%d" % (tag, chkc[tag]):
            raise _Stop()

    def dump(name, ap, trks, shape, dt=F32):
        if not dbg: return
        d = nc.dram_tensor("dbg_" + name, list(shape), dt, kind="ExternalOutput").ap()
        S.dma(d, ap, r=trks)
        dbg_outs[name] = shape

    def rmsnorm_to_hT(b):
        S.op(act, lambda: nc.scalar.activation(out=hn[:], in_=xb[:, b, :], func=AF.Square, accum_out=ss[:, b:b + 1]),
             r=[t_xb[b]], w=[t_hn, t_ss[b]])
        S.op(dve, lambda: nc.vector.tensor_scalar(out=ms[:, b:b + 1], in0=ss[:, b:b + 1], scalar1=1.0 / D, scalar2=EPS,
                                                  op0=ALU.mult, op1=ALU.add), r=[t_ss[b]], w=[t_ms[b]])
        S.op(pool, lambda: nc.gpsimd.tensor_tensor(out=rstd[:, b:b + 1], in0=ms[:, b:b + 1], in1=neghalf[:, 0:1],
                                                   op=ALU.pow), r=[t_ms[b], t_cst], w=[t_rstd[b]])
        S.op(dve, lambda: nc.vector.tensor_scalar(out=hn[:], in0=xb[:, b, :], scalar1=rstd[:, b:b + 1], scalar2=None,
                                                  op0=ALU.mult), r=[t_xb[b], t_rstd[b]], w=[t_hn])
        for kc in range(8):
            S.op(pe, lambda kc=kc: nc.tensor.transpose(psT[:, kc * 128:(kc + 1) * 128], hn[:, kc * 128:(kc + 1) * 128],
                                                      ident_bf[:]), r=[t_hn, t_cst], w=[t_psT])
        evac_copy(hT[:, :, b * 128:(b + 1) * 128], psT[:, :].rearrange("p (k t) -> p k t", k=8), r=[t_psT], wa=[t_hT])

    def load_w(src_ap, nk, cw, prep_t):
        s = wslot()
        S.dma(wbuf[s][:, 0:nk, 0:cw], src_ap, r=[prep_t], w=[t_wbuf[s]])
        return s

    def layernorm_stats(in_ap):
        S.op(dve, lambda: nc.vector.bn_stats(out=st6[:], in_=in_ap[0]), r=in_ap[1], w=[t_ln])
        chk('LN1')
        S.op(dve, lambda: nc.vector.bn_aggr(out=mv[:], in_=st6[:]), r=[t_ln], w=[t_ln])
        chk('LN2')
        S.op(dve, lambda: nc.vector.tensor_scalar(out=lnr[:, 1:2], in0=mv[:, 1:2], scalar1=EPS, scalar2=None, op0=ALU.add),
             r=[t_ln], w=[t_ln])
        chk('LN3')
        S.op(pool, lambda: nc.gpsimd.tensor_tensor(out=lnr[:, 0:1], in0=lnr[:, 1:2], in1=neghalf[:, 0:1], op=ALU.pow),
             r=[t_ln, t_cst], w=[t_ln])

    try:
      for l in range(n_layers):
        lambda_init = 0.8 - 0.6 * math.exp(-0.3 * l)
        last = (l == n_layers - 1)
        xsrc = x_in if l == 0 else xs
        S.dma(idxg_bc[:], idx_g[l:l + 1, :].partition_broadcast(128), r=[t_in], w=[t_par])
        S.dma(idxb_bc[:], idx_b[l:l + 1, :].partition_broadcast(128), r=[t_in], wa=[t_par])
        S.dma(sgug_bc[:], sgu_g[l:l + 1, :].partition_broadcast(128), r=[t_in], wa=[t_par])
        S.dma(sgub_bc[:], sgu_b[l:l + 1, :].partition_broadcast(128), r=[t_in], wa=[t_par])
        S.dma(wsf[:], sgu_w[l].rearrange("g t s -> t g s"), r=[t_in], wa=[t_par])
        S.dma(bs[:], sgu_bs[l].rearrange("g t -> t g"), r=[t_in], wa=[t_par], allow_slow_non_contiguous=True)
        S.dma(lamt[:], dlam[l:l + 1].rearrange("o a b -> o (a b)").partition_broadcast(128), r=[t_in], wa=[t_par])
        S.dma(gsub[:], subln[l:l + 1, :].partition_broadcast(128), r=[t_in], wa=[t_par])
        for g in range(4):
            S.op(dve, lambda g=g: nc.vector.tensor_tensor(out=wsb[:, g, :], in0=wsf[:, g, :], in1=tril, op=ALU.mult),
                 r=[t_par, t_cst], w=[t_par])
        for g in range(4):
            S.op(pe, lambda g=g: nc.tensor.transpose(psT[:, g * 128:(g + 1) * 128], wsb[:, g, :], ident_bf[:]),
                 r=[t_par, t_cst], w=[t_psT])
        S.op(dve, lambda: nc.vector.tensor_copy(out=WcT[:], in_=psT[:, 0:512].rearrange("p (g t) -> p g t", g=4)),
             r=[t_psT], w=[t_par])
        lt4 = lamt[:].rearrange("p (a b) -> p a b", a=4)
        S.op(dve, lambda: nc.vector.tensor_tensor(out=lamp[:].rearrange("p (a b) -> p a b", a=2), in0=lt4[:, 0:4:2, :],
                                                  in1=lt4[:, 1:4:2, :], op=ALU.mult), r=[t_par], w=[t_par])
        S.op(dve, lambda: nc.vector.tensor_reduce(out=lam2[:], in_=lamp[:].rearrange("p (a b) -> p a b", a=2), axis=AX.X,
                                                  op=ALU.add), r=[t_par], w=[t_par])
        S.op(act, lambda: nc.scalar.activation(out=lam2[:], in_=lam2[:], func=AF.Exp), r=[t_par], w=[t_par])
        S.op(dve, lambda: nc.vector.tensor_tensor(out=neglam[:], in0=lam2[:, 1:2], in1=lam2[:, 0:1], op=ALU.subtract),
             r=[t_par], w=[t_par])
        S.op(dve, lambda: nc.vector.tensor_scalar(out=neglam[:], in0=neglam[:], scalar1=-lambda_init, scalar2=None,
                                                  op0=ALU.add), r=[t_par], w=[t_par])
        S.op(dve, lambda: nc.vector.tensor_scalar(out=gsub[:], in0=gsub[:], scalar1=(1.0 - lambda_init), scalar2=None,
                                                  op0=ALU.mult), r=[t_par], w=[t_par])

        chk('par')
        for j in range(n_tiles):
            nkb = 4 * j + 4
            for b in range(4):
                gb = 4 * j + b
                S.dma(xb[:, b, :], xsrc[gb * 128:(gb + 1) * 128, :], r=[t_in if l == 0 else t_xs[j]], w=[t_xb[b]])
            for b in range(4):
                rmsnorm_to_hT(b)

            chk('A')
            s1 = load_w(win_bf[l][:, :, TM_AV:TM_AV + 324].rearrange("k p c -> p k c"), 8, 324, t_prep[l])
            s2 = load_w(win_bf[l][:, :, TM_CV:TM_CV + 256].rearrange("k p c -> p k c"), 8, 256, t_prep[l])
            for b in range(4):
                gb = 4 * j + b
                bk = nb(ALLB)
                for kc in range(8):
                    S.op(pe, lambda kc=kc: nc.tensor.matmul(ps[bk][:, 0:324], lhsT=hT[:, kc, b * 128:(b + 1) * 128],
                                                            rhs=wbuf[s1][:, kc, 0:324], start=(kc == 0), stop=(kc == 7)),
                         r=[t_hT, t_wbuf[s1]], w=[t_ps[bk]])
                chk('B1a')
                evac_copy(Vae[:, gb, :].rearrange("p (h e) -> p h e", h=4)[:, :, 0:64],
                          ps[bk][:, 0:256].rearrange("p (h e) -> p h e", h=4), r=[t_ps[bk]], wa=[t_Vae[j]])
                chk('B1b')
                S.op(act, lambda: nc.scalar.copy(out=ln2[:, 64:128], in_=ps[bk][:, 256:320]), r=[t_ps[bk]], w=[t_ln2])
                chk('LN0')
                layernorm_stats((ln2[:, 64:128], [t_ln2]))
                chk('B1b1')
                S.op(dve, lambda: nc.vector.tensor_scalar(out=ln1[:, 0:64], in0=ln2[:, 64:128], scalar1=mv[:, 0:1],
                                                          scalar2=lnr[:, 0:1], op0=ALU.subtract, op1=ALU.mult),
                     r=[t_ln2, t_ln], w=[t_ln1])
                chk('B1b2')
                S.op(dve, lambda: nc.vector.tensor_tensor(out=ln2[:, 0:64], in0=ln1[:, 0:64], in1=idxg_bc[:], op=ALU.mult),
                     r=[t_ln1, t_par], w=[t_ln2])
                chk('B1b3')
                S.op(dve, lambda: nc.vector.tensor_tensor(out=knd[:, 0:64], in0=ln2[:, 0:64], in1=idxb_bc[:], op=ALU.add),
                     r=[t_ln2, t_par], w=[t_knd])
                S.op(dve, lambda: nc.vector.tensor_tensor(out=knd[:, 64:128], in0=ln2[:, 0:64], in1=idxb_bc[:], op=ALU.add),
                     r=[t_ln2, t_par], wa=[t_knd])
                chk('B1c0')
                S.op(pe, lambda: nc.tensor.transpose(psT[:, 0:128], knd[:], ident_bf[:]), r=[t_knd, t_cst], w=[t_psT])
                evac_copy(KiT[:, gb * 128:(gb + 1) * 128], psT[:, 0:128], r=[t_psT], wa=[t_KiT[j]])
                chk('B1c')
                S.op(dve, lambda: nc.vector.tensor_scalar(out=wsc[:, b * 4:b * 4 + 4], in0=ps[bk][:, 320:324],
                                                          scalar1=1.0 / 16.0, scalar2=None, op0=ALU.mult),
                     r=[t_ps[bk]], w=[t_iw[b]])
                S.op(dve, lambda: nc.vector.scalar_tensor_tensor(out=absw[:, b * 4:b * 4 + 4], in0=wsc[:, b * 4:b * 4 + 4],
                                                                 scalar=-1.0, in1=wsc[:, b * 4:b * 4 + 4], op0=ALU.mult,
                                                                 op1=ALU.max), r=[t_iw[b]], w=[t_iw[b]])
                S.op(dve, lambda: nc.vector.tensor_scalar(out=sgn[:, b * 4:b * 4 + 4], in0=wsc[:, b * 4:b * 4 + 4],
                                                          scalar1=0.0, scalar2=2.0, op0=ALU.is_ge, op1=ALU.mult),
                     r=[t_iw[b]], w=[t_iw[b]])
                S.op(dve, lambda: nc.vector.tensor_scalar(out=sgn[:, b * 4:b * 4 + 4], in0=sgn[:, b * 4:b * 4 + 4],
                                                          scalar1=-1.0, scalar2=None, op0=ALU.add),
                     r=[t_iw[b]], w=[t_iw[b]])
                chk('B1d')
                bk2 = nb(ALLB)
                for kc in range(8):
                    S.op(pe, lambda kc=kc: nc.tensor.matmul(ps[bk2][:, 0:256], lhsT=hT[:, kc, b * 128:(b + 1) * 128],
                                                            rhs=wbuf[s2][:, kc, 0:256], start=(kc == 0), stop=(kc == 7)),
                         r=[t_hT, t_wbuf[s2]], w=[t_ps[bk2]])
                evac_copy(Vce[:, gb, :].rearrange("p (h e) -> p h e", h=4)[:, :, 0:64],
                          ps[bk2][:, 0:256].rearrange("p (h e) -> p h e", h=4), r=[t_ps[bk2]], wa=[t_Vce[j]])
            chk('B1')
            chk('B1e')
            s3 = load_w(win_bf[l][:, :, TM_BUV:TM_BUV + 512].rearrange("k p c -> p k c"), 8, 512, t_prep[l])
            for b in range(4):
                bk = nb(ALLB)
                for kc in range(8):
                    S.op(pe, lambda kc=kc: nc.tensor.matmul(ps[bk][:, 0:512], lhsT=hT[:, kc, b * 128:(b + 1) * 128],
                                                            rhs=wbuf[s3][:, kc, 0:512], start=(kc == 0), stop=(kc == 7)),
                         r=[t_hT, t_wbuf[s3]], w=[t_ps[bk]])
                S.op(act, lambda: nc.scalar.activation(out=gbuf, in_=ps[bk][:, 0:512], func=AF.Gelu_apprx_tanh),
                     r=[t_ps[bk]], w=[t_gbuf])
                layernorm_stats((gbuf[:, 256:512], [t_gbuf]))
                S.op(dve, lambda: nc.vector.tensor_scalar(out=ln1[:], in0=gbuf[:, 256:512], scalar1=mv[:, 0:1],
                                                          scalar2=lnr[:, 0:1], op0=ALU.subtract, op1=ALU.mult),
                     r=[t_gbuf, t_ln], w=[t_ln1])
                S.op(pool, lambda: nc.gpsimd.tensor_tensor(out=ln2[:], in0=ln1[:], in1=sgug_bc[:], op=ALU.mult),
                     r=[t_ln1, t_par], w=[t_ln2])
                S.op(pool, lambda: nc.gpsimd.tensor_tensor(out=vn[:], in0=ln2[:], in1=sgub_bc[:], op=ALU.add),
                     r=[t_ln2, t_par], w=[t_vn])
                bk2 = nb(ALLB)
                for g in range(4):
                    S.op(pe, lambda g=g: nc.tensor.matmul(ps[bk2][:, g * 64:(g + 1) * 64], lhsT=WcT[:, g, :],
                                                          rhs=vn[:, g * 64:(g + 1) * 64], start=(g == 0), stop=(g == 3),
                                                          skip_group_check=True), r=[t_vn, t_par], w=[t_ps[bk2]])
                for g in range(4):
                    S.op(dve, lambda g=g: nc.vector.scalar_tensor_tensor(
                        out=ybt[:, g * 64:(g + 1) * 64], in0=ps[bk2][:, g * 64:(g + 1) * 64], scalar=bs[:, g:g + 1],
                        in1=gbuf[:, g * 64:(g + 1) * 64], op0=ALU.add, op1=ALU.mult),
                        r=[t_ps[bk2], t_par, t_gbuf], w=[t_ybt] if g == 0 else [], wa=[] if g == 0 else [t_ybt])
                for c in range(2):
                    S.op(pe, lambda c=c: nc.tensor.transpose(psT[:, c * 128:(c + 1) * 128], ybt[:, c * 128:(c + 1) * 128],
                                                            ident_bf[:]), r=[t_ybt, t_cst], w=[t_psT])
                evac_copy(yT[:, 1, :, b * 128:(b + 1) * 128], psT[:, 0:256].rearrange("p (c t) -> p c t", c=2),
                          r=[t_psT], wa=[t_yT[1]])
            chk('B2')
            fm = [(FM_AQ, 512, [("qa", 0), ("qa", 1), ("ka", 0), ("ka", 1)]),
                  (FM_IQ, 512, [("qi", 0), ("qi", 1), ("qc", 0), ("qc", 1)]),
                  (FM_CK, 256, [("kc", 0), ("kc", 1)])]
            for (c0, cw, dests) in fm:
                s = load_w(win_bf[l][:, :, c0:c0 + cw].rearrange("k p c -> p k c"), 8, cw, t_prep[l])
                for ci, (kind, c) in enumerate(dests):
                    bk = nb(ALLB)
                    for kc in range(8):
                        S.op(pe, lambda kc=kc: nc.tensor.matmul(ps[bk][:, 0:512], lhsT=wbuf[s][:, kc, ci * 128:(ci + 1) * 128],
                                                                rhs=hT[:, kc, :], start=(kc == 0), stop=(kc == 7)),
                             r=[t_hT, t_wbuf[s]], w=[t_ps[bk]])
                    if kind == "qa": evac_copy(QaT[:, c, :], ps[bk][:, :], r=[t_ps[bk]], wa=[t_QaT])
                    elif kind == "qi": evac_copy(QiT[:, c, :], ps[bk][:, :], r=[t_ps[bk]], wa=[t_QiT])
                    elif kind == "qc": evac_copy(QcT[:, c, :], ps[bk][:, :], r=[t_ps[bk]], wa=[t_QcT])
                    elif kind == "ka": evac_copy(KaT[:, c, j * 512:(j + 1) * 512], ps[bk][:, :], r=[t_ps[bk]], wa=[t_KaT[j]])
                    elif kind == "kc": evac_copy(KcT[:, c, j * 512:(j + 1) * 512], ps[bk][:, :], r=[t_ps[bk]], wa=[t_KcT[j]])

            chk('B3')
            def gen_C():
                for qb in range(4):
                    gb = 4 * j + qb
                    ncols = (gb + 1) * 128
                    for kg in range((ncols + 511) // 512):
                        c0 = kg * 512; cw = min(512, ncols - c0)
                        for h in range(4):
                            bk = nb((6,))
                            rs = slice((h % 2) * 64, (h % 2) * 64 + 64)
                            S.op(pe, lambda: nc.tensor.matmul(ps[bk][:, 0:cw], lhsT=QiT[rs, h // 2, qb * 128:(qb + 1) * 128],
                                                              rhs=KiT[rs, c0:c0 + cw], start=True, stop=True),
                                 r=[t_QiT] + t_KiT[0:j + 1], w=[t_ps[bk]])
                            S.op(act, lambda: nc.scalar.activation(out=ps[bk][:, 0:cw], in_=ps[bk][:, 0:cw], func=AF.Relu,
                                                                   scale=absw[:, qb * 4 + h:qb * 4 + h + 1]),
                                 r=[t_iw[qb]], w=[t_ps[bk]])
                            in1 = ramp[:, c0:c0 + cw] if h == 0 else acc[:, c0:c0 + cw]
                            S.op(dve, lambda: nc.vector.scalar_tensor_tensor(
                                out=acc[:, c0:c0 + cw], in0=ps[bk][:, 0:cw], scalar=sgn[:, qb * 4 + h:qb * 4 + h + 1], in1=in1,
                                op0=ALU.mult, op1=ALU.add), r=[t_ps[bk], t_iw[qb], t_ramp], w=[t_acc])
                            yield
                    S.op(dve, lambda: nc.vector.tensor_tensor(out=acc[:, gb * 128:(gb + 1) * 128],
                                                              in0=acc[:, gb * 128:(gb + 1) * 128], in1=causal_big, op=ALU.add),
                         r=[t_cst], w=[t_acc])
                    yield
                    if gb >= 2:
                        n1 = (int(ncols * 0.50) // 64) * 64; n2 = ncols - n1
                        S.op(dve, lambda: nc.vector.memset(mid[:], 0.0), w=[t_bis, t_mid])
                        for k in range(NBIS):
                            ck = RNG / (2.0 ** k)
                            S.op(dve, lambda: nc.vector.tensor_scalar(out=mb[:, qb, 0:n1], in0=acc[:, 0:n1],
                                                                      scalar1=mid[:, 0:1], scalar2=None, op0=ALU.is_ge,
                                                                      op1=ALU.add, accum_out=cntt[:]),
                                 r=[t_acc, t_mid], w=[t_bis], wa=[t_mb[qb]])
                            S.op(act, lambda: nc.scalar.activation(out=mb[:, qb, n1:ncols], in_=acc[:, n1:ncols], func=AF.Sign,
                                                                   scale=-1.0, bias=mid[:, 0:1], accum_out=sneg[:]),
                                 r=[t_acc, t_mid], w=[t_bisa], wa=[t_mb[qb]])
                            S.op(dve, lambda: nc.vector.scalar_tensor_tensor(out=ttt[:], in0=cntt[:], scalar=2.0, in1=sneg[:],
                                                                             op0=ALU.mult, op1=ALU.subtract),
                                 r=[t_bisa], w=[t_bis])
                            S.op(dve, lambda: nc.vector.tensor_scalar(out=ttt[:], in0=ttt[:], scalar1=512.0 - n2 - 0.5,
                                                                      scalar2=ck, op0=ALU.is_ge, op1=ALU.mult), w=[t_bis])
                            S.op(dve, lambda: nc.vector.scalar_tensor_tensor(out=mid[:], in0=ttt[:], scalar=-ck / 2.0,
                                                                             in1=mid[:], op0=ALU.add, op1=ALU.add),
                                 w=[t_bis, t_mid])
                            yield
                        cK = RNG / (2.0 ** NBIS)
                        S.op(dve, lambda: nc.vector.tensor_scalar(out=thr[:], in0=mid[:], scalar1=-cK, scalar2=None,
                                                                  op0=ALU.add), w=[t_bis])
                    else:
                        S.op(dve, lambda: nc.vector.memset(thr[:], -RNG), w=[t_bis])
                    S.op(dve, lambda: nc.vector.tensor_scalar(out=mb[:, qb, 0:ncols], in0=acc[:, 0:ncols], scalar1=thr[:, 0:1],
                                                              scalar2=MASKV, op0=ALU.is_lt, op1=ALU.mult),
                         r=[t_acc, t_bis], w=[t_mb[qb]])
                    yield

            def attention(kind, comp, bankset, LA):
                if kind == "a":
                    KT, QT, VE, tK, tQ, tV = KaT, QaT, Vae, t_KaT, t_QaT, t_Vae
                    scale = 64 ** -0.5
                else:
                    KT, QT, VE, tK, tQ, tV = KcT, QcT, Vce, t_KcT, t_QcT, t_Vce
                    scale = 32 ** -0.5
                steps = [(h, kb) for h in range(4) for kb in range(nkb)]

                def qk(i):
                    h, kb = steps[i]
                    r0 = max(kb - 4 * j, 0); c0 = r0 * 128
                    bk = nb(bankset)
                    if kind == "a":
                        rs = slice((h % 2) * 64, (h % 2) * 64 + 64); ch = h // 2; tp = None
                    else:
                        idx = h * 2 + comp
                        rs = slice((idx % 4) * 32, (idx % 4) * 32 + 32); ch = idx // 4; tp = ((idx % 4) * 32, 0)
                    kw = {} if tp is None else {"tile_position": tp}
                    S.op(pe, lambda: nc.tensor.matmul(ps[bk][:, c0:512], lhsT=KT[rs, ch, kb * 128:(kb + 1) * 128],
                                                      rhs=QT[rs, ch, c0:512], start=True, stop=False, **kw),
                         r=[tQ, tK[kb // 4]], w=[t_ps[bk]])
                    if kind == "a":
                        for qb in range(r0, 4):
                            S.op(pe, lambda qb=qb: nc.tensor.matmul(ps[bk][:, qb * 128:(qb + 1) * 128],
                                                                    lhsT=mb[:, qb, kb * 128:(kb + 1) * 128], rhs=ident_bf[:],
                                                                    start=False, stop=(qb == 3), skip_group_check=True),
                                 r=[t_mb[qb], t_cst], w=[t_ps[bk]])
                    else:
                        if kb >= 4 * j:
                            S.op(pe, lambda: nc.tensor.matmul(ps[bk][:, r0 * 128:(r0 + 1) * 128], lhsT=cmb_bf[:],
                                                              rhs=ident_bf[:], start=False, stop=True,
                                                              skip_group_check=True), r=[t_cst], w=[t_ps[bk]])
                    pi = i % NPT
                    S.op(act, lambda: nc.scalar.activation(out=PT[pi][:, c0:512], in_=ps[bk][:, c0:512], func=AF.Exp,
                                                           scale=scale), r=[t_ps[bk]], w=[t_PT[pi]])

                def pv(i):
                    h, kb = steps[i]
                    r0 = max(kb - 4 * j, 0)
                    pi = i % NPT
                    for qb in range(r0, 4):
                        S.op(pe, lambda qb=qb: nc.tensor.matmul(ps[qb][:, h * 65:(h + 1) * 65],
                                                                lhsT=PT[pi][:, qb * 128:(qb + 1) * 128],
                                                                rhs=VE[:, kb, h * 65:(h + 1) * 65],
                                                                start=(h == 0 and kb == 0), stop=(kb == 4 * j + qb),
                                                                skip_group_check=True),
                             r=[t_PT[pi], tV[kb // 4]], w=[t_ps[qb]])
                n = len(steps)
                for i in range(min(LA, n)): qk(i)
                for i in range(n):
                    if i + LA < n: qk(i + LA)
                    pv(i)
                    yield

            def out_views(qb):
                o4 = ps[qb][:, 0:260].rearrange("p (h e) -> p h e", h=4)
                return o4[:, :, 0:64], o4[:, :, 64:65]

            def transpose_to_yT(src, t_src, br, qb):
                for c in range(2):
                    S.op(pe, lambda c=c: nc.tensor.transpose(psT[:, c * 128:(c + 1) * 128], src[:, c * 128:(c + 1) * 128],
                                                            ident_bf[:]), r=[t_src, t_cst], w=[t_psT])
                evac_copy(yT[:, br, :, qb * 128:(qb + 1) * 128], psT[:, 0:256].rearrange("p (c t) -> p c t", c=2),
                          r=[t_psT], wa=[t_yT[br]])

            def gen_E():
                yield from attention("c", 0, (4, 5), 1)
                for qb in range(4):
                    ov, dv = out_views(qb)
                    S.op(dve, lambda: nc.vector.reciprocal(out=rden[:].rearrange("p (h o) -> p h o", o=1), in_=dv),
                         r=[t_ps[qb]], w=[t_rden])
                    S.op(dve, lambda: nc.vector.tensor_tensor(out=o1[:, qb, :].rearrange("p (h e) -> p h e", h=4), in0=ov,
                                                              in1=rden[:].rearrange("p (h o) -> p h o", o=1).to_broadcast([128, 4, 64]),
                                                              op=ALU.mult), r=[t_ps[qb], t_rden], w=[t_o1[qb]])
                yield
                yield from attention("c", 1, (4, 5), 1)
                for qb in range(4):
                    ov, dv = out_views(qb)
                    S.op(dve, lambda: nc.vector.reciprocal(out=rden[:].rearrange("p (h o) -> p h o", o=1), in_=dv),
                         r=[t_ps[qb]], w=[t_rden])
                    S.op(dve, lambda: nc.vector.tensor_scalar(out=rden[:], in0=rden[:], scalar1=neglam[:, 0:1], scalar2=None,
                                                              op0=ALU.mult), r=[t_par], w=[t_rden])
                    S.op(dve, lambda: nc.vector.tensor_tensor(out=oo[:].rearrange("p (h e) -> p h e", h=4), in0=ov,
                                                              in1=rden[:].rearrange("p (h o) -> p h o", o=1).to_broadcast([128, 4, 64]),
                                                              op=ALU.mult), r=[t_ps[qb], t_rden], w=[t_oo])
                    S.op(pool, lambda: nc.gpsimd.tensor_tensor(out=oo[:], in0=oo[:], in1=o1[:, qb, :], op=ALU.add),
                         r=[t_o1[qb]], w=[t_oo])
                    S.op(pool, lambda: nc.gpsimd.tensor_tensor(out=sq[:], in0=oo[:], in1=oo[:], op=ALU.mult), r=[t_oo], w=[t_sq])
                    S.op(dve, lambda: nc.vector.tensor_reduce(out=ssc[:], in_=sq[:].rearrange("p (h e) -> p h e", h=4), axis=AX.X,
                                                              op=ALU.add), r=[t_sq], w=[t_ssc])
                    S.op(dve, lambda: nc.vector.tensor_scalar(out=ssc[:], in0=ssc[:], scalar1=1.0 / 64.0, scalar2=EPS,
                                                              op0=ALU.mult, op1=ALU.add), w=[t_ssc])
                    S.op(pool, lambda: nc.gpsimd.tensor_tensor(out=ssc[:], in0=ssc[:], in1=neghalf[:, 0:1].to_broadcast([128, 4]),
                                                               op=ALU.pow), r=[t_cst], w=[t_ssc])
                    S.op(dve, lambda: nc.vector.tensor_tensor(out=sq[:].rearrange("p (h e) -> p h e", h=4),
                                                              in0=oo[:].rearrange("p (h e) -> p h e", h=4),
                                                              in1=ssc[:].rearrange("p (h o) -> p h o", o=1).to_broadcast([128, 4, 64]),
                                                              op=ALU.mult), r=[t_oo, t_ssc], w=[t_sq])
                    S.op(pool, lambda: nc.gpsimd.tensor_tensor(out=ybt[:].rearrange("p (h e) -> p h e", h=4),
                                                               in0=sq[:].rearrange("p (h e) -> p h e", h=4),
                                                               in1=gsub[:].rearrange("p (o e) -> p o e", o=1).to_broadcast([128, 4, 64]),
                                                               op=ALU.mult), r=[t_sq, t_par], w=[t_ybt])
                    transpose_to_yT(ybt, t_ybt, 2, qb)
                    yield

            nC = 0
            for qb in range(4):
                gb = 4 * j + qb
                nC += 4 * ((gb + 1 + 3) // 4) + 1 + (NBIS if gb >= 2 else 0) + 1
            nE = 2 * 4 * nkb + 1 + 4
            gC, gE = gen_C(), gen_E()
            eE = 0
            for ci in range(1, nC + 1):
                next(gC, None)
                target = (ci * nE) // nC
                while eE < target:
                    next(gE, None); eE += 1
            for _ in gC: pass
            for _ in gE: pass
            chk('C')
            for _ in attention("a", 0, (4, 5, 6), 2): pass
            for qb in range(4):
                ov, dv = out_views(qb)
                S.op(dve, lambda: nc.vector.reciprocal(out=rden[:].rearrange("p (h o) -> p h o", o=1), in_=dv),
                     r=[t_ps[qb]], w=[t_rden])
                S.op(dve, lambda: nc.vector.tensor_tensor(out=ybt[:].rearrange("p (h e) -> p h e", h=4), in0=ov,
                                                          in1=rden[:].rearrange("p (h o) -> p h o", o=1).to_broadcast([128, 4, 64]),
                                                          op=ALU.mult), r=[t_ps[qb], t_rden], w=[t_ybt])
                transpose_to_yT(ybt, t_ybt, 0, qb)
            chk('D')
            chk('E')
            for fc in range(8):
                if fc % 4 == 0:
                    S.dma(wbr_sb[:], wbr_bf[l][:, :, (fc // 4) * 512:(fc // 4 + 1) * 512].rearrange("k p c -> p k c"),
                          r=[t_prep[l]], w=[t_wbr])
                sg = load_w(win_bf[l][:, :, FM_G + fc * 384:FM_G + (fc + 1) * 384].rearrange("k p c -> p k c"), 8, 384,
                            t_prep[l])
                for i in range(3):
                    bkg = nb(ALLB)
                    for kc in range(8):
                        S.op(pe, lambda kc=kc: nc.tensor.matmul(ps[bkg][:, 0:512], lhsT=wbuf[sg][:, kc, i * 128:(i + 1) * 128],
                                                                rhs=hT[:, kc, :], start=(kc == 0), stop=(kc == 7)),
                             r=[t_hT, t_wbuf[sg]], w=[t_ps[bkg]])
                    S.op(act, lambda: nc.scalar.activation(out=sig[i], in_=ps[bkg][:, :], func=AF.Sigmoid),
                         r=[t_ps[bkg]], w=[t_sig[i]])
                    bkz = nb(ALLB)
                    for c in range(2):
                        S.op(pe, lambda c=c: nc.tensor.matmul(ps[bkz][:, 0:512],
                                                              lhsT=wbr_sb[:, i * 2 + c, (fc % 4) * 128:(fc % 4 + 1) * 128],
                                                              rhs=yT[:, i, c, :], start=(c == 0), stop=(c == 1)),
                             r=[t_yT[i], t_wbr], w=[t_ps[bkz]])
                    di = 0 if i == 0 else 1
                    S.op(dve, lambda: nc.vector.tensor_tensor(out=tm[di], in0=ps[bkz][:, :], in1=sig[i], op=ALU.mult),
                         r=[t_ps[bkz], t_sig[i]], w=[t_tm[di]])
                    if i == 1:
                        S.op(pool, lambda: nc.gpsimd.tensor_tensor(out=tm[0], in0=tm[0], in1=tm[1], op=ALU.add),
                             r=[t_tm[1]], w=[t_tm[0]])
                    if i == 2:
                        S.op(pool, lambda: nc.gpsimd.tensor_tensor(out=mergedT[:, fc, :], in0=tm[0], in1=tm[1],
                                                                   op=ALU.add), r=[t_tm[0], t_tm[1]], wa=[t_mT])
            for half in range(2):
                s = load_w(wout_bf[l][:, :, half * 512:(half + 1) * 512].rearrange("k p c -> p k c"), 8, 512, t_prep[l])
                for b in range(4):
                    bk = nb(ALLB)
                    for kc in range(8):
                        S.op(pe, lambda kc=kc: nc.tensor.matmul(ps[bk][:, 0:512], lhsT=mergedT[:, kc, b * 128:(b + 1) * 128],
                                                                rhs=wbuf[s][:, kc, :], start=(kc == 0), stop=(kc == 7)),
                             r=[t_mT, t_wbuf[s]], w=[t_ps[bk]])
                    S.op(dve, lambda: nc.vector.tensor_tensor(out=xb[:, b, half * 512:(half + 1) * 512], in0=ps[bk][:, :],
                                                              in1=xb[:, b, half * 512:(half + 1) * 512], op=ALU.add),
                         r=[t_ps[bk]], w=[t_xb[b]])

            chk('F')
            for b in range(4):
                rmsnorm_to_hT(b)
            ri = 0
            for ffh in range(2):
                for grp in range(4):
                    cb = (ffh * 4 + grp) * 512
                    s = load_w(wff1_bf[l][:, :, cb:cb + 512].rearrange("k p c -> p k c"), 8, 512, t_prep[l])
                    for cc in range(4):
                        bk = nb((4, 5, 6))
                        for kc in range(8):
                            S.op(pe, lambda kc=kc: nc.tensor.matmul(ps[bk][:, 0:512], lhsT=wbuf[s][:, kc, cc * 128:(cc + 1) * 128],
                                                                    rhs=hT[:, kc, :], start=(kc == 0), stop=(kc == 7)),
                                 r=[t_hT, t_wbuf[s]], w=[t_ps[bk]])
                        ri ^= 1
                        S.op(act, lambda: nc.scalar.activation(out=rt[ri], in_=ps[bk][:, :], func=AF.Relu),
                             r=[t_ps[bk]], w=[t_rt[ri]])
                        S.op(pool, lambda: nc.gpsimd.tensor_tensor(out=aT[:, grp * 4 + cc, :], in0=rt[ri], in1=rt[ri],
                                                                   op=ALU.mult), r=[t_rt[ri]], wa=[t_aT])
                for half in range(2):
                    for wg in range(2):
                        k0 = ffh * 16 + wg * 8
                        s = load_w(wff2_bf[l][k0:k0 + 8, :, half * 512:(half + 1) * 512].rearrange("k p c -> p k c"), 8, 512,
                                   t_prep[l])
                        for b in range(4):
                            for c in range(8):
                                S.op(pe, lambda c=c: nc.tensor.matmul(ps[b][:, 0:512],
                                                                      lhsT=aT[:, wg * 8 + c, b * 128:(b + 1) * 128],
                                                                      rhs=wbuf[s][:, c, :], start=(wg == 0 and c == 0),
                                                                      stop=(wg == 1 and c == 7)),
                                     r=[t_aT, t_wbuf[s]], w=[t_ps[b]])
                    for b in range(4):
                        S.op(dve, lambda: nc.vector.tensor_tensor(out=xb[:, b, half * 512:(half + 1) * 512], in0=ps[b][:, :],
                                                                  in1=xb[:, b, half * 512:(half + 1) * 512], op=ALU.add),
                             r=[t_ps[b]], w=[t_xb[b]])
            chk('G')
            if last:
                S.dma(gfin, fin_g[0:1, :].partition_broadcast(128), r=[t_in], w=[t_gfin])
            for b in range(4):
                gb = 4 * j + b
                if last:
                    S.op(act, lambda: nc.scalar.activation(out=hn[:], in_=xb[:, b, :], func=AF.Square,
                                                           accum_out=ss[:, b:b + 1]), r=[t_xb[b]], w=[t_hn, t_ss[b]])
                    S.op(dve, lambda: nc.vector.tensor_scalar(out=ms[:, b:b + 1], in0=ss[:, b:b + 1], scalar1=1.0 / D,
                                                              scalar2=EPS, op0=ALU.mult, op1=ALU.add), r=[t_ss[b]], w=[t_ms[b]])
                    S.op(pool, lambda: nc.gpsimd.tensor_tensor(out=rstd[:, b:b + 1], in0=ms[:, b:b + 1], in1=neghalf[:, 0:1],
                                                               op=ALU.pow), r=[t_ms[b], t_cst], w=[t_rstd[b]])
                    S.op(dve, lambda: nc.vector.scalar_tensor_tensor(out=xb[:, b, :], in0=xb[:, b, :], scalar=rstd[:, b:b + 1],
                                                                     in1=gfin, op0=ALU.mult, op1=ALU.mult),
                         r=[t_rstd[b], t_gfin], w=[t_xb[b]])
                    S.dma(out[gb * 128:(gb + 1) * 128, :], xb[:, b, :], r=[t_xb[b]])
                else:
                    S.dma(xs[gb * 128:(gb + 1) * 128, :], xb[:, b, :], r=[t_xb[b]], wa=[t_xs[j]])

    except _Stop:
        pass
    S.barrier()
    es.close()
    S.marks = marks
    return nc, S, dbg_outs


def make_consts():
    t = np.arange(128)[:, None]; s = np.arange(128)[None, :]
    ident = np.eye(128, dtype=np.float32)
    causal_big = np.where(s <= t, 0.0, -1e30).astype(np.float32)
    cmb = np.where(s <= t, 0.0, MASKV).astype(np.float32)
    tril = (s <= t).astype(np.float32)
    consts = np.concatenate([ident, causal_big, cmb, tril], axis=1).astype(np.float32)
    bits = (np.arange(SEQ) + (27 << 7)).astype(np.uint16)
    rampv = -(bits.view(ml_dtypes.bfloat16).astype(np.float32))[None, :]
    return np.ascontiguousarray(consts), np.ascontiguousarray(rampv.astype(np.float32))


_CACHE = {}


def kernel(**inputs):
    if "nc" not in _CACHE:
        _CACHE["nc"] = build_nc()[0]
    nc = _CACHE["nc"]
    consts, rampv = make_consts()
    shared = {}
    for k, v in inputs.items():
        if k == "x": continue
        a = np.ascontiguousarray(np.asarray(v, dtype=np.float32))
        if k == "final_norm_g": a = a.reshape(1, D)
        shared[k] = a
    shared["consts"] = consts; shared["rampv"] = rampv
    x = np.asarray(inputs["x"], dtype=np.float32)
    in_maps = []
    for c in range(8):
        m = dict(shared); m["x"] = np.ascontiguousarray(x[c]); in_maps.append(m)
    res = run_bass_kernel_spmd(nc, in_maps, core_ids=list(range(8)))
    return np.stack([np.asarray(res.results[c]["out"], dtype=np.float32) for c in range(8)], axis=0)
```

```python
import math, contextlib
import numpy as np
import ml_dtypes
import concourse.bass as bass
import concourse.mybir as mybir
from concourse.bass_utils import run_bass_kernel_spmd

F32 = mybir.dt.float32; BF16 = mybir.dt.bfloat16; U8 = mybir.dt.uint8
AF = mybir.ActivationFunctionType; ALU = mybir.AluOpType; AX = mybir.AxisListType

D = 1024; SEQ = 4096; NL = 2; NIN = 5444; DFF = 4096
EPS = 1e-6
NBIS = 24
RNG = 32.0
MASKV = -30000.0
FM_AQ, FM_AK, FM_IQ, FM_CQ, FM_CK, FM_G = 0, 256, 512, 768, 1024, 1280
TM_AV, TM_IK, TM_IW, TM_CV, TM_BUV = 4352, 4608, 4672, 4676, 4932


class Trk:
    __slots__ = ("w", "r", "excl")

    def __init__(self, excl=False):
        self.w = {}; self.r = {}; self.excl = excl


class Eng:
    def __init__(self, key, h, sem, inorder=False):
        self.key = key; self.h = h; self.sem = sem; self.count = 0; self.seen = {}; self.inorder = inorder


class Sync:
    def __init__(self, nc, es, ndma=24):
        self.nc = nc
        self.sems = {}

        def mk(key, h, inorder=False):
            s = es.enter_context(nc.semaphore("s_" + key))
            self.sems[key] = s
            return Eng(key, h, s, inorder)
        self.pe = mk("pe", nc.tensor, True)
        self.act = mk("act", nc.scalar)
        self.dve = mk("dve", nc.vector)
        self.pool = mk("pool", nc.gpsimd)
        self.sp = Eng("sp", nc.sync, None)
        self.dsem = []
        for k in range(ndma):
            s = es.enter_context(nc.semaphore("s_d%d" % k))
            self.sems[("d", k)] = s
            self.dsem.append([s, 0])
        self.dnext = 0
        self.ninstr = 0

    def _waits(self, eng, r, w, wa):
        waits = {}
        for t in r:
            for k, v in t.w.items():
                if v > waits.get(k, 0): waits[k] = v
        for t in w:
            for k, v in t.w.items():
                if v > waits.get(k, 0): waits[k] = v
            for k, v in t.r.items():
                if v > waits.get(k, 0): waits[k] = v
        for t in wa:
            for k, v in t.r.items():
                if v > waits.get(k, 0): waits[k] = v
        for k, v in waits.items():
            if k == eng.key and eng.inorder: continue
            if eng.seen.get(k, 0) >= v: continue
            eng.h.wait_ge(self.sems[k], v)
            eng.seen[k] = v

    def op(self, eng, fn, r=(), w=(), wa=()):
        if any(t.excl for t in r):
            w = list(w) + [t for t in r if t.excl]
            r = [t for t in r if not t.excl]
        self._waits(eng, r, w, wa)
        ins = fn()
        eng.count += 1
        ins.then_inc(eng.sem, 1)
        self.ninstr += 1
        for t in r: t.r[eng.key] = eng.count
        for t in w: t.w[eng.key] = eng.count
        for t in wa: t.w[eng.key] = eng.count
        return ins

    def dma(self, out, in_, r=(), w=(), wa=(), **kw):
        q = self.sp
        self._waits(q, r, w, wa)
        k = self.dnext; self.dnext = (self.dnext + 1) % len(self.dsem)
        s, c = self.dsem[k]
        key = ("d", k)
        if c > 0 and q.seen.get(key, 0) < c:
            q.h.wait_ge(s, c); q.seen[key] = c
        q.h.dma_start(out=out, in_=in_, **kw).then_inc(s, 16)
        c += 16
        self.dsem[k][1] = c
        self.ninstr += 1
        for t in r: t.r[key] = c
        for t in w: t.w[key] = c
        for t in wa: t.w[key] = c

    def barrier(self):
        engs = [self.pe, self.act, self.dve, self.pool, self.sp]
        for e in engs:
            for o in [self.pe, self.act, self.dve, self.pool]:
                if o is e or o.count == 0: continue
                if e.seen.get(o.key, 0) < o.count:
                    e.h.wait_ge(o.sem, o.count); e.seen[o.key] = o.count
            for k, (s, c) in enumerate(self.dsem):
                if c and e.seen.get(("d", k), 0) < c:
                    e.h.wait_ge(s, c); e.seen[("d", k)] = c


class _Stop(Exception):
    pass


def build_nc(n_layers=NL, n_tiles=8, dbg=False, stop=None):
    nc = bass.Bass("TRN2", target_bir_lowering=False, dynamic_dma_scratch_size=512)
    es = contextlib.ExitStack()
    S = Sync(nc, es)

    def din(name, shape):
        return nc.dram_tensor(name, list(shape), F32, kind="ExternalInput").ap()
    x_in = din("x", [SEQ, D])
    attn_norm_g = din("attn_norm_g", [NL, D]); w_in = din("w_in", [NL, D, NIN])
    idx_g = din("idx_k_norm_g", [NL, 64]); idx_b = din("idx_k_norm_b", [NL, 64])
    sgu_g = din("sgu_norm_g", [NL, 256]); sgu_b = din("sgu_norm_b", [NL, 256])
    sgu_w = din("sgu_w_s", [NL, 4, 128, 128]); sgu_bs = din("sgu_b_s", [NL, 4, 128])
    dlam = din("diff_lambda", [NL, 4, 32]); subln = din("diff_subln_g", [NL, 64])
    wbrs = [din("w_branch_a", [NL, 256, D]), din("w_branch_b", [NL, 256, D]), din("w_branch_c", [NL, 256, D])]
    w_out = din("w_out", [NL, D, D]); mlp_norm_g = din("mlp_norm_g", [NL, D])
    w_ff1 = din("w_ff1", [NL, D, DFF]); w_ff2 = din("w_ff2", [NL, DFF, D])
    fin_g = din("final_norm_g", [1, D])
    consts = din("consts", [128, 512]); rampv = din("rampv", [1, SEQ])
    out = nc.dram_tensor("out", [SEQ, D], F32, kind="ExternalOutput").ap()

    def dscr(name, shape, dt):
        return nc.dram_tensor(name, list(shape), dt, kind="Internal").ap()
    win_bf = [dscr("win_bf%d" % l, [8, 128, NIN], BF16) for l in range(NL)]
    wff1_bf = [dscr("wff1_bf%d" % l, [8, 128, DFF], BF16) for l in range(NL)]
    wff2_bf = [dscr("wff2_bf%d" % l, [32, 128, D], BF16) for l in range(NL)]
    wout_bf = [dscr("wout_bf%d" % l, [8, 128, D], BF16) for l in range(NL)]
    wbr_bf = [dscr("wbr_bf%d" % l, [6, 128, D], BF16) for l in range(NL)]
    xs = dscr("xs", [SEQ, D], F32)
    t_prep = [Trk() for _ in range(NL)]
    t_xs = [Trk() for _ in range(8)]
    t_in = Trk()

    def sb(name, shape, dt, stack=es):
        return stack.enter_context(nc.sbuf_tensor(name, list(shape), dt))

    pe, act, dve, pool = S.pe, S.act, S.dve, S.pool

    cst = sb("cst", [128, 512], F32); t_cst = Trk()
    ident_bf = sb("ident_bf", [128, 128], BF16)
    cmb_bf = sb("cmb_bf", [128, 128], BF16)
    neghalf = sb("neghalf", [128, 1], F32)
    gpre = sb("gpre", [128, 2 * NL * 8], F32); t_gpre = Trk()
    S.dma(cst[:], consts[:, :], r=[t_in], w=[t_cst])
    causal_big = cst[:, 128:256]
    tril = cst[:, 384:512]
    S.op(dve, lambda: nc.vector.tensor_copy(out=ident_bf[:], in_=cst[:, 0:128]), r=[t_cst], w=[t_cst])
    S.op(dve, lambda: nc.vector.tensor_copy(out=cmb_bf[:], in_=cst[:, 256:384]), r=[t_cst], w=[t_cst])
    S.op(pool, lambda: nc.gpsimd.memset(neghalf[:], -0.5), w=[t_cst])
    for l in range(NL):
        S.dma(gpre[:, l * 16:l * 16 + 8], attn_norm_g[l].rearrange("(k p) -> p k", p=128), r=[t_in], wa=[t_gpre],
              allow_slow_non_contiguous=True)
        S.dma(gpre[:, l * 16 + 8:l * 16 + 16], mlp_norm_g[l].rearrange("(k p) -> p k", p=128), r=[t_in], wa=[t_gpre],
              allow_slow_non_contiguous=True)

    with contextlib.ExitStack() as pes:
        NB = 4; PD = 3
        pin = [sb("pin%d" % i, [128, 1024], F32, pes) for i in range(NB)]
        pout = [sb("pout%d" % i, [128, 1024], BF16, pes) for i in range(NB)]
        t_pin = [Trk() for _ in range(NB)]; t_pout = [Trk() for _ in range(NB)]
        plist = []

        def piece(l, src_ap, dst_ap, w, gcol, dst_view=None):
            plist.append((l, src_ap, dst_ap, w, gcol, dst_view))

        def p_load(n):
            (l, src_ap, dst_ap, w, gcol, dst_view) = plist[n]
            i = n % NB
            S.dma(pin[i][:, 0:w], src_ap, r=[t_in], w=[t_pin[i]])

        def p_cast_store(n):
            (l, src_ap, dst_ap, w, gcol, dst_view) = plist[n]
            i = n % NB
            e = [dve, act][n % 2]
            if gcol is None:
                if e is act:
                    fn = lambda: nc.scalar.copy(out=pout[i][:, 0:w], in_=pin[i][:, 0:w])
                else:
                    fn = lambda: e.h.tensor_copy(out=pout[i][:, 0:w], in_=pin[i][:, 0:w])
            else:
                g = gpre[:, gcol:gcol + 1]
                if e is act:
                    fn = lambda: nc.scalar.activation(out=pout[i][:, 0:w], in_=pin[i][:, 0:w], func=AF.Copy, scale=g)
                else:
                    fn = lambda: e.h.tensor_scalar(out=pout[i][:, 0:w], in0=pin[i][:, 0:w], scalar1=g, scalar2=None,
                                                   op0=ALU.mult)
            S.op(e, fn, r=[t_pin[i], t_gpre], w=[t_pout[i]])
            src = pout[i][:, 0:w] if dst_view is None else dst_view(pout[i])
            S.dma(dst_ap, src, r=[t_pout[i]], wa=[t_prep[l]])

        segs = [(0, FM_AQ, 512), (768, FM_IQ, 256), (1604, FM_CQ, 512), (512, TM_AV, 256), (1024, TM_IK, 68),
                (2116, TM_CV, 256), (1092, TM_BUV, 512)]
        for l in range(n_layers):
            for kc in range(8):
                rows = slice(kc * 128, (kc + 1) * 128)
                for (s0, d0, w) in segs:
                    piece(l, w_in[l, rows, s0:s0 + w], win_bf[l][kc, :, d0:d0 + w], w, l * 16 + kc)
                for i in range(3):
                    dst = win_bf[l][kc, :, FM_G:FM_G + 3072].rearrange("p (f i c) -> p f i c", f=8, i=3)[:, :, i, :]
                    piece(l, w_in[l, rows, 2372 + i * 1024:2372 + (i + 1) * 1024], dst, 1024, l * 16 + kc,
                          dst_view=lambda t: t[:, 0:1024].rearrange("p (f c) -> p f c", f=8))
                for c in range(4):
                    piece(l, w_ff1[l, rows, c * 1024:(c + 1) * 1024], wff1_bf[l][kc, :, c * 1024:(c + 1) * 1024], 1024,
                          l * 16 + 8 + kc)
                piece(l, w_out[l, rows, :], wout_bf[l][kc, :, :], 1024, None)
            for kc in range(32):
                piece(l, w_ff2[l, kc * 128:(kc + 1) * 128, :], wff2_bf[l][kc, :, :], 1024, None)
            for i in range(3):
                for c in range(2):
                    piece(l, wbrs[i][l, c * 128:(c + 1) * 128, :], wbr_bf[l][i * 2 + c, :, :], 1024, None)
        for n in range(min(PD, len(plist))):
            p_load(n)
        for n in range(len(plist)):
            if n + PD < len(plist):
                p_load(n + PD)
            p_cast_store(n)
        S.barrier()
    if stop == 'prep':
        S.barrier(); es.close(); return nc, S, {}

    ramp = sb("ramp", [128, SEQ], BF16); t_ramp = Trk()
    KaT = sb("KaT", [128, 2, SEQ], BF16); t_KaT = [Trk() for _ in range(8)]
    KcT = sb("KcT", [128, 2, SEQ], BF16); t_KcT = [Trk() for _ in range(8)]
    KiT = sb("KiT", [128, SEQ], BF16); t_KiT = [Trk() for _ in range(8)]
    Vae = sb("Vae", [128, 32, 260], BF16); t_Vae = [Trk() for _ in range(8)]
    Vce = sb("Vce", [128, 32, 260], BF16); t_Vce = [Trk() for _ in range(8)]
    xb = sb("xb", [128, 4, D], F32); t_xb = [Trk() for _ in range(4)]
    hT = sb("hT", [128, 8, 512], BF16); t_hT = Trk()
    QaT = sb("QaT", [128, 2, 512], BF16); t_QaT = Trk()
    QiT = sb("QiT", [128, 2, 512], BF16); t_QiT = Trk()
    QcT = sb("QcT", [128, 2, 512], BF16); t_QcT = Trk()
    acc = sb("acc", [128, SEQ], F32); t_acc = Trk()
    aT = acc[:, :].bitcast(BF16).rearrange("p (c t) -> p c t", c=16); t_aT = t_acc
    mb = sb("mb", [128, 4, SEQ], BF16); t_mb = [Trk() for _ in range(4)]
    mergedT = mb[:, 0, :].rearrange("p (k t) -> p k t", k=8); t_mT = t_mb[0]
    gfin = mb[:, 1, :].bitcast(F32)[:, 0:D]; t_gfin = t_mb[1]
    yT = sb("yT", [128, 3, 2, 512], BF16); t_yT = [Trk() for _ in range(3)]
    NW = 2
    wbuf = [sb("wbuf%d" % i, [128, 8, 512], BF16) for i in range(NW)]; t_wbuf = [Trk() for _ in range(NW)]
    wbr_sb = sb("wbr_sb", [128, 6, 512], BF16); t_wbr = Trk()
    NPT = 3
    PT = [sb("PT%d" % i, [128, 512], BF16) for i in range(NPT)]; t_PT = [Trk() for _ in range(NPT)]
    RR = sb("RR", [128, 5, 512], F32); t_RR = [Trk() for _ in range(5)]
    sig = [RR[:, i, :] for i in range(3)]; t_sig = t_RR[0:3]
    tm = [RR[:, 3 + i, :] for i in range(2)]; t_tm = t_RR[3:5]
    rt = [RR[:, i, :] for i in range(2)]; t_rt = t_RR[0:2]
    gbuf = RR[:, 2, :]; t_gbuf = t_RR[2]
    o1 = RR[:, 0:2, :].rearrange("p a (b e) -> p (a b) e", b=2); t_o1 = [t_RR[0], t_RR[0], t_RR[1], t_RR[1]]
    hn2 = [sb("hn%d" % i, [128, D], BF16) for i in range(2)]; t_hn2 = [Trk() for _ in range(2)]
    ss = sb("ss", [128, 4], F32); t_ss = [Trk() for _ in range(4)]
    ms = sb("ms", [128, 4], F32); t_ms = [Trk() for _ in range(4)]
    rstd = sb("rstd", [128, 4], F32); t_rstd = [Trk() for _ in range(4)]
    absw = sb("absw", [128, 16], F32); sgn = sb("sgn", [128, 16], F32); wsc = sb("wsc", [128, 16], F32)
    t_iw = [Trk() for _ in range(4)]
    st6 = sb("st6", [128, 6], F32); mv = sb("mv", [128, 2], F32); lnr = sb("lnr", [128, 2], F32); t_ln = Trk()
    ln1 = sb("ln1", [128, 256], F32); ln2 = sb("ln2", [128, 256], F32); t_ln1 = Trk(); t_ln2 = Trk()
    knd = sb("knd", [128, 128], BF16); t_knd = Trk()
    vn = sb("vn", [128, 256], BF16); t_vn = Trk()
    ybt = sb("ybt", [128, 256], BF16); t_ybt = Trk()
    mid = sb("mid", [128, 1], F32); cntt = sb("cntt", [128, 1], F32); ttt = sb("ttt", [128, 1], F32)
    thr = sb("thr", [128, 1], F32); t_bis = Trk(); t_mid = Trk(); t_bisa = Trk()
    sneg = sb("sneg", [128, 1], F32)
    rden = sb("rden", [128, 4], F32); t_rden = Trk()
    sq = sb("sq", [128, 256], F32); t_sq = Trk()
    oo = sb("oo", [128, 256], F32); t_oo = Trk()
    ssc = sb("ssc", [128, 4], F32); t_ssc = Trk()
    idxg_bc = sb("idxg_bc", [128, 64], F32); idxb_bc = sb("idxb_bc", [128, 64], F32)
    sgug_bc = sb("sgug_bc", [128, 256], F32); sgub_bc = sb("sgub_bc", [128, 256], F32)
    wsf = sb("wsf", [128, 4, 128], F32); wsb = sb("wsb", [128, 4, 128], BF16); WcT = sb("WcT", [128, 4, 128], BF16)
    bs = sb("bs", [128, 4], F32)
    lamt = sb("lamt", [128, 128], F32); lamp = sb("lamp", [128, 64], F32); lam2 = sb("lam2", [128, 2], F32)
    neglam = sb("neglam", [128, 1], F32)
    gsub = sb("gsub", [128, 64], F32)
    t_par = Trk()

    ps = [es.enter_context(nc.psum_tensor("ps%d" % i, [128, 512], F32)) for i in range(7)]
    psT = es.enter_context(nc.psum_tensor("psT", [128, 1024], BF16))
    t_ps = [Trk(True) for _ in range(7)]; t_psT = Trk(True)
    rot = {"i": 0}

    def nb(lst=(4, 5, 6)):
        rot["i"] += 1
        return lst[rot["i"] % len(lst)]
    ALLB = (0, 1, 2, 3, 4, 5, 6)
    wrot = {"i": 0}

    def wslot():
        wrot["i"] += 1
        return wrot["i"] % NW
    evrot = {"i": 0}

    def evac_copy(out_ap, in_ap, r, w=(), wa=()):
        evrot["i"] += 1
        if evrot["i"] % 2:
            S.op(act, lambda: nc.scalar.copy(out=out_ap, in_=in_ap), r=r, w=w, wa=wa)
        else:
            S.op(dve, lambda: nc.vector.tensor_copy(out=out_ap, in_=in_ap), r=r, w=w, wa=wa)

    S.op(pool, lambda: nc.gpsimd.memset(Vae[:], 1.0), w=t_Vae)
    S.op(pool, lambda: nc.gpsimd.memset(Vce[:], 1.0), w=t_Vce)
    for q in range(4):
        S.dma(acc[:, q * 1024:(q + 1) * 1024], rampv[0:1, q * 1024:(q + 1) * 1024].partition_broadcast(128), r=[t_in],
              wa=[t_acc])
    S.op(dve, lambda: nc.vector.tensor_copy(out=ramp[:], in_=acc[:]), r=[t_acc], w=[t_ramp])

    dbg_outs = {}

    chkc = {}

    marks = []

    def chk(tag):
        chkc[tag] = chkc.get(tag, 0) + 1
        marks.append((tag, chkc[tag], S.pe.count, S.act.count, S.dve.count, S.pool.count))
        if stop == tag or stop == "%s:%d" % (tag, chkc[tag]):
            raise _Stop()

    def dump(name, ap, trks, shape, dt=F32):
        if not dbg: return
        d = nc.dram_tensor("dbg_" + name, list(shape), dt, kind="ExternalOutput").ap()
        S.dma(d, ap, r=trks)
        dbg_outs[name] = shape

    def rmsnorm_to_hT(b):
        hn = hn2[b % 2]; t_hn = t_hn2[b % 2]
        S.op(act, lambda: nc.scalar.activation(out=hn[:], in_=xb[:, b, :], func=AF.Square, accum_out=ss[:, b:b + 1]),
             r=[t_xb[b]], w=[t_hn, t_ss[b]])
        S.op(dve, lambda: nc.vector.tensor_scalar(out=ms[:, b:b + 1], in0=ss[:, b:b + 1], scalar1=1.0 / D, scalar2=EPS,
                                                  op0=ALU.mult, op1=ALU.add), r=[t_ss[b]], w=[t_ms[b]])
        S.op(pool, lambda: nc.gpsimd.tensor_tensor(out=rstd[:, b:b + 1], in0=ms[:, b:b + 1], in1=neghalf[:, 0:1],
                                                   op=ALU.pow), r=[t_ms[b], t_cst], w=[t_rstd[b]])
        S.op(dve, lambda: nc.vector.tensor_scalar(out=hn[:], in0=xb[:, b, :], scalar1=rstd[:, b:b + 1], scalar2=None,
                                                  op0=ALU.mult), r=[t_xb[b], t_rstd[b]], w=[t_hn])
        for kc in range(8):
            S.op(pe, lambda kc=kc: nc.tensor.transpose(psT[:, kc * 128:(kc + 1) * 128], hn[:, kc * 128:(kc + 1) * 128],
                                                      ident_bf[:]), r=[t_hn, t_cst], w=[t_psT])
        evac_copy(hT[:, :, b * 128:(b + 1) * 128], psT[:, :].rearrange("p (k t) -> p k t", k=8), r=[t_psT], wa=[t_hT])

    def load_w(src_ap, nk, cw, prep_t):
        s = wslot()
        S.dma(wbuf[s][:, 0:nk, 0:cw], src_ap, r=[prep_t], w=[t_wbuf[s]])
        return s

    def layernorm_stats(in_ap):
        S.op(dve, lambda: nc.vector.bn_stats(out=st6[:], in_=in_ap[0]), r=in_ap[1], w=[t_ln])
        chk('LN1')
        S.op(dve, lambda: nc.vector.bn_aggr(out=mv[:], in_=st6[:]), r=[t_ln], w=[t_ln])
        chk('LN2')
        S.op(dve, lambda: nc.vector.tensor_scalar(out=lnr[:, 1:2], in0=mv[:, 1:2], scalar1=EPS, scalar2=None, op0=ALU.add),
             r=[t_ln], w=[t_ln])
        chk('LN3')
        S.op(pool, lambda: nc.gpsimd.tensor_tensor(out=lnr[:, 0:1], in0=lnr[:, 1:2], in1=neghalf[:, 0:1], op=ALU.pow),
             r=[t_ln, t_cst], w=[t_ln])

    try:
      for l in range(n_layers):
        lambda_init = 0.8 - 0.6 * math.exp(-0.3 * l)
        last = (l == n_layers - 1)
        xsrc = x_in if l == 0 else xs
        S.dma(idxg_bc[:], idx_g[l:l + 1, :].partition_broadcast(128), r=[t_in], w=[t_par])
        S.dma(idxb_bc[:], idx_b[l:l + 1, :].partition_broadcast(128), r=[t_in], wa=[t_par])
        S.dma(sgug_bc[:], sgu_g[l:l + 1, :].partition_broadcast(128), r=[t_in], wa=[t_par])
        S.dma(sgub_bc[:], sgu_b[l:l + 1, :].partition_broadcast(128), r=[t_in], wa=[t_par])
        S.dma(wsf[:], sgu_w[l].rearrange("g t s -> t g s"), r=[t_in], wa=[t_par])
        S.dma(bs[:], sgu_bs[l].rearrange("g t -> t g"), r=[t_in], wa=[t_par], allow_slow_non_contiguous=True)
        S.dma(lamt[:], dlam[l:l + 1].rearrange("o a b -> o (a b)").partition_broadcast(128), r=[t_in], wa=[t_par])
        S.dma(gsub[:], subln[l:l + 1, :].partition_broadcast(128), r=[t_in], wa=[t_par])
        for g in range(4):
            S.op(dve, lambda g=g: nc.vector.tensor_tensor(out=wsb[:, g, :], in0=wsf[:, g, :], in1=tril, op=ALU.mult),
                 r=[t_par, t_cst], w=[t_par])
        for g in range(4):
            S.op(pe, lambda g=g: nc.tensor.transpose(psT[:, g * 128:(g + 1) * 128], wsb[:, g, :], ident_bf[:]),
                 r=[t_par, t_cst], w=[t_psT])
        S.op(dve, lambda: nc.vector.tensor_copy(out=WcT[:], in_=psT[:, 0:512].rearrange("p (g t) -> p g t", g=4)),
             r=[t_psT], w=[t_par])
        lt4 = lamt[:].rearrange("p (a b) -> p a b", a=4)
        S.op(dve, lambda: nc.vector.tensor_tensor(out=lamp[:].rearrange("p (a b) -> p a b", a=2), in0=lt4[:, 0:4:2, :],
                                                  in1=lt4[:, 1:4:2, :], op=ALU.mult), r=[t_par], w=[t_par])
        S.op(dve, lambda: nc.vector.tensor_reduce(out=lam2[:], in_=lamp[:].rearrange("p (a b) -> p a b", a=2), axis=AX.X,
                                                  op=ALU.add), r=[t_par], w=[t_par])
        S.op(act, lambda: nc.scalar.activation(out=lam2[:], in_=lam2[:], func=AF.Exp), r=[t_par], w=[t_par])
        S.op(dve, lambda: nc.vector.tensor_tensor(out=neglam[:], in0=lam2[:, 1:2], in1=lam2[:, 0:1], op=ALU.subtract),
             r=[t_par], w=[t_par])
        S.op(dve, lambda: nc.vector.tensor_scalar(out=neglam[:], in0=neglam[:], scalar1=-lambda_init, scalar2=None,
                                                  op0=ALU.add), r=[t_par], w=[t_par])
        S.op(dve, lambda: nc.vector.tensor_scalar(out=gsub[:], in0=gsub[:], scalar1=(1.0 - lambda_init), scalar2=None,
                                                  op0=ALU.mult), r=[t_par], w=[t_par])

        chk('par')
        for j in range(n_tiles):
            nkb = 4 * j + 4
            for b in range(4):
                gb = 4 * j + b
                S.dma(xb[:, b, :], xsrc[gb * 128:(gb + 1) * 128, :], r=[t_in if l == 0 else t_xs[j]], w=[t_xb[b]])
            for b in range(4):
                rmsnorm_to_hT(b)

            chk('A')
            s1 = load_w(win_bf[l][:, :, TM_AV:TM_AV + 324].rearrange("k p c -> p k c"), 8, 324, t_prep[l])
            s2 = load_w(win_bf[l][:, :, TM_CV:TM_CV + 256].rearrange("k p c -> p k c"), 8, 256, t_prep[l])
            for b in range(4):
                gb = 4 * j + b
                bk = nb(ALLB)
                for kc in range(8):
                    S.op(pe, lambda kc=kc: nc.tensor.matmul(ps[bk][:, 0:324], lhsT=hT[:, kc, b * 128:(b + 1) * 128],
                                                            rhs=wbuf[s1][:, kc, 0:324], start=(kc == 0), stop=(kc == 7)),
                         r=[t_hT, t_wbuf[s1]], w=[t_ps[bk]])
                chk('B1a')
                evac_copy(Vae[:, gb, :].rearrange("p (h e) -> p h e", h=4)[:, :, 0:64],
                          ps[bk][:, 0:256].rearrange("p (h e) -> p h e", h=4), r=[t_ps[bk]], wa=[t_Vae[j]])
                chk('B1b')
                S.op(act, lambda: nc.scalar.copy(out=ln2[:, 64:128], in_=ps[bk][:, 256:320]), r=[t_ps[bk]], w=[t_ln2])
                chk('LN0')
                layernorm_stats((ln2[:, 64:128], [t_ln2]))
                chk('B1b1')
                S.op(dve, lambda: nc.vector.tensor_scalar(out=ln1[:, 0:64], in0=ln2[:, 64:128], scalar1=mv[:, 0:1],
                                                          scalar2=lnr[:, 0:1], op0=ALU.subtract, op1=ALU.mult),
                     r=[t_ln2, t_ln], w=[t_ln1])
                chk('B1b2')
                S.op(dve, lambda: nc.vector.tensor_tensor(out=ln2[:, 0:64], in0=ln1[:, 0:64], in1=idxg_bc[:], op=ALU.mult),
                     r=[t_ln1, t_par], w=[t_ln2])
                chk('B1b3')
                S.op(dve, lambda: nc.vector.tensor_tensor(out=knd[:, 0:64], in0=ln2[:, 0:64], in1=idxb_bc[:], op=ALU.add),
                     r=[t_ln2, t_par], w=[t_knd])
                S.op(dve, lambda: nc.vector.tensor_tensor(out=knd[:, 64:128], in0=ln2[:, 0:64], in1=idxb_bc[:], op=ALU.add),
                     r=[t_ln2, t_par], wa=[t_knd])
                chk('B1c0')
                S.op(pe, lambda: nc.tensor.transpose(psT[:, 0:128], knd[:], ident_bf[:]), r=[t_knd, t_cst], w=[t_psT])
                evac_copy(KiT[:, gb * 128:(gb + 1) * 128], psT[:, 0:128], r=[t_psT], wa=[t_KiT[j]])
                chk('B1c')
                S.op(dve, lambda: nc.vector.tensor_scalar(out=wsc[:, b * 4:b * 4 + 4], in0=ps[bk][:, 320:324],
                                                          scalar1=1.0 / 16.0, scalar2=None, op0=ALU.mult),
                     r=[t_ps[bk]], w=[t_iw[b]])
                S.op(dve, lambda: nc.vector.scalar_tensor_tensor(out=absw[:, b * 4:b * 4 + 4], in0=wsc[:, b * 4:b * 4 + 4],
                                                                 scalar=-1.0, in1=wsc[:, b * 4:b * 4 + 4], op0=ALU.mult,
                                                                 op1=ALU.max), r=[t_iw[b]], w=[t_iw[b]])
                S.op(dve, lambda: nc.vector.tensor_scalar(out=sgn[:, b * 4:b * 4 + 4], in0=wsc[:, b * 4:b * 4 + 4],
                                                          scalar1=0.0, scalar2=2.0, op0=ALU.is_ge, op1=ALU.mult),
                     r=[t_iw[b]], w=[t_iw[b]])
                S.op(dve, lambda: nc.vector.tensor_scalar(out=sgn[:, b * 4:b * 4 + 4], in0=sgn[:, b * 4:b * 4 + 4],
                                                          scalar1=-1.0, scalar2=None, op0=ALU.add),
                     r=[t_iw[b]], w=[t_iw[b]])
                chk('B1d')
                bk2 = nb(ALLB)
                for kc in range(8):
                    S.op(pe, lambda kc=kc: nc.tensor.matmul(ps[bk2][:, 0:256], lhsT=hT[:, kc, b * 128:(b + 1) * 128],
                                                            rhs=wbuf[s2][:, kc, 0:256], start=(kc == 0), stop=(kc == 7)),
                         r=[t_hT, t_wbuf[s2]], w=[t_ps[bk2]])
                evac_copy(Vce[:, gb, :].rearrange("p (h e) -> p h e", h=4)[:, :, 0:64],
                          ps[bk2][:, 0:256].rearrange("p (h e) -> p h e", h=4), r=[t_ps[bk2]], wa=[t_Vce[j]])
            chk('B1')
            chk('B1e')
            s3 = load_w(win_bf[l][:, :, TM_BUV:TM_BUV + 512].rearrange("k p c -> p k c"), 8, 512, t_prep[l])
            for b in range(4):
                bk = nb(ALLB)
                for kc in range(8):
                    S.op(pe, lambda kc=kc: nc.tensor.matmul(ps[bk][:, 0:512], lhsT=hT[:, kc, b * 128:(b + 1) * 128],
                                                            rhs=wbuf[s3][:, kc, 0:512], start=(kc == 0), stop=(kc == 7)),
                         r=[t_hT, t_wbuf[s3]], w=[t_ps[bk]])
                S.op(act, lambda: nc.scalar.activation(out=gbuf, in_=ps[bk][:, 0:512], func=AF.Gelu_apprx_tanh),
                     r=[t_ps[bk]], w=[t_gbuf])
                layernorm_stats((gbuf[:, 256:512], [t_gbuf]))
                S.op(dve, lambda: nc.vector.tensor_scalar(out=ln1[:], in0=gbuf[:, 256:512], scalar1=mv[:, 0:1],
                                                          scalar2=lnr[:, 0:1], op0=ALU.subtract, op1=ALU.mult),
                     r=[t_gbuf, t_ln], w=[t_ln1])
                S.op(pool, lambda: nc.gpsimd.tensor_tensor(out=ln2[:], in0=ln1[:], in1=sgug_bc[:], op=ALU.mult),
                     r=[t_ln1, t_par], w=[t_ln2])
                S.op(pool, lambda: nc.gpsimd.tensor_tensor(out=vn[:], in0=ln2[:], in1=sgub_bc[:], op=ALU.add),
                     r=[t_ln2, t_par], w=[t_vn])
                bk2 = nb(ALLB)
                for g in range(4):
                    S.op(pe, lambda g=g: nc.tensor.matmul(ps[bk2][:, g * 64:(g + 1) * 64], lhsT=WcT[:, g, :],
                                                          rhs=vn[:, g * 64:(g + 1) * 64], start=(g == 0), stop=(g == 3),
                                                          skip_group_check=True), r=[t_vn, t_par], w=[t_ps[bk2]])
                for g in range(4):
                    S.op(dve, lambda g=g: nc.vector.scalar_tensor_tensor(
                        out=ybt[:, g * 64:(g + 1) * 64], in0=ps[bk2][:, g * 64:(g + 1) * 64], scalar=bs[:, g:g + 1],
                        in1=gbuf[:, g * 64:(g + 1) * 64], op0=ALU.add, op1=ALU.mult),
                        r=[t_ps[bk2], t_par, t_gbuf], w=[t_ybt] if g == 0 else [], wa=[] if g == 0 else [t_ybt])
                for c in range(2):
                    S.op(pe, lambda c=c: nc.tensor.transpose(psT[:, c * 128:(c + 1) * 128], ybt[:, c * 128:(c + 1) * 128],
                                                            ident_bf[:]), r=[t_ybt, t_cst], w=[t_psT])
                evac_copy(yT[:, 1, :, b * 128:(b + 1) * 128], psT[:, 0:256].rearrange("p (c t) -> p c t", c=2),
                          r=[t_psT], wa=[t_yT[1]])
            chk('B2')
            fm = [(FM_AQ, 512, [("qa", 0), ("qa", 1), ("ka", 0), ("ka", 1)]),
                  (FM_IQ, 512, [("qi", 0), ("qi", 1), ("qc", 0), ("qc", 1)]),
                  (FM_CK, 256, [("kc", 0), ("kc", 1)])]
            for (c0, cw, dests) in fm:
                s = load_w(win_bf[l][:, :, c0:c0 + cw].rearrange("k p c -> p k c"), 8, cw, t_prep[l])
                for ci, (kind, c) in enumerate(dests):
                    bk = nb(ALLB)
                    for kc in range(8):
                        S.op(pe, lambda kc=kc: nc.tensor.matmul(ps[bk][:, 0:512], lhsT=wbuf[s][:, kc, ci * 128:(ci + 1) * 128],
                                                                rhs=hT[:, kc, :], start=(kc == 0), stop=(kc == 7)),
                             r=[t_hT, t_wbuf[s]], w=[t_ps[bk]])
                    if kind == "qa": evac_copy(QaT[:, c, :], ps[bk][:, :], r=[t_ps[bk]], wa=[t_QaT])
                    elif kind == "qi": evac_copy(QiT[:, c, :], ps[bk][:, :], r=[t_ps[bk]], wa=[t_QiT])
                    elif kind == "qc": evac_copy(QcT[:, c, :], ps[bk][:, :], r=[t_ps[bk]], wa=[t_QcT])
                    elif kind == "ka": evac_copy(KaT[:, c, j * 512:(j + 1) * 512], ps[bk][:, :], r=[t_ps[bk]], wa=[t_KaT[j]])
                    elif kind == "kc": evac_copy(KcT[:, c, j * 512:(j + 1) * 512], ps[bk][:, :], r=[t_ps[bk]], wa=[t_KcT[j]])

            chk('B3')
            def gen_C():
                for qb in range(4):
                    gb = 4 * j + qb
                    ncols = (gb + 1) * 128
                    for kg in range((ncols + 511) // 512):
                        c0 = kg * 512; cw = min(512, ncols - c0)
                        for h in range(4):
                            bk = nb((6,))
                            rs = slice((h % 2) * 64, (h % 2) * 64 + 64)
                            S.op(pe, lambda: nc.tensor.matmul(ps[bk][:, 0:cw], lhsT=QiT[rs, h // 2, qb * 128:(qb + 1) * 128],
                                                              rhs=KiT[rs, c0:c0 + cw], start=True, stop=True),
                                 r=[t_QiT] + t_KiT[0:j + 1], w=[t_ps[bk]])
                            S.op(act, lambda: nc.scalar.activation(out=ps[bk][:, 0:cw], in_=ps[bk][:, 0:cw], func=AF.Relu,
                                                                   scale=absw[:, qb * 4 + h:qb * 4 + h + 1]),
                                 r=[t_iw[qb]], w=[t_ps[bk]])
                            in1 = ramp[:, c0:c0 + cw] if h == 0 else acc[:, c0:c0 + cw]
                            S.op(dve, lambda: nc.vector.scalar_tensor_tensor(
                                out=acc[:, c0:c0 + cw], in0=ps[bk][:, 0:cw], scalar=sgn[:, qb * 4 + h:qb * 4 + h + 1], in1=in1,
                                op0=ALU.mult, op1=ALU.add), r=[t_ps[bk], t_iw[qb], t_ramp], w=[t_acc])
                            yield
                    S.op(dve, lambda: nc.vector.tensor_tensor(out=acc[:, gb * 128:(gb + 1) * 128],
                                                              in0=acc[:, gb * 128:(gb + 1) * 128], in1=causal_big, op=ALU.add),
                         r=[t_cst], w=[t_acc])
                    yield
                    if gb >= 2:
                        n1 = (int(ncols * 0.50) // 64) * 64; n2 = ncols - n1
                        S.op(dve, lambda: nc.vector.memset(mid[:], 0.0), w=[t_bis, t_mid])
                        for k in range(NBIS):
                            ck = RNG / (2.0 ** k)
                            S.op(dve, lambda: nc.vector.tensor_scalar(out=mb[:, qb, 0:n1], in0=acc[:, 0:n1],
                                                                      scalar1=mid[:, 0:1], scalar2=None, op0=ALU.is_ge,
                                                                      op1=ALU.add, accum_out=cntt[:]),
                                 r=[t_acc, t_mid], w=[t_bis], wa=[t_mb[qb]])
                            S.op(act, lambda: nc.scalar.activation(out=mb[:, qb, n1:ncols], in_=acc[:, n1:ncols], func=AF.Sign,
                                                                   scale=-1.0, bias=mid[:, 0:1], accum_out=sneg[:]),
                                 r=[t_acc, t_mid], w=[t_bisa], wa=[t_mb[qb]])
                            S.op(dve, lambda: nc.vector.scalar_tensor_tensor(out=ttt[:], in0=cntt[:], scalar=2.0, in1=sneg[:],
                                                                             op0=ALU.mult, op1=ALU.subtract),
                                 r=[t_bisa], w=[t_bis])
                            S.op(dve, lambda: nc.vector.tensor_scalar(out=ttt[:], in0=ttt[:], scalar1=512.0 - n2 - 0.5,
                                                                      scalar2=ck, op0=ALU.is_ge, op1=ALU.mult), w=[t_bis])
                            S.op(dve, lambda: nc.vector.scalar_tensor_tensor(out=mid[:], in0=ttt[:], scalar=-ck / 2.0,
                                                                             in1=mid[:], op0=ALU.add, op1=ALU.add),
                                 w=[t_bis, t_mid])
                            yield
                        cK = RNG / (2.0 ** NBIS)
                        S.op(dve, lambda: nc.vector.tensor_scalar(out=thr[:], in0=mid[:], scalar1=-cK, scalar2=None,
                                                                  op0=ALU.add), w=[t_bis])
                    else:
                        S.op(dve, lambda: nc.vector.memset(thr[:], -RNG), w=[t_bis])
                    S.op(dve, lambda: nc.vector.tensor_scalar(out=mb[:, qb, 0:ncols], in0=acc[:, 0:ncols], scalar1=thr[:, 0:1],
                                                              scalar2=MASKV, op0=ALU.is_lt, op1=ALU.mult),
                         r=[t_acc, t_bis], w=[t_mb[qb]])
                    yield

            def attention(kind, comp, bankset, LA):
                if kind == "a":
                    KT, QT, VE, tK, tQ, tV = KaT, QaT, Vae, t_KaT, t_QaT, t_Vae
                    scale = 64 ** -0.5
                else:
                    KT, QT, VE, tK, tQ, tV = KcT, QcT, Vce, t_KcT, t_QcT, t_Vce
                    scale = 32 ** -0.5
                steps = [(h, kb) for h in range(4) for kb in range(nkb)]

                def qk(i):
                    h, kb = steps[i]
                    r0 = max(kb - 4 * j, 0); c0 = r0 * 128
                    bk = nb(bankset)
                    if kind == "a":
                        rs = slice((h % 2) * 64, (h % 2) * 64 + 64); ch = h // 2; tp = None
                    else:
                        idx = h * 2 + comp
                        rs = slice((idx % 4) * 32, (idx % 4) * 32 + 32); ch = idx // 4; tp = ((idx % 4) * 32, 0)
                    kw = {} if tp is None else {"tile_position": tp}
                    S.op(pe, lambda: nc.tensor.matmul(ps[bk][:, c0:512], lhsT=KT[rs, ch, kb * 128:(kb + 1) * 128],
                                                      rhs=QT[rs, ch, c0:512], start=True, stop=False, **kw),
                         r=[tQ, tK[kb // 4]], w=[t_ps[bk]])
                    if kind == "a":
                        for qb in range(r0, 4):
                            S.op(pe, lambda qb=qb: nc.tensor.matmul(ps[bk][:, qb * 128:(qb + 1) * 128],
                                                                    lhsT=mb[:, qb, kb * 128:(kb + 1) * 128], rhs=ident_bf[:],
                                                                    start=False, stop=(qb == 3), skip_group_check=True),
                                 r=[t_mb[qb], t_cst], w=[t_ps[bk]])
                    else:
                        if kb >= 4 * j:
                            S.op(pe, lambda: nc.tensor.matmul(ps[bk][:, r0 * 128:(r0 + 1) * 128], lhsT=cmb_bf[:],
                                                              rhs=ident_bf[:], start=False, stop=True,
                                                              skip_group_check=True), r=[t_cst], w=[t_ps[bk]])
                    pi = i % NPT
                    S.op(act, lambda: nc.scalar.activation(out=PT[pi][:, c0:512], in_=ps[bk][:, c0:512], func=AF.Exp,
                                                           scale=scale), r=[t_ps[bk]], w=[t_PT[pi]])

                def pv(i):
                    h, kb = steps[i]
                    r0 = max(kb - 4 * j, 0)
                    pi = i % NPT
                    for qb in range(r0, 4):
                        S.op(pe, lambda qb=qb: nc.tensor.matmul(ps[qb][:, h * 65:(h + 1) * 65],
                                                                lhsT=PT[pi][:, qb * 128:(qb + 1) * 128],
                                                                rhs=VE[:, kb, h * 65:(h + 1) * 65],
                                                                start=(h == 0 and kb == 0), stop=(kb == 4 * j + qb),
                                                                skip_group_check=True),
                             r=[t_PT[pi], tV[kb // 4]], w=[t_ps[qb]])
                n = len(steps)
                for i in range(min(LA, n)): qk(i)
                for i in range(n):
                    if i + LA < n: qk(i + LA)
                    pv(i)
                    yield

            def out_views(qb):
                o4 = ps[qb][:, 0:260].rearrange("p (h e) -> p h e", h=4)
                return o4[:, :, 0:64], o4[:, :, 64:65]

            def transpose_to_yT(src, t_src, br, qb):
                for c in range(2):
                    S.op(pe, lambda c=c: nc.tensor.transpose(psT[:, c * 128:(c + 1) * 128], src[:, c * 128:(c + 1) * 128],
                                                            ident_bf[:]), r=[t_src, t_cst], w=[t_psT])
                evac_copy(yT[:, br, :, qb * 128:(qb + 1) * 128], psT[:, 0:256].rearrange("p (c t) -> p c t", c=2),
                          r=[t_psT], wa=[t_yT[br]])

            def gen_E():
                yield from attention("c", 0, (4, 5), 1)
                for qb in range(4):
                    ov, dv = out_views(qb)
                    S.op(dve, lambda: nc.vector.reciprocal(out=rden[:].rearrange("p (h o) -> p h o", o=1), in_=dv),
                         r=[t_ps[qb]], w=[t_rden])
                    S.op(dve, lambda: nc.vector.tensor_tensor(out=o1[:, qb, :].rearrange("p (h e) -> p h e", h=4), in0=ov,
                                                              in1=rden[:].rearrange("p (h o) -> p h o", o=1).to_broadcast([128, 4, 64]),
                                                              op=ALU.mult), r=[t_ps[qb], t_rden], w=[t_o1[qb]])
                yield
                yield from attention("c", 1, (4, 5), 1)
                for qb in range(4):
                    ov, dv = out_views(qb)
                    S.op(dve, lambda: nc.vector.reciprocal(out=rden[:].rearrange("p (h o) -> p h o", o=1), in_=dv),
                         r=[t_ps[qb]], w=[t_rden])
                    S.op(dve, lambda: nc.vector.tensor_scalar(out=rden[:], in0=rden[:], scalar1=neglam[:, 0:1], scalar2=None,
                                                              op0=ALU.mult), r=[t_par], w=[t_rden])
                    S.op(dve, lambda: nc.vector.tensor_tensor(out=oo[:].rearrange("p (h e) -> p h e", h=4), in0=ov,
                                                              in1=rden[:].rearrange("p (h o) -> p h o", o=1).to_broadcast([128, 4, 64]),
                                                              op=ALU.mult), r=[t_ps[qb], t_rden], w=[t_oo])
                    S.op(pool, lambda: nc.gpsimd.tensor_tensor(out=oo[:], in0=oo[:], in1=o1[:, qb, :], op=ALU.add),
                         r=[t_o1[qb]], w=[t_oo])
                    S.op(pool, lambda: nc.gpsimd.tensor_tensor(out=sq[:], in0=oo[:], in1=oo[:], op=ALU.mult), r=[t_oo], w=[t_sq])
                    S.op(dve, lambda: nc.vector.tensor_reduce(out=ssc[:], in_=sq[:].rearrange("p (h e) -> p h e", h=4), axis=AX.X,
                                                              op=ALU.add), r=[t_sq], w=[t_ssc])
                    S.op(dve, lambda: nc.vector.tensor_scalar(out=ssc[:], in0=ssc[:], scalar1=1.0 / 64.0, scalar2=EPS,
                                                              op0=ALU.mult, op1=ALU.add), w=[t_ssc])
                    S.op(pool, lambda: nc.gpsimd.tensor_tensor(out=ssc[:], in0=ssc[:], in1=neghalf[:, 0:1].to_broadcast([128, 4]),
                                                               op=ALU.pow), r=[t_cst], w=[t_ssc])
                    S.op(dve, lambda: nc.vector.tensor_tensor(out=sq[:].rearrange("p (h e) -> p h e", h=4),
                                                              in0=oo[:].rearrange("p (h e) -> p h e", h=4),
                                                              in1=ssc[:].rearrange("p (h o) -> p h o", o=1).to_broadcast([128, 4, 64]),
                                                              op=ALU.mult), r=[t_oo, t_ssc], w=[t_sq])
                    S.op(pool, lambda: nc.gpsimd.tensor_tensor(out=ybt[:].rearrange("p (h e) -> p h e", h=4),
                                                               in0=sq[:].rearrange("p (h e) -> p h e", h=4),
                                                               in1=gsub[:].rearrange("p (o e) -> p o e", o=1).to_broadcast([128, 4, 64]),
                                                               op=ALU.mult), r=[t_sq, t_par], w=[t_ybt])
                    transpose_to_yT(ybt, t_ybt, 2, qb)
                    yield

            nC = 0
            for qb in range(4):
                gb = 4 * j + qb
                nC += 4 * ((gb + 1 + 3) // 4) + 1 + (NBIS if gb >= 2 else 0) + 1
            nE = 2 * 4 * nkb + 1 + 4
            gC, gE = gen_C(), gen_E()
            eE = 0
            for ci in range(1, nC + 1):
                next(gC, None)
                target = (ci * nE) // nC
                while eE < target:
                    next(gE, None); eE += 1
            for _ in gC: pass
            for _ in gE: pass
            chk('C')
            for _ in attention("a", 0, (4, 5, 6), 2): pass
            for qb in range(4):
                ov, dv = out_views(qb)
                S.op(dve, lambda: nc.vector.reciprocal(out=rden[:].rearrange("p (h o) -> p h o", o=1), in_=dv),
                     r=[t_ps[qb]], w=[t_rden])
                S.op(dve, lambda: nc.vector.tensor_tensor(out=ybt[:].rearrange("p (h e) -> p h e", h=4), in0=ov,
                                                          in1=rden[:].rearrange("p (h o) -> p h o", o=1).to_broadcast([128, 4, 64]),
                                                          op=ALU.mult), r=[t_ps[qb], t_rden], w=[t_ybt])
                transpose_to_yT(ybt, t_ybt, 0, qb)
            chk('D')
            chk('E')
            for fc in range(8):
                if fc % 4 == 0:
                    S.dma(wbr_sb[:], wbr_bf[l][:, :, (fc // 4) * 512:(fc // 4 + 1) * 512].rearrange("k p c -> p k c"),
                          r=[t_prep[l]], w=[t_wbr])
                sg = load_w(win_bf[l][:, :, FM_G + fc * 384:FM_G + (fc + 1) * 384].rearrange("k p c -> p k c"), 8, 384,
                            t_prep[l])
                for i in range(3):
                    bkg = nb(ALLB)
                    for kc in range(8):
                        S.op(pe, lambda kc=kc: nc.tensor.matmul(ps[bkg][:, 0:512], lhsT=wbuf[sg][:, kc, i * 128:(i + 1) * 128],
                                                                rhs=hT[:, kc, :], start=(kc == 0), stop=(kc == 7)),
                             r=[t_hT, t_wbuf[sg]], w=[t_ps[bkg]])
                    S.op(act, lambda: nc.scalar.activation(out=sig[i], in_=ps[bkg][:, :], func=AF.Sigmoid),
                         r=[t_ps[bkg]], w=[t_sig[i]])
                    bkz = nb(ALLB)
                    for c in range(2):
                        S.op(pe, lambda c=c: nc.tensor.matmul(ps[bkz][:, 0:512],
                                                              lhsT=wbr_sb[:, i * 2 + c, (fc % 4) * 128:(fc % 4 + 1) * 128],
                                                              rhs=yT[:, i, c, :], start=(c == 0), stop=(c == 1)),
                             r=[t_yT[i], t_wbr], w=[t_ps[bkz]])
                    di = 0 if i == 0 else 1
                    S.op(dve, lambda: nc.vector.tensor_tensor(out=tm[di], in0=ps[bkz][:, :], in1=sig[i], op=ALU.mult),
                         r=[t_ps[bkz], t_sig[i]], w=[t_tm[di]])
                    if i == 1:
                        S.op(pool, lambda: nc.gpsimd.tensor_tensor(out=tm[0], in0=tm[0], in1=tm[1], op=ALU.add),
                             r=[t_tm[1]], w=[t_tm[0]])
                    if i == 2:
                        S.op(pool, lambda: nc.gpsimd.tensor_tensor(out=mergedT[:, fc, :], in0=tm[0], in1=tm[1],
                                                                   op=ALU.add), r=[t_tm[0], t_tm[1]], wa=[t_mT])
            for half in range(2):
                s = load_w(wout_bf[l][:, :, half * 512:(half + 1) * 512].rearrange("k p c -> p k c"), 8, 512, t_prep[l])
                for b in range(4):
                    bk = nb(ALLB)
                    for kc in range(8):
                        S.op(pe, lambda kc=kc: nc.tensor.matmul(ps[bk][:, 0:512], lhsT=mergedT[:, kc, b * 128:(b + 1) * 128],
                                                                rhs=wbuf[s][:, kc, :], start=(kc == 0), stop=(kc == 7)),
                             r=[t_mT, t_wbuf[s]], w=[t_ps[bk]])
                    S.op(dve, lambda: nc.vector.tensor_tensor(out=xb[:, b, half * 512:(half + 1) * 512], in0=ps[bk][:, :],
                                                              in1=xb[:, b, half * 512:(half + 1) * 512], op=ALU.add),
                         r=[t_ps[bk]], w=[t_xb[b]])

            chk('F')
            for b in range(4):
                rmsnorm_to_hT(b)
            ri = 0
            for ffh in range(2):
                for grp in range(4):
                    cb = (ffh * 4 + grp) * 512
                    s = load_w(wff1_bf[l][:, :, cb:cb + 512].rearrange("k p c -> p k c"), 8, 512, t_prep[l])
                    for cc in range(4):
                        bk = nb((4, 5, 6))
                        for kc in range(8):
                            S.op(pe, lambda kc=kc: nc.tensor.matmul(ps[bk][:, 0:512], lhsT=wbuf[s][:, kc, cc * 128:(cc + 1) * 128],
                                                                    rhs=hT[:, kc, :], start=(kc == 0), stop=(kc == 7)),
                                 r=[t_hT, t_wbuf[s]], w=[t_ps[bk]])
                        ri ^= 1
                        S.op(act, lambda: nc.scalar.activation(out=rt[ri], in_=ps[bk][:, :], func=AF.Relu),
                             r=[t_ps[bk]], w=[t_rt[ri]])
                        S.op(pool, lambda: nc.gpsimd.tensor_tensor(out=aT[:, grp * 4 + cc, :], in0=rt[ri], in1=rt[ri],
                                                                   op=ALU.mult), r=[t_rt[ri]], wa=[t_aT])
                for half in range(2):
                    for wg in range(2):
                        k0 = ffh * 16 + wg * 8
                        s = load_w(wff2_bf[l][k0:k0 + 8, :, half * 512:(half + 1) * 512].rearrange("k p c -> p k c"), 8, 512,
                                   t_prep[l])
                        for b in range(4):
                            for c in range(8):
                                S.op(pe, lambda c=c: nc.tensor.matmul(ps[b][:, 0:512],
                                                                      lhsT=aT[:, wg * 8 + c, b * 128:(b + 1) * 128],
                                                                      rhs=wbuf[s][:, c, :], start=(wg == 0 and c == 0),
                                                                      stop=(wg == 1 and c == 7)),
                                     r=[t_aT, t_wbuf[s]], w=[t_ps[b]])
                    for b in range(4):
                        S.op(dve, lambda: nc.vector.tensor_tensor(out=xb[:, b, half * 512:(half + 1) * 512], in0=ps[b][:, :],
                                                                  in1=xb[:, b, half * 512:(half + 1) * 512], op=ALU.add),
                             r=[t_ps[b]], w=[t_xb[b]])
            chk('G')
            if last:
                S.dma(gfin, fin_g[0:1, :].partition_broadcast(128), r=[t_in], w=[t_gfin])
            for b in range(4):
                gb = 4 * j + b
                if last:
                    hn = hn2[b % 2]; t_hn = t_hn2[b % 2]
                    S.op(act, lambda: nc.scalar.activation(out=hn[:], in_=xb[:, b, :], func=AF.Square,
                                                           accum_out=ss[:, b:b + 1]), r=[t_xb[b]], w=[t_hn, t_ss[b]])
                    S.op(dve, lambda: nc.vector.tensor_scalar(out=ms[:, b:b + 1], in0=ss[:, b:b + 1], scalar1=1.0 / D,
                                                              scalar2=EPS, op0=ALU.mult, op1=ALU.add), r=[t_ss[b]], w=[t_ms[b]])
                    S.op(pool, lambda: nc.gpsimd.tensor_tensor(out=rstd[:, b:b + 1], in0=ms[:, b:b + 1], in1=neghalf[:, 0:1],
                                                               op=ALU.pow), r=[t_ms[b], t_cst], w=[t_rstd[b]])
                    S.op(dve, lambda: nc.vector.scalar_tensor_tensor(out=xb[:, b, :], in0=xb[:, b, :], scalar=rstd[:, b:b + 1],
                                                                     in1=gfin, op0=ALU.mult, op1=ALU.mult),
                         r=[t_rstd[b], t_gfin], w=[t_xb[b]])
                    S.dma(out[gb * 128:(gb + 1) * 128, :], xb[:, b, :], r=[t_xb[b]])
                else:
                    S.dma(xs[gb * 128:(gb + 1) * 128, :], xb[:, b, :], r=[t_xb[b]], wa=[t_xs[j]])

    except _Stop:
        pass
    S.barrier()
    es.close()
    S.marks = marks
    return nc, S, dbg_outs


def make_consts():
    t = np.arange(128)[:, None]; s = np.arange(128)[None, :]
    ident = np.eye(128, dtype=np.float32)
    causal_big = np.where(s <= t, 0.0, -1e30).astype(np.float32)
    cmb = np.where(s <= t, 0.0, MASKV).astype(np.float32)
    tril = (s <= t).astype(np.float32)
    consts = np.concatenate([ident, causal_big, cmb, tril], axis=1).astype(np.float32)
    bits = (np.arange(SEQ) + (27 << 7)).astype(np.uint16)
    rampv = -(bits.view(ml_dtypes.bfloat16).astype(np.float32))[None, :]
    return np.ascontiguousarray(consts), np.ascontiguousarray(rampv.astype(np.float32))


_CACHE = {}


def kernel(**inputs):
    if "nc" not in _CACHE:
        _CACHE["nc"] = build_nc()[0]
    nc = _CACHE["nc"]
    consts, rampv = make_consts()
    shared = {}
    for k, v in inputs.items():
        if k == "x": continue
        a = np.ascontiguousarray(np.asarray(v, dtype=np.float32))
        if k == "final_norm_g": a = a.reshape(1, D)
        shared[k] = a
    shared["consts"] = consts; shared["rampv"] = rampv
    x = np.asarray(inputs["x"], dtype=np.float32)
    in_maps = []
    for c in range(8):
        m = dict(shared); m["x"] = np.ascontiguousarray(x[c]); in_maps.append(m)
    res = run_bass_kernel_spmd(nc, in_maps, core_ids=list(range(8)))
    return np.stack([np.asarray(res.results[c]["out"], dtype=np.float32) for c in range(8)], axis=0)
```
